# Optimizing a Trainium2 kernel written in Bass

```python
import jax, jax.numpy as jnp
from jax import lax
import numpy as np

D_MODEL = 4096
BATCH = 2
SEQ = 8192
DEPTH = 1
DEC_BATCH = 8
DEC_SEQ = 2048
PAST_LEN = 128

N_HEADS = 16
N_KV_HEADS = 4
HEAD_DIM = 128
ATTN_WIDTH = N_HEADS * HEAD_DIM
KV_WIDTH = N_KV_HEADS * HEAD_DIM
Q_BLOCK = 128
ROPE_THETA = 10000.0
GRID_W = 64
LRU_WIDTH = 2048
LRU_BLOCKS = 16
LRU_BLOCK_W = LRU_WIDTH // LRU_BLOCKS
CONV_W = 4
CONV_LEFT = 2
LRU_C = 8.0
LRU_A_MIN = 0.9
LRU_A_MAX = 0.999
PEER_HEADS = 8
PEER_N_KEYS = 128
PEER_N_EXPERTS = PEER_N_KEYS * PEER_N_KEYS
PEER_HALF = 128
PEER_QUERY_DIM = 2 * PEER_HALF
PEER_TOPK = 16
PEER_TOKEN_CHUNK = 128
N_MOD = 6
EPS = 1e-6
IN_COLS = ATTN_WIDTH + 2 * KV_WIDTH + 2 * LRU_WIDTH + 2 * D_MODEL

kernel_name = 'hybrid_rglru_gqa_peer_encoder'


def rmsnorm(x, g):
    xf = x.astype(jnp.float32)
    y = xf * lax.rsqrt(jnp.mean(xf * xf, axis=-1, keepdims=True) + EPS)
    return (y * g.astype(jnp.float32)).astype(x.dtype)


def modulate(h, shift, scale):
    return h * (1 + scale[:, None, :]) + shift[:, None, :]


def axial_rope_tables(seq_len, dtype):
    rows_count = seq_len // GRID_W
    rows = jnp.repeat(jnp.arange(rows_count), GRID_W)
    cols = jnp.tile(jnp.arange(GRID_W), rows_count)
    axis_dim = HEAD_DIM // 2
    inv_freq = ROPE_THETA ** (-jnp.arange(0, axis_dim, 2, dtype=jnp.float32) / axis_dim)
    ang_r = rows.astype(jnp.float32)[:, None] * inv_freq
    ang_c = cols.astype(jnp.float32)[:, None] * inv_freq
    f = lambda a: a[None, :, None, :].astype(dtype)
    return f(jnp.cos(ang_r)), f(jnp.sin(ang_r)), f(jnp.cos(ang_c)), f(jnp.sin(ang_c))


def rope_rotate(p, cos, sin):
    p1, p2 = jnp.split(p, 2, axis=-1)
    return jnp.concatenate([p1 * cos - p2 * sin, p2 * cos + p1 * sin], axis=-1)


def apply_axial_rope(x, tables):
    cos_r, sin_r, cos_c, sin_c = tables
    xr, xc = jnp.split(x, 2, axis=-1)
    return jnp.concatenate([rope_rotate(xr, cos_r, sin_r), rope_rotate(xc, cos_c, sin_c)], axis=-1)


def gqa_attention(q, k, v):
    B, S = q.shape[0], q.shape[1]
    G = N_HEADS // N_KV_HEADS
    nb = S // Q_BLOCK
    qb = q.reshape(B, nb, Q_BLOCK, N_KV_HEADS, G, HEAD_DIM).transpose(1, 0, 2, 3, 4, 5)
    scale = HEAD_DIM ** -0.5

    def one_block(qblk):
        s = jnp.einsum('bqkgd,bskd->bkgqs', qblk, k, preferred_element_type=jnp.float32) * scale
        p = jax.nn.softmax(s, axis=-1)
        return jnp.einsum('bkgqs,bskd->bqkgd', p.astype(v.dtype), v)

    o = lax.map(one_block, qb)
    return o.transpose(1, 0, 2, 3, 4, 5).reshape(B, S, ATTN_WIDTH)


def centred_depthwise_conv(x, w, b):
    S = x.shape[1]
    xp = jnp.pad(x, ((0, 0), (CONV_LEFT, CONV_W - 1 - CONV_LEFT), (0, 0)))
    out = b + xp[:, 0:S] * w[0]
    for j in range(1, CONV_W):
        out = out + xp[:, j:j + S] * w[j]
    return out


def block_diag(xb, w, b):
    B, S = xb.shape[0], xb.shape[1]
    return jnp.einsum('bsnc,ncd->bsnd', xb, w.astype(jnp.float32)).reshape(B, S, LRU_WIDTH) + b.astype(jnp.float32)


def rg_lru(x, lam, w_a, b_a, w_i, b_i, reverse):
    B, S, W = x.shape
    xf = x.astype(jnp.float32)
    xb = xf.reshape(B, S, LRU_BLOCKS, LRU_BLOCK_W)
    r = jax.nn.sigmoid(block_diag(xb, w_a, b_a))
    i = jax.nn.sigmoid(block_diag(xb, w_i, b_i))
    log_a = -LRU_C * r * jax.nn.softplus(-lam.astype(jnp.float32))
    a = jnp.exp(log_a)
    mult = jnp.sqrt(jnp.maximum(-jnp.expm1(2.0 * log_a), 0.0))
    u = mult * (i * xf)

    def combine(left, right):
        a1, b1 = left
        a2, b2 = right
        return a1 * a2, a2 * b1 + b2

    _, h = lax.associative_scan(combine, (a, u), axis=1, reverse=reverse)
    return h


def peer_ffn(h, w_q, sub_keys, expert_u, expert_v):
    B, S, D = h.shape
    T = B * S
    hf = h.reshape(T, D)
    q = (hf @ w_q).reshape(T, PEER_HEADS, 2, PEER_HALF)
    scores = jnp.einsum('thpc,hpnc->thpn', q, sub_keys, preferred_element_type=jnp.float32)
    s_top, i_top = lax.top_k(scores, PEER_TOPK)
    cand = (s_top[:, :, 0, :, None] + s_top[:, :, 1, None, :]).reshape(T, PEER_HEADS, PEER_TOPK * PEER_TOPK)
    cand_idx = (i_top[:, :, 0, :, None] * PEER_N_KEYS + i_top[:, :, 1, None, :]).reshape(T, PEER_HEADS, PEER_TOPK * PEER_TOPK)
    best_s, best_pos = lax.top_k(cand, PEER_TOPK)
    expert_idx = jnp.take_along_axis(cand_idx, best_pos, axis=-1)
    gates = jax.nn.softmax(best_s, axis=-1)
    HK = PEER_HEADS * PEER_TOPK
    n_chunks = T // PEER_TOKEN_CHUNK

    def chunk(args):
        xc, idx, g = args
        u = jnp.take(expert_u, idx, axis=0)
        act = jax.nn.gelu(jnp.einsum('cd,ced->ce', xc, u, preferred_element_type=jnp.float32), approximate=False)
        coef = (g * act).astype(expert_v.dtype)
        v = jnp.take(expert_v, idx, axis=0)
        return jnp.einsum('ce,ced->cd', coef, v)

    out = lax.map(chunk, (hf.reshape(n_chunks, PEER_TOKEN_CHUNK, D),
                          expert_idx.reshape(n_chunks, PEER_TOKEN_CHUNK, HK),
                          gates.reshape(n_chunks, PEER_TOKEN_CHUNK, HK)))
    return out.reshape(B, S, D).astype(h.dtype)


def encoder_layer(x, c, w_ada, b_ada, g_norm1, g_norm2, w_in, q_gain, k_gain, conv_w, conv_b,
                  lru_lam, lru_wa, lru_ba, lru_wi, lru_bi, w_attn_o, w_lru_o, w_out,
                  peer_wq, peer_keys, peer_u, peer_v):
    B, S, _ = x.shape
    mod = jax.nn.silu(c) @ w_ada + b_ada
    shift1, scale1, gate1, shift2, scale2, gate2 = jnp.split(mod, N_MOD, axis=-1)

    h = modulate(rmsnorm(x, g_norm1), shift1, scale1)
    proj = h @ w_in
    c0 = ATTN_WIDTH
    c1 = c0 + KV_WIDTH
    c2 = c1 + KV_WIDTH
    c3 = c2 + LRU_WIDTH
    c4 = c3 + LRU_WIDTH
    c5 = c4 + D_MODEL
    q, k, v, xl, yl, g_attn, g_lru = jnp.split(proj, [c0, c1, c2, c3, c4, c5], axis=-1)

    tables = axial_rope_tables(S, x.dtype)
    q = apply_axial_rope(rmsnorm(q.reshape(B, S, N_HEADS, HEAD_DIM), q_gain), tables)
    k = apply_axial_rope(rmsnorm(k.reshape(B, S, N_KV_HEADS, HEAD_DIM), k_gain), tables)
    v = v.reshape(B, S, N_KV_HEADS, HEAD_DIM)
    attn = gqa_attention(q, k, v)

    xc = centred_depthwise_conv(xl, conv_w, conv_b)
    hl = (rg_lru(xc, lru_lam[0], lru_wa[0], lru_ba[0], lru_wi[0], lru_bi[0], False)
          + rg_lru(xc, lru_lam[1], lru_wa[1], lru_ba[1], lru_wi[1], lru_bi[1], True))
    lru = (hl * jax.nn.gelu(yl.astype(jnp.float32), approximate=False)).astype(x.dtype)

    merged = jax.nn.sigmoid(g_attn) * (attn @ w_attn_o) + jax.nn.sigmoid(g_lru) * (lru @ w_lru_o)
    x = x + gate1[:, None, :] * (merged @ w_out)

    h2 = modulate(rmsnorm(x, g_norm2), shift2, scale2)
    x = x + gate2[:, None, :] * peer_ffn(h2, peer_wq, peer_keys, peer_u, peer_v)
    return x


def setup_inputs(seed: int = 0) -> dict:
    key = jax.random.key(seed)
    ks = jax.random.split(key, 32)
    nrm = lambda k, shape, s: jax.random.normal(k, shape, jnp.float32) * s
    a0 = jax.random.uniform(ks[12], (DEPTH, 2, LRU_WIDTH), jnp.float32, LRU_A_MIN, LRU_A_MAX)
    return {
        'x_prompt': nrm(ks[0], (BATCH, SEQ, D_MODEL), 1.0),
        'x_sample': nrm(ks[1], (DEC_BATCH, DEC_SEQ, D_MODEL), 1.0),
        'c_prompt': nrm(ks[2], (BATCH, D_MODEL), 1.0),
        'c_sample': nrm(ks[3], (DEC_BATCH, D_MODEL), 1.0),
        'w_ada': nrm(ks[4], (DEPTH, D_MODEL, N_MOD * D_MODEL), 0.5 * D_MODEL ** -0.5),
        'b_ada': nrm(ks[5], (DEPTH, N_MOD * D_MODEL), 0.01),
        'g_norm1': 1.0 + nrm(ks[6], (DEPTH, D_MODEL), 0.02),
        'g_norm2': 1.0 + nrm(ks[7], (DEPTH, D_MODEL), 0.02),
        'w_in': nrm(ks[8], (DEPTH, D_MODEL, IN_COLS), D_MODEL ** -0.5),
        'q_gain': 1.0 + nrm(ks[9], (DEPTH, HEAD_DIM), 0.02),
        'k_gain': 1.0 + nrm(ks[10], (DEPTH, HEAD_DIM), 0.02),
        'conv_w': nrm(ks[11], (DEPTH, CONV_W, LRU_WIDTH), CONV_W ** -0.5),
        'conv_b': nrm(ks[13], (DEPTH, LRU_WIDTH), 0.01),
        'lru_lam': jnp.log(a0) - jnp.log1p(-a0),
        'lru_wa': nrm(ks[14], (DEPTH, 2, LRU_BLOCKS, LRU_BLOCK_W, LRU_BLOCK_W), LRU_BLOCK_W ** -0.5),
        'lru_ba': nrm(ks[15], (DEPTH, 2, LRU_WIDTH), 0.01),
        'lru_wi': nrm(ks[16], (DEPTH, 2, LRU_BLOCKS, LRU_BLOCK_W, LRU_BLOCK_W), LRU_BLOCK_W ** -0.5),
        'lru_bi': nrm(ks[17], (DEPTH, 2, LRU_WIDTH), 0.01),
        'w_attn_o': nrm(ks[18], (DEPTH, ATTN_WIDTH, D_MODEL), ATTN_WIDTH ** -0.5),
        'w_lru_o': nrm(ks[19], (DEPTH, LRU_WIDTH, D_MODEL), LRU_WIDTH ** -0.5),
        'w_out': nrm(ks[20], (DEPTH, D_MODEL, D_MODEL), D_MODEL ** -0.5),
        'peer_wq': nrm(ks[21], (DEPTH, D_MODEL, PEER_HEADS * PEER_QUERY_DIM), D_MODEL ** -0.5),
        'peer_keys': nrm(ks[22], (DEPTH, PEER_HEADS, 2, PEER_N_KEYS, PEER_HALF), PEER_HALF ** -0.5),
        'peer_u': nrm(ks[23], (DEPTH, PEER_N_EXPERTS, D_MODEL), D_MODEL ** -0.5),
        'peer_v': nrm(ks[24], (DEPTH, PEER_N_EXPERTS, D_MODEL), 0.5),
    }


def reference(x_prompt, x_sample, c_prompt, c_sample, w_ada, b_ada, g_norm1, g_norm2, w_in,
              q_gain, k_gain, conv_w, conv_b, lru_lam, lru_wa, lru_ba, lru_wi, lru_bi,
              w_attn_o, w_lru_o, w_out, peer_wq, peer_keys, peer_u, peer_v):
    y_prompt = x_prompt
    y_sample = x_sample
    for l in range(DEPTH):
        layer_params = (w_ada[l], b_ada[l], g_norm1[l], g_norm2[l], w_in[l], q_gain[l], k_gain[l],
                        conv_w[l], conv_b[l], lru_lam[l], lru_wa[l], lru_ba[l], lru_wi[l], lru_bi[l],
                        w_attn_o[l], w_lru_o[l], w_out[l], peer_wq[l], peer_keys[l], peer_u[l], peer_v[l])
        y_prompt = encoder_layer(y_prompt, c_prompt, *layer_params)
        y_sample = encoder_layer(y_sample, c_sample, *layer_params)
    return (y_prompt, y_sample)
```

```python
import contextlib
import numpy as np
import concourse.bass as bass
import concourse.mybir as mybir
from concourse.bass_utils import run_bass_kernel_spmd

F32 = mybir.dt.float32
BF16 = mybir.dt.bfloat16
U32 = mybir.dt.uint32
I32 = mybir.dt.int32
AF = mybir.ActivationFunctionType
ALU = mybir.AluOpType
AX = mybir.AxisListType

D = 4096
TS = 2048
TC = 8192
NCOL = 15360
EPS = 1e-6
SEM_LIMIT = 30000
DBG = {}


class Op:
    __slots__ = ("eng", "fn", "deps", "dma", "signal", "sem", "val")

    def __init__(self, eng, fn, dma):
        self.eng = eng
        self.fn = fn
        self.dma = dma
        self.deps = ()
        self.signal = False
        self.sem = None
        self.val = 0


class Sched:
    def __init__(self, nc, es):
        self.nc = nc
        self.es = es
        self.engs = {"pe": nc.tensor, "act": nc.scalar, "dve": nc.vector, "pool": nc.gpsimd, "sp": nc.sync}
        self.ops = []
        self.last_w = {}
        self.readers = {}
        self.nsem = 0
        self.cur_sem = {}
        self.cnt = {}
        for e in ("pe", "act", "dve", "pool"):
            self.cur_sem[e] = self._new_sem()
            self.cnt[e] = 0
        self.slots = {"sp": [[self._new_sem(), 0] for _ in range(24)],
                      "pool": [[self._new_sem(), 0] for _ in range(16)]}
        self.slot_i = {"sp": 0, "pool": 0}
        self.seen = {e: {} for e in self.engs}
        self.last_op = {}
        self.trace = {e: [] for e in self.engs} if DBG.get("trace") else None

    def _new_sem(self):
        self.nsem += 1
        return self.es.enter_context(self.nc.semaphore(f"sm{self.nsem}"))

    def op(self, eng, fn, r=(), w=(), dma=False):
        w = list(w) + [k for k in r if isinstance(k, tuple) and k[0] in ("ps", "psb")]
        o = Op(eng, fn, dma)
        deps = {}
        for k in r:
            lw = self.last_w.get(k)
            if lw is not None:
                deps[id(lw)] = lw
        for k in w:
            lw = self.last_w.get(k)
            if lw is not None:
                deps[id(lw)] = lw
            rd = self.readers.get(k)
            if rd:
                for x in rd.values():
                    deps[id(x)] = x
        rk = ("d", id(o)) if dma else eng
        for k in r:
            self.readers.setdefault(k, {})[rk] = o
        for k in w:
            self.last_w[k] = o
            self.readers[k] = {}
        dl = []
        for d in deps.values():
            if eng == "pe" and not dma and d.eng == "pe" and not d.dma:
                continue
            d.signal = True
            dl.append(d)
        o.deps = dl
        self.ops.append(o)
        return o

    def _wait(self, E, sem, val):
        sn = self.seen[E]
        if sn.get(id(sem), 0) < val:
            self.engs[E].wait_ge(sem, val)
            sn[id(sem)] = val
            if self.trace is not None:
                self.trace[E].append(("w", id(sem), val))

    def flush(self, barrier=True):
        for lw in self.last_w.values():
            lw.signal = True
        for rd in self.readers.values():
            for x in rd.values():
                x.signal = True
        lastc = {}
        for o in self.ops:
            if not o.dma:
                lastc[o.eng] = o
        for o in lastc.values():
            o.signal = True
        for o in self.ops:
            E = o.eng
            eng = self.engs[E]
            for d in o.deps:
                self._wait(E, d.sem, d.val)
            if o.dma:
                sl = self.slots[E]
                i = self.slot_i[E]
                self.slot_i[E] = (i + 1) % len(sl)
                sem, uses = sl[i]
                if uses > 0:
                    self._wait(E, sem, 16 * uses)
                if 16 * (uses + 1) > SEM_LIMIT:
                    sem = self._new_sem()
                    uses = 0
                    sl[i][0] = sem
                ins = o.fn()
                ins.then_inc(sem, 16)
                if self.trace is not None:
                    self.trace[E].append(("s", id(sem), 16))
                uses += 1
                sl[i][1] = uses
                o.sem = sem
                o.val = 16 * uses
            else:
                ins = o.fn()
                if o.signal:
                    if self.cnt[E] >= SEM_LIMIT:
                        self.cur_sem[E] = self._new_sem()
                        self.cnt[E] = 0
                    self.cnt[E] += 1
                    ins.then_inc(self.cur_sem[E], 1)
                    if self.trace is not None:
                        self.trace[E].append(("s", id(self.cur_sem[E]), 1))
                    o.sem = self.cur_sem[E]
                    o.val = self.cnt[E]
            o.fn = None
        self.ops = []
        if barrier:
            for E in self.engs:
                for E2 in ("pe", "act", "dve", "pool"):
                    if E2 != E and self.cnt[E2] > 0:
                        self._wait(E, self.cur_sem[E2], self.cnt[E2])
                for q in ("sp", "pool"):
                    for sem, uses in self.slots[q]:
                        if uses > 0:
                            self._wait(E, sem, 16 * uses)

    def dma(self, out, in_, r, w, q="sp"):
        eng = self.engs[q]
        return self.op(q, lambda: eng.dma_start(out=out, in_=in_), r, w, dma=True)

    def mm(self, out, lhsT, rhs, start, stop, r, w):
        t = self.nc.tensor
        return self.op("pe", lambda: t.matmul(out, lhsT=lhsT, rhs=rhs, start=start, stop=stop), r, w)

    def tr(self, out, in_, ident, r, w):
        t = self.nc.tensor
        return self.op("pe", lambda: t.transpose(out=out, in_=in_, identity=ident), r, w)

    def act(self, out, in_, func, r, w, bias=None, scale=None, accum_out=None):
        a = self.nc.scalar
        kw = {}
        if bias is not None:
            kw["bias"] = bias
        if scale is not None:
            kw["scale"] = scale
        if accum_out is not None:
            kw["accum_out"] = accum_out
        return self.op("act", lambda: a.activation(out=out, in_=in_, func=func, **kw), r, w)

    def ts(self, out, in0, s1, s2, op0, op1, r, w, eng="dve"):
        e = self.engs[eng]
        if op1 is None:
            return self.op(eng, lambda: e.tensor_scalar(out=out, in0=in0, scalar1=s1, scalar2=None, op0=op0), r, w)
        return self.op(eng, lambda: e.tensor_scalar(out=out, in0=in0, scalar1=s1, scalar2=s2, op0=op0, op1=op1), r, w)

    def tt(self, out, in0, in1, op, r, w, eng="dve"):
        e = self.engs[eng]
        return self.op(eng, lambda: e.tensor_tensor(out=out, in0=in0, in1=in1, op=op), r, w)

    def stt(self, out, in0, scalar, in1, op0, op1, r, w):
        e = self.nc.vector
        return self.op("dve", lambda: e.scalar_tensor_tensor(out=out, in0=in0, scalar=scalar, in1=in1, op0=op0, op1=op1), r, w)

    def cp(self, out, in_, r, w, eng="dve"):
        e = self.engs[eng]
        if eng == "act":
            return self.op(eng, lambda: e.copy(out=out, in_=in_), r, w)
        return self.op(eng, lambda: e.tensor_copy(out=out, in_=in_), r, w)


def build(debug=False, dbg_names=(), stop=None):
    nc = bass.Bass("TRN2", target_bir_lowering=False)

    def din(name, shape, dt=F32):
        return nc.dram_tensor(name, list(shape), dt, kind="ExternalInput").ap()

    def dscr(name, shape, dt=F32):
        kind = "ExternalOutput" if name in dbg_names else "Internal"
        return nc.dram_tensor(name, list(shape), dt, kind=kind).ap()

    xs = din("xs", [TS, D])
    xo = din("xo", [TS, D])
    xc = din("xc", [TC, D])
    cT = din("cT", [128, 32, 2])
    w_ada = din("w_ada", [D, 6 * D])
    badaT = din("badaT", [128, 192])
    g1T = din("g1T", [128, 32])
    g2T = din("g2T", [128, 32])
    w_in = din("w_in", [D, NCOL])
    qkg = din("qkg", [128, 2])
    convwT = din("convwT", [128, 16, 4])
    convbT = din("convbT", [128, 16])
    lamT = din("lamT", [128, 32])
    baT = din("baT", [128, 32])
    biT = din("biT", [128, 32])
    lru_wa = din("lru_wa", [32, 128, 128])
    lru_wi = din("lru_wi", [32, 128, 128])
    w_attn_o = din("w_attn_o", [2048, D])
    w_lru_o = din("w_lru_o", [2048, D])
    w_out = din("w_out", [D, D])
    peer_wq = din("peer_wq", [D, 2048])
    keysT = din("keysT", [128, 16, 128])
    peer_u = din("peer_u", [16384, D])
    peer_v = din("peer_v", [16384, D])
    ropeC = din("ropeC", [128, TC])
    ropeS = din("ropeS", [128, TC])
    ropeCo = din("ropeCo", [128, TS])
    ropeSo = din("ropeSo", [128, TS])
    consts = din("consts", [128, 3, 128])
    iota_in = din("iota256", [128, 256])
    msel = din("msel", [128, 4])

    ys = nc.dram_tensor("ys", [TS, D], F32, kind="ExternalOutput").ap()
    yo = nc.dram_tensor("yo", [TS, D], F32, kind="ExternalOutput").ap()

    winb = dscr("winb", [120, 128, 32, 128], BF16)
    waob = dscr("waob", [32, 128, 16, 128], BF16)
    wlob = dscr("wlob", [32, 128, 16, 128], BF16)
    woutb = dscr("woutb", [8, 128, 32, 512], BF16)
    wqb = dscr("wqb", [16, 128, 32, 128], BF16)
    modd = dscr("modd", [2, 4, D])
    QT = dscr("QT", [2, 16, 128, TS], BF16)
    KT_s = dscr("KT_s", [4, 128, TS], BF16)
    KT_c = dscr("KT_c", [4, 128, TC], BF16)
    V_s = dscr("V_s", [TS, 512], BF16)
    V_c = dscr("V_c", [TC, 512], BF16)
    xl_s = dscr("xl_s", [16, 128, TS])
    xl_c = dscr("xl_c", [16, 128, TC])
    gy = dscr("gy", [2, 16, 128, TS])
    ga = dscr("ga", [2, 32, 128, TS])
    gl = dscr("gl", [2, 32, 128, TS])
    attnT = dscr("attnT", [2, 16, 128, TS], BF16)
    lruT = dscr("lruT", [2, 16, 128, TS], BF16)
    x1 = dscr("x1", [2 * TS, D])
    dbg = {}
    if debug:
        dbg["modT"] = nc.dram_tensor("dbg_modT", [128, 192, 2], F32, kind="ExternalOutput").ap()

    es = contextlib.ExitStack()
    with es:
        S = Sched(nc, es)
        nc._sched = S

        nm = [0]

        def sb(st, name, shape, dt=F32):
            nm[0] += 1
            return st.enter_context(nc.sbuf_tensor(f"{name}_{nm[0]}", list(shape), dt))

        ps = es.enter_context(nc.psum_tensor("ps", [128, 3072], F32))
        psb = es.enter_context(nc.psum_tensor("psb", [128, 2048], BF16))
        cst = sb(es, "cst", [128, 3, 128])
        identb = sb(es, "identb", [128, 128], BF16)
        onesb = sb(es, "onesb", [128, 128], BF16)
        modT = sb(es, "modT", [128, 192, 2])
        G1 = sb(es, "G1", [128, 32, 2])
        G2 = sb(es, "G2", [128, 32, 2])
        qkg_t = sb(es, "qkg_t", [128, 2])
        msel_t = sb(es, "msel_t", [128, 4])
        identf = cst[:, 0, :]
        rotT = cst[:, 1, :]
        onesf = cst[:, 2, :]

        def bank(i):
            return ps[:, i * 512:(i + 1) * 512]

        S.dma(cst[:], consts[:, :, :], [], ["cst"])
        S.dma(qkg_t[:], qkg[:, :], [], ["qkg"])
        S.dma(msel_t[:], msel[:, :], [], ["msel"])
        S.cp(identb[:], cst[:, 0, :], ["cst"], ["identb"])
        S.cp(onesb[:], cst[:, 2, :], ["cst"], ["onesb"])

        def cast_w(dst, src, nf, kc, cw):
            for f in range(nf):
                S.dma(dst[f], src[:, f * cw:(f + 1) * cw].rearrange("(kc p) c -> p kc c", p=128),
                      [], [(dst.tensor.name, f)], q="pool")
        cast_w(winb, w_in, 120, 32, 128)
        cast_w(waob, w_attn_o, 32, 16, 128)
        cast_w(wlob, w_lru_o, 32, 16, 128)
        cast_w(woutb, w_out, 8, 32, 512)
        cast_w(wqb, peer_wq, 16, 32, 128)

        with contextlib.ExitStack() as st:
            cT_t = sb(st, "cT_t", [128, 32, 2])
            scT = sb(st, "scT", [128, 32, 2])
            bada_t = sb(st, "bada_t", [128, 192])
            g1_t = sb(st, "g1_t", [128, 32])
            g2_t = sb(st, "g2_t", [128, 32])
            wa = [sb(st, f"wa{i}", [128, 32, 256]) for i in range(2)]
            tmpc = sb(st, "tmpc", [128, 32])
            tmpr = sb(st, "tmpr", [32, 128])
            S.dma(cT_t[:], cT[:, :, :], [], ["cT"])
            S.dma(bada_t[:], badaT[:, :], [], ["bada"])
            S.dma(g1_t[:], g1T[:, :], [], ["g1"])
            S.dma(g2_t[:], g2T[:, :], [], ["g2"])
            S.act(scT[:], cT_t[:], AF.Silu, ["cT"], ["scT"])
            for j2 in range(96):
                buf = wa[j2 % 2]
                S.dma(buf[:], w_ada[:, j2 * 256:(j2 + 1) * 256].rearrange("(kc p) c -> p kc c", p=128),
                      [], [("wa", j2 % 2)])
                for half in range(2):
                    j = j2 * 2 + half
                    b = j % 6
                    pst = ps[:, b * 512:b * 512 + 2]
                    for kc in range(32):
                        S.mm(pst, buf[:, kc, half * 128:(half + 1) * 128], scT[:, kc, :], kc == 0, kc == 31,
                             [("wa", j2 % 2), "scT"], [("ps", b)])
                    S.ts(modT[:, j, :], pst, bada_t[:, j:j + 1], None, ALU.add, None,
                         [("ps", b), "bada"], [("modT", j)])
            allmod = [("modT", j) for j in range(192)]
            S.ts(G1[:], modT[:, 32:64, :], 1.0, None, ALU.add, None, allmod, ["G1"])
            S.tt(G1[:], G1[:], g1_t[:].unsqueeze(2).to_broadcast([128, 32, 2]), ALU.mult, ["G1", "g1"], ["G1"])
            S.ts(G2[:], modT[:, 128:160, :], 1.0, None, ALU.add, None, allmod, ["G2"])
            S.tt(G2[:], G2[:], g2_t[:].unsqueeze(2).to_broadcast([128, 32, 2]), ALU.mult, ["G2", "g2"], ["G2"])
            srcs = [(modT, 64), (G2, 0), (modT, 96), (modT, 160)]
            for m in range(2):
                for sl, (srct, off) in enumerate(srcs):
                    S.cp(tmpc[:], srct[:, off:off + 32, m], allmod + ["G2"], ["tmpc"])
                    S.tr(ps[0:32, 0:128], tmpc[:], identf, ["tmpc", "cst"], [("ps", 0)])
                    S.cp(tmpr[:], ps[0:32, 0:128], [("ps", 0)], ["tmpr"])
                    S.dma(modd[m, sl].rearrange("(kc p) -> kc p", p=128), tmpr[:], ["tmpr"], [("modd", m, sl)])
            if debug:
                S.dma(dbg["modT"][:, :, :], modT[:], allmod, ["dbg_modT"])
            S.flush()

        def phase_c(tag, x_ap, T, m, cols, rc, rs, oq, ok, ov, oxl, sidx):
            with contextlib.ExitStack() as st:
                NXB = DBG.get("nxb", 2)
                xt = [sb(st, f"xt{i}", [128, D]) for i in range(NXB)]
                NXN = DBG.get("nxn", NXB)
                xn = [sb(st, f"xn{i}", [128, D], BF16) for i in range(NXN)]
                hT = [sb(st, f"hT{i}", [128, 32, 512], BF16) for i in range(DBG.get("nhT", 2))]
                wt = [sb(st, f"wt{i}", [128, 32, 128], BF16) for i in range(3)]
                ssq = sb(st, "ssq", [128, 2])
                rstd = sb(st, "rstd", [128, 2])
                sqjunk = sb(st, "sqjunk", [128, D], BF16)
                ct = [sb(st, f"ct{i}", [128, 512]) for i in range(2)]
                stt_ = [sb(st, f"st{i}", [128, 512]) for i in range(2)]
                sqf = sb(st, "sqf", [128, 512])
                rsf = sb(st, "rsf", [128, 512])
                qn = sb(st, "qn", [128, 512])
                t1 = sb(st, "t1", [128, 512])
                t2 = sb(st, "t2", [128, 512])
                ob = [sb(st, f"ob{i}", [128, 512], BF16) for i in range(2)]
                of = [sb(st, f"of{i}", [128, 512]) for i in range(2)]
                vT = sb(st, "vT", [128, 512], BF16)
                vtile = [sb(st, f"vtile{i}", [128, 4, 128], BF16) for i in range(2)]
                nblk = DBG.get("nblk", T // 512)
                if "kinds" in DBG:
                    cols = [c_ for c_ in cols if c_[1] in DBG["kinds"]][:DBG.get("ncols", 1000)]
                has_rope = any(kind in ("q", "k") for (_, kind, _) in cols)
                wcount = 0
                pcount = 0
                ocount = 0
                for b in range(nblk):
                    hb = hT[b % len(hT)]
                    hk = ("hT", b % len(hT))
                    for tt_ in range(DBG.get("ntile", 4)):
                        ti = b * 4 + tt_
                        xb = xt[ti % NXB]
                        xk = ("xt", ti % NXB)
                        nb_ = xn[ti % NXN]
                        nk = ("xn", ti % NXN)
                        S.dma(xb[:], x_ap[ti * 128:(ti + 1) * 128, :], [], [xk])
                        S.act(sqjunk[:], xb[:], AF.Square, [xk], ["sqjunk", ("ssq", ti % 2)], accum_out=ssq[:, ti % 2:ti % 2 + 1])
                        S.ts(rstd[:, ti % 2:ti % 2 + 1], ssq[:, ti % 2:ti % 2 + 1], 1.0 / D, EPS, ALU.mult, ALU.add,
                             [("ssq", ti % 2)], [("rstd", ti % 2)])
                        S.act(rstd[:, ti % 2:ti % 2 + 1], rstd[:, ti % 2:ti % 2 + 1], AF.Sqrt,
                              [("rstd", ti % 2)], [("rstd", ti % 2)])
                        S.op("dve", lambda ti=ti: nc.vector.reciprocal(out=rstd[:, ti % 2:ti % 2 + 1], in_=rstd[:, ti % 2:ti % 2 + 1]),
                             [("rstd", ti % 2)], [("rstd", ti % 2)])
                        S.act(nb_[:], xb[:], AF.Identity, [xk, ("rstd", ti % 2)], [nk], scale=rstd[:, ti % 2:ti % 2 + 1])
                        fes = DBG.get("fe", 9)
                        if tt_ >= DBG.get("fe_tiles", 4):
                            fes = 1
                        for g8 in range(DBG.get("ng8", 4) if fes >= 2 else 0):
                            pb = g8 % 2
                            for k8 in range(8):
                                kc = g8 * 8 + k8
                                S.tr(psb[:, pb * 1024 + k8 * 128: pb * 1024 + (k8 + 1) * 128],
                                     nb_[:, kc * 128:(kc + 1) * 128], identb[:], [nk, "identb"], [("psb", pb)])
                            for k8 in range(8 if fes >= 3 else 0):
                                kc = g8 * 8 + k8
                                src = psb[:, pb * 1024 + k8 * 128: pb * 1024 + (k8 + 1) * 128]
                                dst = hb[:, kc, tt_ * 128:(tt_ + 1) * 128]
                                if (pb == 0 and fes != 4) or fes == 3:
                                    S.ts(dst, src, G1[:, kc, m:m + 1], modT[:, kc, m:m + 1], ALU.mult, ALU.add,
                                         [("psb", pb), "G1"], [(hk, kc, tt_)])
                                else:
                                    S.act(dst, src, AF.Identity, [("psb", pb), "G1"], [(hk, kc, tt_)],
                                          bias=modT[:, kc, m:m + 1], scale=G1[:, kc, m:m + 1])
                    hkeys = [(hk, kc, t4) for kc in range(32) for t4 in range(4)]
                    if has_rope:
                        cb = ct[b % 2]
                        sbb = stt_[b % 2]
                        S.dma(cb[:], rc[:, b * 512:(b + 1) * 512], [], [("ct", b % 2)])
                        S.dma(sbb[:], rs[:, b * 512:(b + 1) * 512], [], [("st", b % 2)])
                    tsl = slice(b * 512, (b + 1) * 512)
                    for (f, kind, idx) in cols:
                        wb = wt[wcount % 3]
                        wk = ("wt", wcount % 3)
                        wcount += 1
                        S.dma(wb[:], winb[f], [("winb", f)], [wk])
                        pbk = pcount % 4
                        pcount += 1
                        pq = bank(pbk)
                        pk = ("ps", pbk)
                        for kc in range(32):
                            S.mm(pq, wb[:, kc, :], hb[:, kc, :], kc == 0, kc == 31,
                                 [wk] + [(hk, kc, t4) for t4 in range(4)], [pk])
                        if kind in ("q", "k"):
                            S.act(sqf[:], pq, AF.Square, [pk], ["sqf"])
                            S.mm(bank(4), onesf, sqf[:], True, True, ["sqf", "cst"], [("ps", 4)])
                            S.ts(rsf[:], bank(4), 1.0 / 128, EPS, ALU.mult, ALU.add, [("ps", 4)], ["rsf"])
                            S.act(rsf[:], rsf[:], AF.Sqrt, ["rsf"], ["rsf"])
                            S.op("dve", lambda: nc.vector.reciprocal(out=rsf[:], in_=rsf[:]), ["rsf"], ["rsf"])
                            gcol = 0 if kind == "q" else 1
                            S.stt(qn[:], pq, qkg_t[:, gcol:gcol + 1], rsf[:], ALU.mult, ALU.mult,
                                  [pk, "rsf", "qkg"], ["qn"])
                            S.mm(bank(5), rotT, qn[:], True, True, ["qn", "cst"], [("ps", 5)])
                            S.tt(t1[:], qn[:], cb[:], ALU.mult, ["qn", ("ct", b % 2)], ["t1"])
                            S.tt(t2[:], bank(5), sbb[:], ALU.mult, [("ps", 5), ("st", b % 2)], ["t2"])
                            o = ob[ocount % 2]
                            okk = ("ob", ocount % 2)
                            ocount += 1
                            S.tt(o[:], t1[:], t2[:], ALU.add, ["t1", "t2"], [okk], eng="pool")
                            dst = oq[idx][:, tsl] if kind == "q" else ok[idx][:, tsl]
                            S.dma(dst, o[:], [okk], [(tag, kind, idx, b)])
                        elif kind == "v":
                            S.cp(vT[:], pq, [pk], ["vT"], eng="act")
                            vt_ = vtile[idx % 2]
                            for t4 in range(4):
                                S.tr(psb[:, t4 * 128:(t4 + 1) * 128], vT[:, t4 * 128:(t4 + 1) * 128], identb[:],
                                     ["vT", "identb"], [("psb", 0)])
                            S.cp(vt_[:], psb[:, 0:512].rearrange("p (a d) -> p a d", a=4),
                                 [("psb", 0)], [("vtile", idx % 2)])
                            S.dma(ov[b * 512:(b + 1) * 512, idx * 128:(idx + 1) * 128].rearrange("(a p) d -> p a d", p=128),
                                  vt_[:], [("vtile", idx % 2)], [(tag, "v", idx, b)])
                        else:
                            o = of[ocount % 2]
                            okk = ("of", ocount % 2)
                            ocount += 1
                            if kind == "xl":
                                S.cp(o[:], pq, [pk], [okk], eng="act")
                                dst = oxl[idx][:, tsl]
                            elif kind == "yl":
                                S.act(o[:], pq, AF.Gelu, [pk], [okk])
                                dst = gy[sidx, idx][:, tsl]
                            elif kind == "ga":
                                S.act(o[:], pq, AF.Sigmoid, [pk], [okk])
                                dst = ga[sidx, idx][:, tsl]
                            else:
                                S.act(o[:], pq, AF.Sigmoid, [pk], [okk])
                                dst = gl[sidx, idx][:, tsl]
                            S.dma(dst, o[:], [okk], [(tag, kind, idx, b)])
                    S.flush(barrier=False)
                S.flush()

        cols_q = [(h, "q", h) for h in range(16)]
        cols_k = [(16 + g, "k", g) for g in range(4)]
        cols_v = [(20 + g, "v", g) for g in range(4)]
        cols_xl = [(24 + n, "xl", n) for n in range(16)]
        cols_yl = [(40 + n, "yl", n) for n in range(16)]
        cols_ga = [(56 + n, "ga", n) for n in range(32)]
        cols_gl = [(88 + n, "gl", n) for n in range(32)]

        if stop == "A":
            return nc
        phase_c("S", xs, TS, 0, cols_q + cols_k + cols_v + cols_xl + cols_yl + cols_ga + cols_gl,
                ropeC, ropeS, QT[0], KT_s, V_s, xl_s, 0)
        if stop == "CS":
            return nc
        phase_c("O", xo, TS, 1, cols_q + cols_yl + cols_ga + cols_gl, ropeCo, ropeSo, QT[1], None, None, None, 1)
        phase_c("C", xc, TC, 1, cols_k + cols_v + cols_xl, ropeC, ropeS, None, KT_c, V_c, xl_c, 1)

        def phase_d(tag, xl_ap, T, sidx, use_sel, tag_xl, gy_tag):
            nq = T // TS
            with contextlib.ExitStack() as st:
                cw = sb(st, "cw", [128, 16, 4])
                cbt = sb(st, "cbt", [128, 16])
                lam = sb(st, "lam", [128, 32])
                cA = sb(st, "cA", [128, 32])
                cA2 = sb(st, "cA2", [128, 32])
                ba_t = sb(st, "ba_t", [128, 32])
                bi_t = sb(st, "bi_t", [128, 32])
                wa_t = sb(st, "wa_t", [128, 32, 128])
                wi_t = sb(st, "wi_t", [128, 32, 128])
                xpad = sb(st, "xpad", [128, TS + 3])
                xcv = sb(st, "xcv", [128, TS])
                rg = sb(st, "rg", [128, TS])
                ig = sb(st, "ig", [128, TS])
                a_t = sb(st, "a_t", [128, TS])
                mu = sb(st, "mu", [128, TS])
                hh = sb(st, "hh", [128, TS])
                acc = sb(st, "acc", [128, TS])
                gyt = sb(st, "gyt", [128, TS])
                lo = sb(st, "lo", [128, TS], BF16)
                stt = sb(st, "stt", [128, 2])
                S.dma(cw[:], convwT[:, :, :], [], ["cw"])
                S.dma(cbt[:], convbT[:, :], [], ["cbt"])
                S.dma(lam[:], lamT[:, :], [], ["lam"])
                S.dma(ba_t[:], baT[:, :], [], ["ba"])
                S.dma(bi_t[:], biT[:, :], [], ["bi"])
                S.dma(wa_t[:], lru_wa.rearrange("n c d -> c n d"), [], ["wa_t"])
                S.dma(wi_t[:], lru_wi.rearrange("n c d -> c n d"), [], ["wi_t"])
                S.act(cA[:], lam[:], AF.Exp, ["lam"], ["cA"], scale=-1.0)
                S.act(cA[:], cA[:], AF.Ln, ["cA"], ["cA"], bias=1.0)
                S.ts(cA[:], cA[:], -8.0, None, ALU.mult, None, ["cA"], ["cA"])
                S.ts(cA2[:], cA[:], 2.0, None, ALU.mult, None, ["cA"], ["cA2"])
                for n in range(16):
                    first = True
                    for dr in range(2):
                        gi = dr * 16 + n
                        S.op("pool", lambda dr=dr: nc.gpsimd.memset(stt[:, dr:dr + 1], 0.0), [], [("stt", dr)])
                        qs = range(nq) if dr == 0 else range(nq - 1, -1, -1)
                        for q in qs:
                            lo_t = q * TS - 2
                            hi_t = q * TS + TS + 1
                            a0 = max(lo_t, 0)
                            a1 = min(hi_t, T)
                            if lo_t < 0:
                                S.op("pool", lambda: nc.gpsimd.memset(xpad[:, 0:2], 0.0), [], ["xpad"])
                            if hi_t > T:
                                S.op("pool", lambda: nc.gpsimd.memset(xpad[:, TS + 2:TS + 3], 0.0), [], ["xpad"])
                            S.dma(xpad[:, a0 - lo_t:a1 - lo_t], xl_ap[n][:, a0:a1],
                                  [(tag_xl, "xl", n, bb) for bb in range(T // 512)], ["xpad"])
                            S.ts(xcv[:], xpad[:, 0:TS], cw[:, n, 0:1], cbt[:, n:n + 1], ALU.mult, ALU.add,
                                 ["xpad", "cw", "cbt"], ["xcv"])
                            for j in range(1, 4):
                                S.stt(xcv[:], xpad[:, j:j + TS], cw[:, n, j:j + 1], xcv[:], ALU.mult, ALU.add,
                                      ["xpad", "cw", "xcv"], ["xcv"])
                            for (wt_, bt_, gt_, gk, wkey, bkey) in ((wa_t, ba_t, rg, "rg", "wa_t", "ba"), (wi_t, bi_t, ig, "ig", "wi_t", "bi")):
                                for half in range(2):
                                    pb0 = 0 if gk == "rg" else 2
                                    for blk in range(2):
                                        c0 = half * 1024 + blk * 512
                                        S.mm(bank(pb0 + blk), wt_[:, gi, :], xcv[:, c0:c0 + 512], True, True,
                                             ["xcv", wkey], [("ps", pb0 + blk)])
                                    S.act(gt_[:, half * 1024:(half + 1) * 1024], ps[:, pb0 * 512:(pb0 + 2) * 512],
                                          AF.Sigmoid, [("ps", pb0), ("ps", pb0 + 1), bkey], [gk],
                                          bias=bt_[:, gi:gi + 1])
                            S.act(a_t[:], rg[:], AF.Exp, ["rg", "cA"], ["a_t"], scale=cA[:, gi:gi + 1])
                            S.act(mu[:], rg[:], AF.Exp, ["rg", "cA2"], ["mu"], scale=cA2[:, gi:gi + 1])
                            S.ts(mu[:], mu[:], -1.0, 1.0, ALU.mult, ALU.add, ["mu"], ["mu"])
                            S.ts(mu[:], mu[:], 0.0, None, ALU.max, None, ["mu"], ["mu"])
                            S.act(mu[:], mu[:], AF.Sqrt, ["mu"], ["mu"])
                            S.tt(ig[:], ig[:], xcv[:], ALU.mult, ["ig", "xcv"], ["ig"], eng="pool")
                            S.tt(ig[:], ig[:], mu[:], ALU.mult, ["ig", "mu"], ["ig"], eng="pool")
                            if dr == 0:
                                S.op("dve", lambda: nc.vector.tensor_tensor_scan(
                                    out=hh[:], data0=a_t[:], data1=ig[:], initial=stt[:, 0:1], op0=ALU.mult, op1=ALU.add),
                                    ["a_t", "ig", ("stt", 0)], ["hh"])
                                S.cp(stt[:, 0:1], hh[:, TS - 1:TS], ["hh"], [("stt", 0)])
                            else:
                                S.op("dve", lambda: nc.vector.tensor_tensor_scan(
                                    out=hh[:, ::-1], data0=a_t[:, ::-1], data1=ig[:, ::-1], initial=stt[:, 1:2],
                                    op0=ALU.mult, op1=ALU.add),
                                    ["a_t", "ig", ("stt", 1)], ["hh"])
                                S.cp(stt[:, 1:2], hh[:, 0:1], ["hh"], [("stt", 1)])
                            sel = msel_t[:, q:q + 1] if use_sel else 1.0
                            if first:
                                S.ts(acc[:], hh[:], sel, None, ALU.mult, None, ["hh", "msel"], ["acc"])
                                first = False
                            else:
                                S.stt(acc[:], hh[:], sel, acc[:], ALU.mult, ALU.add, ["hh", "msel", "acc"], ["acc"])
                    S.dma(gyt[:], gy[sidx, n], [(gy_tag, "yl", n, bb) for bb in range(4)], ["gyt"])
                    S.tt(lo[:], acc[:], gyt[:], ALU.mult, ["acc", "gyt"], ["lo"])
                    S.dma(lruT[sidx, n], lo[:], ["lo"], [("lruT", sidx, n)])
                    S.flush(barrier=False)
                S.flush()

        if stop == "C":
            return nc
        phase_d("S", xl_s, TS, 0, False, "S", "S")
        if stop == "DS":
            return nc
        phase_d("C", xl_c, TC, 1, True, "C", "O")

        def phase_e(sidx, qtag, ktag, KT_ap, V_ap, Tk):
            nkt = Tk // 128
            with contextlib.ExitStack() as st:
                kT = sb(st, "kT", [128, Tk], BF16)
                vt = sb(st, "vt", [128, nkt, 128], BF16)
                qT = [sb(st, f"qT{i}", [128, TS], BF16) for i in range(2)]
                pT = [sb(st, f"pT{i}", [128, 512], BF16) for i in range(3)]
                rden = sb(st, "rden", [128, 512])
                ot = [sb(st, f"ot{i}", [128, 512], BF16) for i in range(2)]
                sc = 128.0 ** -0.5
                hcount = 0
                ucount = 0
                for g in range(4):
                    S.dma(kT[:], KT_ap[g], [(ktag, "k", g, bb) for bb in range(Tk // 512)], ["kT"])
                    S.dma(vt[:], V_ap[:, g * 128:(g + 1) * 128].rearrange("(kt p) d -> p kt d", p=128),
                          [(ktag, "v", g, bb) for bb in range(Tk // 512)], ["vt"])
                    for hh_ in range(4):
                        h = g * 4 + hh_
                        qb_ = qT[hcount % 2]
                        qk = ("qT", hcount % 2)
                        hcount += 1
                        S.dma(qb_[:], QT[sidx, h], [(qtag, "q", h, bb) for bb in range(4)], [qk])
                        for qb in range(4):
                            po = bank(2 + ucount % 2)
                            pok = ("ps", 2 + ucount % 2)
                            pd = bank(4 + ucount % 2)
                            pdk = ("ps", 4 + ucount % 2)
                            ucount += 1
                            qsl = qb_[:, qb * 512:(qb + 1) * 512]

                            def qk_mm(kt):
                                S.mm(bank(kt % 2), kT[:, kt * 128:(kt + 1) * 128], qsl, True, True,
                                     ["kT", qk], [("ps", kt % 2)])
                            qk_mm(0)
                            for kt in range(nkt):
                                if kt + 1 < nkt:
                                    qk_mm(kt + 1)
                                p_ = pT[kt % 3]
                                pk_ = ("pT", kt % 3)
                                S.act(p_[:], bank(kt % 2), AF.Exp, [("ps", kt % 2)], [pk_], scale=sc)
                                S.mm(po, vt[:, kt, :], p_[:], kt == 0, kt == nkt - 1, ["vt", pk_], [pok])
                                S.mm(pd, onesb[:], p_[:], kt == 0, kt == nkt - 1, ["onesb", pk_], [pdk])
                            S.op("dve", lambda pd=pd: nc.vector.reciprocal(out=rden[:], in_=pd), [pdk], ["rden"])
                            o_ = ot[ucount % 2]
                            S.tt(o_[:], po, rden[:], ALU.mult, [pok, "rden"], [("ot", ucount % 2)])
                            S.dma(attnT[sidx, h][:, qb * 512:(qb + 1) * 512], o_[:], [("ot", ucount % 2)],
                                  [("attnT", sidx, h, qb)])
                        S.flush(barrier=False)
                S.flush()

        if stop == "D":
            return nc
        phase_e(0, "S", "S", KT_s, V_s, TS)
        if stop == "ES":
            return nc
        phase_e(1, "O", "C", KT_c, V_c, TC)

        if stop == "E":
            return nc
        with contextlib.ExitStack() as st:
            aT = sb(st, "aT", [128, 16, 512], BF16)
            lT = sb(st, "lT", [128, 16, 512], BF16)
            mg = sb(st, "mg", [128, 32, 512], BF16)
            wao_t = [sb(st, f"wao{i}", [128, 16, 128], BF16) for i in range(2)]
            wlo_t = [sb(st, f"wlo{i}", [128, 16, 128], BF16) for i in range(2)]
            ga_t = [sb(st, f"ga_t{i}", [128, 512]) for i in range(2)]
            gl_t = [sb(st, f"gl_t{i}", [128, 512]) for i in range(2)]
            f1 = sb(st, "f1", [128, 512])
            f2 = sb(st, "f2", [128, 512])
            wo_t = sb(st, "wo_t", [128, 32, 512], BF16)
            g1bc = sb(st, "g1bc", [128, D])
            xx = [sb(st, f"xx{i}", [128, 512]) for i in range(2)]
            xo_ = [sb(st, f"xo_{i}", [128, 512]) for i in range(2)]
            cnt = 0
            for sidx in range(2):
                x_ap = xs if sidx == 0 else xo
                S.dma(g1bc[:], modd[sidx, 0].partition_broadcast(128), [("modd", sidx, 0)], ["g1bc"])
                for b in range(4):
                    tsl = slice(b * 512, (b + 1) * 512)
                    S.dma(aT[:], attnT[sidx][:, :, tsl].rearrange("h p t -> p h t"),
                          [("attnT", sidx, h, b) for h in range(16)], ["aT"])
                    S.dma(lT[:], lruT[sidx][:, :, tsl].rearrange("h p t -> p h t"),
                          [("lruT", sidx, n) for n in range(16)], ["lT"])
                    for f in range(32):
                        i2 = f % 2
                        S.dma(wao_t[i2][:], waob[f], [("waob", f)], [("wao", i2)])
                        S.dma(wlo_t[i2][:], wlob[f], [("wlob", f)], [("wlo", i2)])
                        S.dma(ga_t[i2][:], ga[sidx, f][:, tsl], [("S" if sidx == 0 else "O", "ga", f, b)], [("ga_t", i2)])
                        S.dma(gl_t[i2][:], gl[sidx, f][:, tsl], [("S" if sidx == 0 else "O", "gl", f, b)], [("gl_t", i2)])
                        pB = bank(i2 * 2)
                        pA = bank(i2 * 2 + 1)
                        for kc in range(16):
                            S.mm(pB, wao_t[i2][:, kc, :], aT[:, kc, :], kc == 0, kc == 15, [("wao", i2), "aT"], [("ps", i2 * 2)])
                        for kc in range(16):
                            S.mm(pA, wlo_t[i2][:, kc, :], lT[:, kc, :], kc == 0, kc == 15, [("wlo", i2), "lT"], [("ps", i2 * 2 + 1)])
                        S.tt(f1[:], pB, ga_t[i2][:], ALU.mult, [("ps", i2 * 2), ("ga_t", i2)], ["f1"])
                        S.tt(f2[:], pA, gl_t[i2][:], ALU.mult, [("ps", i2 * 2 + 1), ("gl_t", i2)], ["f2"])
                        S.tt(mg[:, f, :], f1[:], f2[:], ALU.add, ["f1", "f2"], [("mg", f)], eng="pool")
                    mgk = [("mg", f) for f in range(32)]
                    for nch in range(8):
                        S.dma(wo_t[:], woutb[nch], [("woutb", nch)], ["wo_t"])
                        csl = slice(nch * 512, (nch + 1) * 512)
                        for t4 in range(4):
                            i2 = cnt % 2
                            cnt += 1
                            r0 = b * 512 + t4 * 128
                            S.dma(xx[i2][:], x_ap[r0:r0 + 128, csl], [], [("xx", i2)])
                            po = bank(4 + i2)
                            for kc in range(32):
                                S.mm(po, mg[:, kc, t4 * 128:(t4 + 1) * 128], wo_t[:, kc, :], kc == 0, kc == 31,
                                     mgk + ["wo_t"], [("ps", 4 + i2)])
                            S.tt(xo_[i2][:], po, g1bc[:, csl], ALU.mult, [("ps", 4 + i2), "g1bc"], [("xo_", i2)])
                            S.tt(xo_[i2][:], xo_[i2][:], xx[i2][:], ALU.add, [("xo_", i2), ("xx", i2)], [("xo_", i2)], eng="pool")
                            S.dma(x1[sidx * TS + r0: sidx * TS + r0 + 128, csl], xo_[i2][:], [("xo_", i2)],
                                  [("x1", sidx * 16 + b * 4 + t4, nch)])
                    S.flush(barrier=False)
            S.flush()

        if stop == "F":
            return nc
        with contextlib.ExitStack() as st:
            x1t = sb(st, "x1t", [128, D])
            h2 = sb(st, "h2", [128, D])
            accp = sb(st, "accp", [128, D])
            gb = [sb(st, f"gb{i}", [128, D]) for i in range(3)]
            h2b = sb(st, "h2b", [128, D], BF16)
            h2T = sb(st, "h2T", [128, 32, 128], BF16)
            wq_t = [sb(st, f"wq_t{i}", [128, 32, 128], BF16) for i in range(2)]
            qTp = sb(st, "qTp", [128, 16, 128])
            keys_t = sb(st, "keys_t", [128, 16, 128])
            scs = sb(st, "scs", [128, 16, 128])
            wk = sb(st, "wk", [128, 256])
            stop_ = sb(st, "stop_", [128, 16, 16])
            itop = sb(st, "itop", [128, 16, 16], U32)
            itf = sb(st, "itf", [128, 16, 16])
            i0s = sb(st, "i0s", [128, 8, 16])
            cand = sb(st, "cand", [128, 8, 256])
            cidx = sb(st, "cidx", [128, 8, 256])
            bs = sb(st, "bs", [128, 8, 16])
            posu = sb(st, "posu", [128, 8, 16], U32)
            posf = sb(st, "posf", [128, 8, 16])
            eidx = sb(st, "eidx", [128, 128])
            eidi = sb(st, "eidi", [128, 128], I32)
            gsum = sb(st, "gsum", [128, 8])
            gates = sb(st, "gates", [128, 128])
            dots = sb(st, "dots", [128, 128])
            coef = sb(st, "coef", [128, 128])
            iot = sb(st, "iot", [128, 256])
            ssq2 = sb(st, "ssq2", [128, 1])
            rstd2 = sb(st, "rstd2", [128, 1])
            S.dma(keys_t[:], keysT[:, :, :], [], ["keys_t"])
            S.dma(iot[:], iota_in[:, :], [], ["iot"])
            V = nc.vector
            gcount = 0

            def top16(src_ap, n, vals_ap, idx_ap, rk):
                S.op("dve", lambda: V.max(out=vals_ap[:, 0:8], in_=src_ap), rk, ["tv"])
                S.op("dve", lambda: V.max_index(out=idx_ap[:, 0:8], in_max=vals_ap[:, 0:8], in_values=src_ap),
                     rk + ["tv"], ["ti"])
                S.op("dve", lambda: V.match_replace(out=wk[:, 0:n], in_to_replace=vals_ap[:, 0:8], in_values=src_ap,
                                                    imm_value=-1e30), rk + ["tv"], ["wk"])
                S.op("dve", lambda: V.max(out=vals_ap[:, 8:16], in_=wk[:, 0:n]), ["wk"], ["tv"])
                S.op("dve", lambda: V.max_index(out=idx_ap[:, 8:16], in_max=vals_ap[:, 8:16], in_values=wk[:, 0:n]),
                     ["wk", "tv"], ["ti"])

            for ti in range(32):
                sidx = ti // 16
                y_ap = ys if sidx == 0 else yo
                rloc = (ti % 16) * 128
                S.dma(x1t[:], x1[ti * 128:(ti + 1) * 128, :], [("x1", ti, nch) for nch in range(8)], ["x1t"])
                S.dma(gb[0][:], modd[sidx, 1].partition_broadcast(128), [("modd", sidx, 1)], [("gb", 0)])
                S.dma(gb[1][:], modd[sidx, 2].partition_broadcast(128), [("modd", sidx, 2)], [("gb", 1)])
                S.act(gb[2][:].bitcast(BF16)[:, 0:D], x1t[:], AF.Square, ["x1t"], [("gb", 2), "ssq2"], accum_out=ssq2[:])
                S.ts(rstd2[:], ssq2[:], 1.0 / D, EPS, ALU.mult, ALU.add, ["ssq2"], ["rstd2"])
                S.act(rstd2[:], rstd2[:], AF.Sqrt, ["rstd2"], ["rstd2"])
                S.op("dve", lambda: nc.vector.reciprocal(out=rstd2[:], in_=rstd2[:]), ["rstd2"], ["rstd2"])
                S.stt(h2[:], x1t[:], rstd2[:, 0:1], gb[0][:], ALU.mult, ALU.mult, ["x1t", "rstd2", ("gb", 0)], ["h2"])
                S.tt(h2[:], h2[:], gb[1][:], ALU.add, ["h2", ("gb", 1)], ["h2"])
                S.cp(h2b[:], h2[:], ["h2"], ["h2b"], eng="act")
                for g8 in range(4):
                    pb = g8 % 2
                    for k8 in range(8):
                        kc = g8 * 8 + k8
                        S.tr(psb[:, pb * 1024 + k8 * 128: pb * 1024 + (k8 + 1) * 128],
                             h2b[:, kc * 128:(kc + 1) * 128], identb[:], ["h2b", "identb"], [("psb", pb)])
                    S.cp(h2T[:, g8 * 8:(g8 + 1) * 8, :], psb[:, pb * 1024:(pb + 1) * 1024].rearrange("p (a t) -> p a t", a=8),
                         [("psb", pb)], [("h2T", g8)], eng=("dve" if g8 % 2 == 0 else "act"))
                h2Tk = [("h2T", g8) for g8 in range(4)]
                for f in range(16):
                    i2 = f % 2
                    S.dma(wq_t[i2][:], wqb[f], [("wqb", f)], [("wq_t", i2)])
                    pq = ps[:, i2 * 512:i2 * 512 + 128]
                    for kc in range(32):
                        S.mm(pq, wq_t[i2][:, kc, :], h2T[:, kc, :], kc == 0, kc == 31, [("wq_t", i2)] + h2Tk, [("ps", i2)])
                    S.cp(qTp[:, f, :], pq, [("ps", i2)], [("qTp", f)], eng=("dve" if f % 2 == 0 else "act"))
                for hp in range(16):
                    bk = 2 + hp // 4
                    S.mm(ps[:, bk * 512 + (hp % 4) * 128: bk * 512 + (hp % 4 + 1) * 128], qTp[:, hp, :], keys_t[:, hp, :],
                         True, True, [("qTp", hp), "keys_t"], [("ps", bk)])
                S.cp(scs[:].rearrange("p a n -> p (a n)"), ps[:, 1024:3072], [("ps", 2), ("ps", 3), ("ps", 4), ("ps", 5)], ["scs"])
                for hp in range(16):
                    top16(scs[:, hp, :], 128, stop_[:, hp, :], itop[:, hp, :], ["scs"])
                S.cp(itf[:], itop[:], ["ti"], ["itf"])
                st4 = stop_[:].rearrange("p (h two) k -> p h two k", two=2)
                it4 = itf[:].rearrange("p (h two) k -> p h two k", two=2)
                c4 = cand[:].rearrange("p h (i j) -> p h i j", i=16)
                x4 = cidx[:].rearrange("p h (i j) -> p h i j", i=16)
                S.tt(c4, st4[:, :, 0, :].unsqueeze(3).to_broadcast([128, 8, 16, 16]),
                     st4[:, :, 1, :].unsqueeze(2).to_broadcast([128, 8, 16, 16]), ALU.add, ["tv"], ["cand"])
                S.ts(i0s[:], it4[:, :, 0, :], 128.0, None, ALU.mult, None, ["itf"], ["i0s"])
                S.tt(x4, i0s[:].unsqueeze(3).to_broadcast([128, 8, 16, 16]),
                     it4[:, :, 1, :].unsqueeze(2).to_broadcast([128, 8, 16, 16]), ALU.add, ["i0s", "itf"], ["cidx"])
                oh = gb[2]
                oh3 = oh[:].rearrange("p (k n) -> p k n", k=16)
                for h in range(8):
                    top16(cand[:, h, :], 256, bs[:, h, :], posu[:, h, :], ["cand"])
                    S.cp(posf[:, h, :], posu[:, h, :], ["ti"], [("posf", h)])
                    S.tt(oh3, iot[:].unsqueeze(1).to_broadcast([128, 16, 256]),
                         posf[:, h, :].unsqueeze(2).to_broadcast([128, 16, 256]), ALU.is_equal,
                         ["iot", ("posf", h)], [("gb", 2)])
                    S.tt(oh3, oh3, cidx[:, h, :].unsqueeze(1).to_broadcast([128, 16, 256]), ALU.mult,
                         [("gb", 2), "cidx"], [("gb", 2)])
                    S.op("dve", lambda h=h: V.tensor_reduce(out=eidx[:, h * 16:(h + 1) * 16], in_=oh3, axis=AX.X, op=ALU.add),
                         [("gb", 2)], [("eidx", h)])
                bsk = ["tv"]
                g3 = gates[:].rearrange("p (h k) -> p h k", h=8)
                S.tt(g3, bs[:], bs[:, :, 0:1].to_broadcast([128, 8, 16]), ALU.subtract, bsk, ["gates"])
                S.act(gates[:], gates[:], AF.Exp, ["gates"], ["gates"])
                S.op("dve", lambda: V.tensor_reduce(out=gsum[:], in_=g3, axis=AX.X, op=ALU.add), ["gates"], ["gsum"])
                S.op("dve", lambda: V.reciprocal(out=gsum[:], in_=gsum[:]), ["gsum"], ["gsum"])
                S.tt(g3, g3, gsum[:].unsqueeze(2).to_broadcast([128, 8, 16]), ALU.mult, ["gates", "gsum"], ["gates"])
                eik = [("eidx", h) for h in range(8)]
                S.ts(eidx[:], eidx[:], 0.0, 16383.0, ALU.max, ALU.min, eik, eik)
                S.cp(eidi[:], eidx[:], eik, ["eidi"])
                G = nc.gpsimd
                for e in range(128):
                    bi_ = gcount % 3
                    gcount += 1
                    S.op("pool", lambda bi_=bi_, e=e: G.indirect_dma_start(
                        out=gb[bi_][:], out_offset=None, in_=peer_u[:, :],
                        in_offset=bass.IndirectOffsetOnAxis(ap=eidi[:, e:e + 1], axis=0)),
                        ["eidi"], [("gb", bi_)], dma=True)
                    S.op("dve", lambda bi_=bi_, e=e: V.scalar_tensor_tensor(
                        out=h2b[:], in0=gb[bi_][:], scalar=1.0, in1=h2[:], op0=ALU.mult, op1=ALU.mult,
                        accum_out=dots[:, e:e + 1]), [("gb", bi_), "h2"], ["h2b", ("dots", e)])
                dk = [("dots", e) for e in range(128)]
                S.act(coef[:], dots[:], AF.Gelu, dk, ["coef"])
                S.tt(coef[:], coef[:], gates[:], ALU.mult, ["coef", "gates"], ["coef"])
                for e in range(128):
                    bi_ = gcount % 3
                    gcount += 1
                    S.op("pool", lambda bi_=bi_, e=e: G.indirect_dma_start(
                        out=gb[bi_][:], out_offset=None, in_=peer_v[:, :],
                        in_offset=bass.IndirectOffsetOnAxis(ap=eidi[:, e:e + 1], axis=0)),
                        ["eidi"], [("gb", bi_)], dma=True)
                    if e == 0:
                        S.ts(accp[:], gb[bi_][:], coef[:, 0:1], None, ALU.mult, None, [("gb", bi_), "coef"], ["accp"])
                    else:
                        S.stt(accp[:], gb[bi_][:], coef[:, e:e + 1], accp[:], ALU.mult, ALU.add,
                              [("gb", bi_), "coef", "accp"], ["accp"])
                bi_ = gcount % 3
                gcount += 1
                S.dma(gb[bi_][:], modd[sidx, 3].partition_broadcast(128), [("modd", sidx, 3)], [("gb", bi_)])
                S.tt(accp[:], accp[:], gb[bi_][:], ALU.mult, ["accp", ("gb", bi_)], ["accp"])
                S.tt(accp[:], accp[:], x1t[:], ALU.add, ["accp", "x1t"], ["accp"], eng="pool")
                S.dma(y_ap[rloc:rloc + 128, :], accp[:], ["accp"], [("y", ti)])
                S.flush(barrier=False)
            S.flush()
    return nc


def _rope_tables(T):
    pos = np.arange(T)
    rows = (pos // 64).astype(np.float32)
    cols = (pos % 64).astype(np.float32)
    inv = (10000.0 ** (-np.arange(0, 64, 2, dtype=np.float32) / 64.0)).astype(np.float32)
    ar = rows[None, :] * inv[:, None]
    ac = cols[None, :] * inv[:, None]
    C = np.concatenate([np.cos(ar), np.cos(ar), np.cos(ac), np.cos(ac)], axis=0).astype(np.float32)
    Sn = np.concatenate([np.sin(ar), np.sin(ar), np.sin(ac), np.sin(ac)], axis=0).astype(np.float32)
    return np.ascontiguousarray(C), np.ascontiguousarray(Sn)


def _consts():
    ident = np.eye(128, dtype=np.float32)
    R = np.zeros((128, 128), np.float32)
    for base in (0, 64):
        for i in range(32):
            R[base + i, base + 32 + i] = -1.0
            R[base + 32 + i, base + i] = 1.0
    rotT = np.ascontiguousarray(R.T)
    ones = np.ones((128, 128), np.float32)
    return np.ascontiguousarray(np.stack([ident, rotT, ones], axis=1))


def make_in_maps(inp):
    f = lambda a: np.ascontiguousarray(np.asarray(a, dtype=np.float32))
    C, Sn = _rope_tables(TC)
    cst = _consts()
    iota = np.ascontiguousarray(np.tile(np.arange(256, dtype=np.float32)[None, :], (128, 1)))
    fm = lambda v, nch: np.ascontiguousarray(f(v).reshape(nch, 128).T)
    shared = {
        "w_ada": f(inp["w_ada"][0]), "badaT": fm(inp["b_ada"][0], 192),
        "g1T": fm(inp["g_norm1"][0], 32), "g2T": fm(inp["g_norm2"][0], 32),
        "w_in": f(inp["w_in"][0]),
        "qkg": np.ascontiguousarray(np.stack([f(inp["q_gain"][0]), f(inp["k_gain"][0])], axis=1)),
        "convwT": np.ascontiguousarray(f(inp["conv_w"][0]).reshape(4, 16, 128).transpose(2, 1, 0)),
        "convbT": fm(inp["conv_b"][0], 16),
        "lamT": np.ascontiguousarray(f(inp["lru_lam"][0]).reshape(32, 128).T),
        "baT": np.ascontiguousarray(f(inp["lru_ba"][0]).reshape(32, 128).T),
        "biT": np.ascontiguousarray(f(inp["lru_bi"][0]).reshape(32, 128).T),
        "lru_wa": f(inp["lru_wa"][0]).reshape(32, 128, 128),
        "lru_wi": f(inp["lru_wi"][0]).reshape(32, 128, 128),
        "w_attn_o": f(inp["w_attn_o"][0]), "w_lru_o": f(inp["w_lru_o"][0]), "w_out": f(inp["w_out"][0]),
        "peer_wq": f(inp["peer_wq"][0]),
        "keysT": np.ascontiguousarray(f(inp["peer_keys"][0]).reshape(16, 128, 128).transpose(2, 0, 1)),
        "peer_u": f(inp["peer_u"][0]), "peer_v": f(inp["peer_v"][0]),
        "ropeC": C, "ropeS": Sn, "consts": cst, "iota256": iota,
    }
    xp = f(inp["x_prompt"])
    xsm = f(inp["x_sample"])
    cp = f(inp["c_prompt"])
    csm = f(inp["c_sample"])
    maps = []
    for c in range(8):
        b, j = c // 4, c % 4
        cc = np.stack([csm[c], cp[b]], axis=0)
        cTl = np.ascontiguousarray(cc.reshape(2, 32, 128).transpose(2, 1, 0))
        ms = np.zeros((128, 4), np.float32)
        ms[:, j] = 1.0
        m = dict(shared)
        m.update({
            "xs": xsm[c], "xo": np.ascontiguousarray(xp[b, j * TS:(j + 1) * TS]), "xc": xp[b],
            "cT": cTl, "msel": ms,
            "ropeCo": np.ascontiguousarray(C[:, j * TS:(j + 1) * TS]),
            "ropeSo": np.ascontiguousarray(Sn[:, j * TS:(j + 1) * TS]),
        })
        maps.append(m)
    return maps


def kernel(**inputs):
    nc = build()
    maps = make_in_maps(inputs)
    res = run_bass_kernel_spmd(nc, maps, core_ids=list(range(8)))
    y_prompt = np.zeros((2, 8192, D), np.float32)
    y_sample = np.zeros((8, TS, D), np.float32)
    for c in range(8):
        b, j = c // 4, c % 4
        y_sample[c] = res.results[c]["ys"]
        y_prompt[b, j * TS:(j + 1) * TS] = res.results[c]["yo"]
    return (y_prompt, y_sample)
```

```python
import contextlib
import numpy as np
import concourse.bass as bass
import concourse.mybir as mybir
from concourse.bass_utils import run_bass_kernel_spmd

F32 = mybir.dt.float32
BF16 = mybir.dt.bfloat16
U32 = mybir.dt.uint32
I32 = mybir.dt.int32
AF = mybir.ActivationFunctionType
ALU = mybir.AluOpType
AX = mybir.AxisListType

D = 4096
TS = 2048
TC = 8192
NCOL = 15360
EPS = 1e-6
SEM_LIMIT = 30000
DBG = {}


class Op:
    __slots__ = ("eng", "fn", "deps", "dma", "signal", "sem", "val")

    def __init__(self, eng, fn, dma):
        self.eng = eng
        self.fn = fn
        self.dma = dma
        self.deps = ()
        self.signal = False
        self.sem = None
        self.val = 0


class Sched:
    def __init__(self, nc, es):
        self.nc = nc
        self.es = es
        self.engs = {"pe": nc.tensor, "act": nc.scalar, "dve": nc.vector, "pool": nc.gpsimd, "sp": nc.sync}
        self.ops = []
        self.last_w = {}
        self.readers = {}
        self.nsem = 0
        self.cur_sem = {}
        self.cnt = {}
        for e in ("pe", "act", "dve", "pool"):
            self.cur_sem[e] = self._new_sem()
            self.cnt[e] = 0
        self.slots = {"sp": [[self._new_sem(), 0] for _ in range(24)],
                      "pool": [[self._new_sem(), 0] for _ in range(16)]}
        self.slot_i = {"sp": 0, "pool": 0}
        self.seen = {e: {} for e in self.engs}
        self.last_op = {}
        self.trace = {e: [] for e in self.engs} if DBG.get("trace") else None

    def _new_sem(self):
        self.nsem += 1
        return self.es.enter_context(self.nc.semaphore(f"sm{self.nsem}"))

    def op(self, eng, fn, r=(), w=(), dma=False):
        w = list(w) + [k for k in r if isinstance(k, tuple) and k[0] in ("ps", "psb")]
        o = Op(eng, fn, dma)
        deps = {}
        for k in r:
            lw = self.last_w.get(k)
            if lw is not None:
                deps[id(lw)] = lw
        for k in w:
            lw = self.last_w.get(k)
            if lw is not None:
                deps[id(lw)] = lw
            rd = self.readers.get(k)
            if rd:
                for x in rd.values():
                    deps[id(x)] = x
        rk = ("d", id(o)) if dma else eng
        for k in r:
            self.readers.setdefault(k, {})[rk] = o
        for k in w:
            self.last_w[k] = o
            self.readers[k] = {}
        dl = []
        for d in deps.values():
            if eng == "pe" and not dma and d.eng == "pe" and not d.dma:
                continue
            d.signal = True
            dl.append(d)
        o.deps = dl
        self.ops.append(o)
        return o

    def _wait(self, E, sem, val):
        sn = self.seen[E]
        if sn.get(id(sem), 0) < val:
            self.engs[E].wait_ge(sem, val)
            sn[id(sem)] = val
            if self.trace is not None:
                self.trace[E].append(("w", id(sem), val))

    def flush(self, barrier=True):
        for lw in self.last_w.values():
            lw.signal = True
        for rd in self.readers.values():
            for x in rd.values():
                x.signal = True
        lastc = {}
        for o in self.ops:
            if not o.dma:
                lastc[o.eng] = o
        for o in lastc.values():
            o.signal = True
        for o in self.ops:
            E = o.eng
            eng = self.engs[E]
            for d in o.deps:
                self._wait(E, d.sem, d.val)
            if o.dma:
                sl = self.slots[E]
                i = self.slot_i[E]
                self.slot_i[E] = (i + 1) % len(sl)
                sem, uses = sl[i]
                if uses > 0:
                    self._wait(E, sem, 16 * uses)
                if 16 * (uses + 1) > SEM_LIMIT:
                    sem = self._new_sem()
                    uses = 0
                    sl[i][0] = sem
                ins = o.fn()
                ins.then_inc(sem, 16)
                if self.trace is not None:
                    self.trace[E].append(("s", id(sem), 16))
                uses += 1
                sl[i][1] = uses
                o.sem = sem
                o.val = 16 * uses
            else:
                ins = o.fn()
                if o.signal:
                    if self.cnt[E] >= SEM_LIMIT:
                        self.cur_sem[E] = self._new_sem()
                        self.cnt[E] = 0
                    self.cnt[E] += 1
                    ins.then_inc(self.cur_sem[E], 1)
                    if self.trace is not None:
                        self.trace[E].append(("s", id(self.cur_sem[E]), 1))
                    o.sem = self.cur_sem[E]
                    o.val = self.cnt[E]
            o.fn = None
        self.ops = []
        if barrier:
            for E in self.engs:
                for E2 in ("pe", "act", "dve", "pool"):
                    if E2 != E and self.cnt[E2] > 0:
                        self._wait(E, self.cur_sem[E2], self.cnt[E2])
                for q in ("sp", "pool"):
                    for sem, uses in self.slots[q]:
                        if uses > 0:
                            self._wait(E, sem, 16 * uses)

    def dma(self, out, in_, r, w, q="sp"):
        eng = self.engs[q]
        return self.op(q, lambda: eng.dma_start(out=out, in_=in_), r, w, dma=True)

    def mm(self, out, lhsT, rhs, start, stop, r, w):
        t = self.nc.tensor
        return self.op("pe", lambda: t.matmul(out, lhsT=lhsT, rhs=rhs, start=start, stop=stop), r, w)

    def tr(self, out, in_, ident, r, w):
        t = self.nc.tensor
        return self.op("pe", lambda: t.transpose(out=out, in_=in_, identity=ident), r, w)

    def act(self, out, in_, func, r, w, bias=None, scale=None, accum_out=None):
        a = self.nc.scalar
        kw = {}
        if bias is not None:
            kw["bias"] = bias
        if scale is not None:
            kw["scale"] = scale
        if accum_out is not None:
            kw["accum_out"] = accum_out
        return self.op("act", lambda: a.activation(out=out, in_=in_, func=func, **kw), r, w)

    def ts(self, out, in0, s1, s2, op0, op1, r, w, eng="dve"):
        e = self.engs[eng]
        if op1 is None:
            return self.op(eng, lambda: e.tensor_scalar(out=out, in0=in0, scalar1=s1, scalar2=None, op0=op0), r, w)
        return self.op(eng, lambda: e.tensor_scalar(out=out, in0=in0, scalar1=s1, scalar2=s2, op0=op0, op1=op1), r, w)

    def tt(self, out, in0, in1, op, r, w, eng="dve"):
        e = self.engs[eng]
        return self.op(eng, lambda: e.tensor_tensor(out=out, in0=in0, in1=in1, op=op), r, w)

    def stt(self, out, in0, scalar, in1, op0, op1, r, w):
        e = self.nc.vector
        return self.op("dve", lambda: e.scalar_tensor_tensor(out=out, in0=in0, scalar=scalar, in1=in1, op0=op0, op1=op1), r, w)

    def cp(self, out, in_, r, w, eng="dve"):
        e = self.engs[eng]
        if eng == "act":
            return self.op(eng, lambda: e.copy(out=out, in_=in_), r, w)
        return self.op(eng, lambda: e.tensor_copy(out=out, in_=in_), r, w)


def build(debug=False, dbg_names=(), stop=None):
    nc = bass.Bass("TRN2", target_bir_lowering=False)

    def din(name, shape, dt=F32):
        return nc.dram_tensor(name, list(shape), dt, kind="ExternalInput").ap()

    def dscr(name, shape, dt=F32):
        kind = "ExternalOutput" if name in dbg_names else "Internal"
        return nc.dram_tensor(name, list(shape), dt, kind=kind).ap()

    xs = din("xs", [TS, D])
    xo = din("xo", [TS, D])
    xc = din("xc", [TC, D])
    cT = din("cT", [128, 32, 2])
    w_ada = din("w_ada", [D, 6 * D])
    badaT = din("badaT", [128, 192])
    g1T = din("g1T", [128, 32])
    g2T = din("g2T", [128, 32])
    w_in = din("w_in", [D, NCOL])
    qkg = din("qkg", [128, 2])
    convwT = din("convwT", [128, 16, 4])
    convbT = din("convbT", [128, 16])
    lamT = din("lamT", [128, 32])
    baT = din("baT", [128, 32])
    biT = din("biT", [128, 32])
    lru_wa = din("lru_wa", [32, 128, 128])
    lru_wi = din("lru_wi", [32, 128, 128])
    w_attn_o = din("w_attn_o", [2048, D])
    w_lru_o = din("w_lru_o", [2048, D])
    w_out = din("w_out", [D, D])
    peer_wq = din("peer_wq", [D, 2048])
    keysT = din("keysT", [128, 16, 128])
    peer_u = din("peer_u", [16384, D])
    peer_v = din("peer_v", [16384, D])
    ropeC = din("ropeC", [128, TC])
    ropeS = din("ropeS", [128, TC])
    ropeCo = din("ropeCo", [128, TS])
    ropeSo = din("ropeSo", [128, TS])
    consts = din("consts", [128, 3, 128])
    iota_in = din("iota256", [128, 256])
    msel = din("msel", [128, 4])

    ys = nc.dram_tensor("ys", [TS, D], F32, kind="ExternalOutput").ap()
    yo = nc.dram_tensor("yo", [TS, D], F32, kind="ExternalOutput").ap()

    winb = dscr("winb", [120, 128, 32, 128], BF16)
    waob = dscr("waob", [32, 128, 16, 128], BF16)
    wlob = dscr("wlob", [32, 128, 16, 128], BF16)
    woutb = dscr("woutb", [8, 128, 32, 512], BF16)
    wqb = dscr("wqb", [16, 128, 32, 128], BF16)
    modd = dscr("modd", [2, 4, D])
    QT = dscr("QT", [2, 16, 128, TS], BF16)
    KT_s = dscr("KT_s", [4, 128, TS], BF16)
    KT_c = dscr("KT_c", [4, 128, TC], BF16)
    V_s = dscr("V_s", [TS, 512], BF16)
    V_c = dscr("V_c", [TC, 512], BF16)
    xl_s = dscr("xl_s", [16, 128, TS])
    xl_c = dscr("xl_c", [16, 128, TC])
    gy = dscr("gy", [2, 16, 128, TS])
    ga = dscr("ga", [2, 32, 128, TS])
    gl = dscr("gl", [2, 32, 128, TS])
    attnT = dscr("attnT", [2, 16, 128, TS], BF16)
    lruT = dscr("lruT", [2, 16, 128, TS], BF16)
    x1 = dscr("x1", [2 * TS, D])
    ub16 = dscr("ub16", [16384, D], BF16)
    vb16 = dscr("vb16", [16384, D], BF16)
    dbg = {}
    if debug:
        dbg["modT"] = nc.dram_tensor("dbg_modT", [128, 192, 2], F32, kind="ExternalOutput").ap()

    es = contextlib.ExitStack()
    with es:
        S = Sched(nc, es)
        nc._sched = S

        nm = [0]

        def sb(st, name, shape, dt=F32):
            nm[0] += 1
            return st.enter_context(nc.sbuf_tensor(f"{name}_{nm[0]}", list(shape), dt))

        ps = es.enter_context(nc.psum_tensor("ps", [128, 3072], F32))
        psb = es.enter_context(nc.psum_tensor("psb", [128, 2048], BF16))
        cst = sb(es, "cst", [128, 3, 128])
        identb = sb(es, "identb", [128, 128], BF16)
        onesb = sb(es, "onesb", [128, 128], BF16)
        modT = sb(es, "modT", [128, 192, 2])
        G1 = sb(es, "G1", [128, 32, 2])
        G2 = sb(es, "G2", [128, 32, 2])
        qkg_t = sb(es, "qkg_t", [128, 2])
        msel_t = sb(es, "msel_t", [128, 4])
        identf = cst[:, 0, :]
        rotT = cst[:, 1, :]
        onesf = cst[:, 2, :]

        def bank(i):
            return ps[:, i * 512:(i + 1) * 512]

        S.dma(cst[:], consts[:, :, :], [], ["cst"])
        S.dma(qkg_t[:], qkg[:, :], [], ["qkg"])
        S.dma(msel_t[:], msel[:, :], [], ["msel"])
        S.cp(identb[:], cst[:, 0, :], ["cst"], ["identb"])
        S.cp(onesb[:], cst[:, 2, :], ["cst"], ["onesb"])

        def cast_w(dst, src, nf, kc, cw):
            for f in range(nf):
                S.dma(dst[f], src[:, f * cw:(f + 1) * cw].rearrange("(kc p) c -> p kc c", p=128),
                      [], [(dst.tensor.name, f)], q="pool")
        cast_w(winb, w_in, 120, 32, 128)
        cast_w(waob, w_attn_o, 32, 16, 128)
        cast_w(wlob, w_lru_o, 32, 16, 128)
        cast_w(woutb, w_out, 8, 32, 512)
        cast_w(wqb, peer_wq, 16, 32, 128)

        with contextlib.ExitStack() as st:
            cT_t = sb(st, "cT_t", [128, 32, 2])
            scT = sb(st, "scT", [128, 32, 2])
            bada_t = sb(st, "bada_t", [128, 192])
            g1_t = sb(st, "g1_t", [128, 32])
            g2_t = sb(st, "g2_t", [128, 32])
            wa = [sb(st, f"wa{i}", [128, 32, 256]) for i in range(2)]
            tmpc = sb(st, "tmpc", [128, 32])
            tmpr = sb(st, "tmpr", [32, 128])
            S.dma(cT_t[:], cT[:, :, :], [], ["cT"])
            S.dma(bada_t[:], badaT[:, :], [], ["bada"])
            S.dma(g1_t[:], g1T[:, :], [], ["g1"])
            S.dma(g2_t[:], g2T[:, :], [], ["g2"])
            S.act(scT[:], cT_t[:], AF.Silu, ["cT"], ["scT"])
            for j2 in range(96):
                buf = wa[j2 % 2]
                S.dma(buf[:], w_ada[:, j2 * 256:(j2 + 1) * 256].rearrange("(kc p) c -> p kc c", p=128),
                      [], [("wa", j2 % 2)])
                for half in range(2):
                    j = j2 * 2 + half
                    b = j % 6
                    pst = ps[:, b * 512:b * 512 + 2]
                    for kc in range(32):
                        S.mm(pst, buf[:, kc, half * 128:(half + 1) * 128], scT[:, kc, :], kc == 0, kc == 31,
                             [("wa", j2 % 2), "scT"], [("ps", b)])
                    S.ts(modT[:, j, :], pst, bada_t[:, j:j + 1], None, ALU.add, None,
                         [("ps", b), "bada"], [("modT", j)])
            allmod = [("modT", j) for j in range(192)]
            S.ts(G1[:], modT[:, 32:64, :], 1.0, None, ALU.add, None, allmod, ["G1"])
            S.tt(G1[:], G1[:], g1_t[:].unsqueeze(2).to_broadcast([128, 32, 2]), ALU.mult, ["G1", "g1"], ["G1"])
            S.ts(G2[:], modT[:, 128:160, :], 1.0, None, ALU.add, None, allmod, ["G2"])
            S.tt(G2[:], G2[:], g2_t[:].unsqueeze(2).to_broadcast([128, 32, 2]), ALU.mult, ["G2", "g2"], ["G2"])
            srcs = [(modT, 64), (G2, 0), (modT, 96), (modT, 160)]
            for m in range(2):
                for sl, (srct, off) in enumerate(srcs):
                    S.cp(tmpc[:], srct[:, off:off + 32, m], allmod + ["G2"], ["tmpc"])
                    S.tr(ps[0:32, 0:128], tmpc[:], identf, ["tmpc", "cst"], [("ps", 0)])
                    S.cp(tmpr[:], ps[0:32, 0:128], [("ps", 0)], ["tmpr"])
                    S.dma(modd[m, sl].rearrange("(kc p) -> kc p", p=128), tmpr[:], ["tmpr"], [("modd", m, sl)])
            if debug:
                S.dma(dbg["modT"][:, :, :], modT[:], allmod, ["dbg_modT"])
            S.flush()

        def phase_c(tag, x_ap, T, m, cols, rc, rs, oq, ok, ov, oxl, sidx):
            with contextlib.ExitStack() as st:
                NXB = DBG.get("nxb", 2)
                xt = [sb(st, f"xt{i}", [128, D]) for i in range(NXB)]
                NXN = DBG.get("nxn", NXB)
                xn = [sb(st, f"xn{i}", [128, D], BF16) for i in range(NXN)]
                hT = [sb(st, f"hT{i}", [128, 32, 512], BF16) for i in range(DBG.get("nhT", 2))]
                wt = [sb(st, f"wt{i}", [128, 32, 128], BF16) for i in range(3)]
                ssq = sb(st, "ssq", [128, 2])
                rstd = sb(st, "rstd", [128, 2])
                sqjunk = sb(st, "sqjunk", [128, D], BF16)
                ct = [sb(st, f"ct{i}", [128, 512]) for i in range(2)]
                stt_ = [sb(st, f"st{i}", [128, 512]) for i in range(2)]
                sqf = sb(st, "sqf", [128, 512])
                rsf = sb(st, "rsf", [128, 512])
                qn = sb(st, "qn", [128, 512])
                t1 = sb(st, "t1", [128, 512])
                t2 = sb(st, "t2", [128, 512])
                ob = [sb(st, f"ob{i}", [128, 512], BF16) for i in range(2)]
                of = [sb(st, f"of{i}", [128, 512]) for i in range(2)]
                vT = sb(st, "vT", [128, 512], BF16)
                vtile = [sb(st, f"vtile{i}", [128, 4, 128], BF16) for i in range(2)]
                nblk = DBG.get("nblk", T // 512)
                if "kinds" in DBG:
                    cols = [c_ for c_ in cols if c_[1] in DBG["kinds"]][:DBG.get("ncols", 1000)]
                has_rope = any(kind in ("q", "k") for (_, kind, _) in cols)
                wcount = 0
                pcount = 0
                ocount = 0
                for b in range(nblk):
                    hb = hT[b % len(hT)]
                    hk = ("hT", b % len(hT))
                    for tt_ in range(DBG.get("ntile", 4)):
                        ti = b * 4 + tt_
                        xb = xt[ti % NXB]
                        xk = ("xt", ti % NXB)
                        nb_ = xn[ti % NXN]
                        nk = ("xn", ti % NXN)
                        S.dma(xb[:], x_ap[ti * 128:(ti + 1) * 128, :], [], [xk])
                        S.act(sqjunk[:], xb[:], AF.Square, [xk], ["sqjunk", ("ssq", ti % 2)], accum_out=ssq[:, ti % 2:ti % 2 + 1])
                        S.ts(rstd[:, ti % 2:ti % 2 + 1], ssq[:, ti % 2:ti % 2 + 1], 1.0 / D, EPS, ALU.mult, ALU.add,
                             [("ssq", ti % 2)], [("rstd", ti % 2)])
                        S.act(rstd[:, ti % 2:ti % 2 + 1], rstd[:, ti % 2:ti % 2 + 1], AF.Sqrt,
                              [("rstd", ti % 2)], [("rstd", ti % 2)])
                        S.op("dve", lambda ti=ti: nc.vector.reciprocal(out=rstd[:, ti % 2:ti % 2 + 1], in_=rstd[:, ti % 2:ti % 2 + 1]),
                             [("rstd", ti % 2)], [("rstd", ti % 2)])
                        S.act(nb_[:], xb[:], AF.Identity, [xk, ("rstd", ti % 2)], [nk], scale=rstd[:, ti % 2:ti % 2 + 1])
                        fes = DBG.get("fe", 9)
                        if tt_ >= DBG.get("fe_tiles", 4):
                            fes = 1
                        for g8 in range(DBG.get("ng8", 4) if fes >= 2 else 0):
                            pb = g8 % 2
                            for k8 in range(8):
                                kc = g8 * 8 + k8
                                S.tr(psb[:, pb * 1024 + k8 * 128: pb * 1024 + (k8 + 1) * 128],
                                     nb_[:, kc * 128:(kc + 1) * 128], identb[:], [nk, "identb"], [("psb", pb)])
                            for k8 in range(8 if fes >= 3 else 0):
                                kc = g8 * 8 + k8
                                src = psb[:, pb * 1024 + k8 * 128: pb * 1024 + (k8 + 1) * 128]
                                dst = hb[:, kc, tt_ * 128:(tt_ + 1) * 128]
                                if (pb == 0 and fes != 4) or fes == 3:
                                    S.ts(dst, src, G1[:, kc, m:m + 1], modT[:, kc, m:m + 1], ALU.mult, ALU.add,
                                         [("psb", pb), "G1"], [(hk, kc, tt_)])
                                else:
                                    S.act(dst, src, AF.Identity, [("psb", pb), "G1"], [(hk, kc, tt_)],
                                          bias=modT[:, kc, m:m + 1], scale=G1[:, kc, m:m + 1])
                    hkeys = [(hk, kc, t4) for kc in range(32) for t4 in range(4)]
                    if has_rope:
                        cb = ct[b % 2]
                        sbb = stt_[b % 2]
                        S.dma(cb[:], rc[:, b * 512:(b + 1) * 512], [], [("ct", b % 2)])
                        S.dma(sbb[:], rs[:, b * 512:(b + 1) * 512], [], [("st", b % 2)])
                    tsl = slice(b * 512, (b + 1) * 512)
                    for (f, kind, idx) in cols:
                        wb = wt[wcount % 3]
                        wk = ("wt", wcount % 3)
                        wcount += 1
                        S.dma(wb[:], winb[f], [("winb", f)], [wk])
                        pbk = pcount % 4
                        pcount += 1
                        pq = bank(pbk)
                        pk = ("ps", pbk)
                        for kc in range(32):
                            S.mm(pq, wb[:, kc, :], hb[:, kc, :], kc == 0, kc == 31,
                                 [wk] + [(hk, kc, t4) for t4 in range(4)], [pk])
                        if kind in ("q", "k"):
                            S.act(sqf[:], pq, AF.Square, [pk], ["sqf"])
                            S.mm(bank(4), onesf, sqf[:], True, True, ["sqf", "cst"], [("ps", 4)])
                            S.ts(rsf[:], bank(4), 1.0 / 128, EPS, ALU.mult, ALU.add, [("ps", 4)], ["rsf"])
                            S.act(rsf[:], rsf[:], AF.Sqrt, ["rsf"], ["rsf"])
                            S.op("dve", lambda: nc.vector.reciprocal(out=rsf[:], in_=rsf[:]), ["rsf"], ["rsf"])
                            gcol = 0 if kind == "q" else 1
                            S.stt(qn[:], pq, qkg_t[:, gcol:gcol + 1], rsf[:], ALU.mult, ALU.mult,
                                  [pk, "rsf", "qkg"], ["qn"])
                            S.mm(bank(5), rotT, qn[:], True, True, ["qn", "cst"], [("ps", 5)])
                            S.tt(t1[:], qn[:], cb[:], ALU.mult, ["qn", ("ct", b % 2)], ["t1"])
                            S.tt(t2[:], bank(5), sbb[:], ALU.mult, [("ps", 5), ("st", b % 2)], ["t2"])
                            o = ob[ocount % 2]
                            okk = ("ob", ocount % 2)
                            ocount += 1
                            S.tt(o[:], t1[:], t2[:], ALU.add, ["t1", "t2"], [okk])
                            dst = oq[idx][:, tsl] if kind == "q" else ok[idx][:, tsl]
                            S.dma(dst, o[:], [okk], [(tag, kind, idx, b)])
                        elif kind == "v":
                            S.cp(vT[:], pq, [pk], ["vT"], eng="act")
                            vt_ = vtile[idx % 2]
                            for t4 in range(4):
                                S.tr(psb[:, t4 * 128:(t4 + 1) * 128], vT[:, t4 * 128:(t4 + 1) * 128], identb[:],
                                     ["vT", "identb"], [("psb", 0)])
                            S.cp(vt_[:], psb[:, 0:512].rearrange("p (a d) -> p a d", a=4),
                                 [("psb", 0)], [("vtile", idx % 2)])
                            S.dma(ov[b * 512:(b + 1) * 512, idx * 128:(idx + 1) * 128].rearrange("(a p) d -> p a d", p=128),
                                  vt_[:], [("vtile", idx % 2)], [(tag, "v", idx, b)])
                        else:
                            o = of[ocount % 2]
                            okk = ("of", ocount % 2)
                            ocount += 1
                            if kind == "xl":
                                S.cp(o[:], pq, [pk], [okk], eng="act")
                                dst = oxl[idx][:, tsl]
                            elif kind == "yl":
                                S.act(o[:], pq, AF.Gelu, [pk], [okk])
                                dst = gy[sidx, idx][:, tsl]
                            elif kind == "ga":
                                S.act(o[:], pq, AF.Sigmoid, [pk], [okk])
                                dst = ga[sidx, idx][:, tsl]
                            else:
                                S.act(o[:], pq, AF.Sigmoid, [pk], [okk])
                                dst = gl[sidx, idx][:, tsl]
                            S.dma(dst, o[:], [okk], [(tag, kind, idx, b)])
                    S.flush(barrier=False)
                S.flush()

        cols_q = [(h, "q", h) for h in range(16)]
        cols_k = [(16 + g, "k", g) for g in range(4)]
        cols_v = [(20 + g, "v", g) for g in range(4)]
        cols_xl = [(24 + n, "xl", n) for n in range(16)]
        cols_yl = [(40 + n, "yl", n) for n in range(16)]
        cols_ga = [(56 + n, "ga", n) for n in range(32)]
        cols_gl = [(88 + n, "gl", n) for n in range(32)]

        if stop == "A":
            return nc
        for i in range(64):
            S.dma(ub16[i * 256:(i + 1) * 256, :], peer_u[i * 256:(i + 1) * 256, :], [], [("ub16", i)], q="pool")
        for i in range(64):
            S.dma(vb16[i * 256:(i + 1) * 256, :], peer_v[i * 256:(i + 1) * 256, :], [], [("vb16", i)], q="pool")
        phase_c("S", xs, TS, 0, cols_q + cols_k + cols_v + cols_xl + cols_yl + cols_ga + cols_gl,
                ropeC, ropeS, QT[0], KT_s, V_s, xl_s, 0)
        if stop == "CS":
            return nc
        phase_c("O", xo, TS, 1, cols_q + cols_yl + cols_ga + cols_gl, ropeCo, ropeSo, QT[1], None, None, None, 1)
        phase_c("C", xc, TC, 1, cols_k + cols_v + cols_xl, ropeC, ropeS, None, KT_c, V_c, xl_c, 1)

        def phase_d(tag, xl_ap, T, sidx, use_sel, tag_xl, gy_tag):
            nq = T // TS
            with contextlib.ExitStack() as st:
                cw = sb(st, "cw", [128, 16, 4])
                cbt = sb(st, "cbt", [128, 16])
                lam = sb(st, "lam", [128, 32])
                cA = sb(st, "cA", [128, 32])
                cA2 = sb(st, "cA2", [128, 32])
                ba_t = sb(st, "ba_t", [128, 32])
                bi_t = sb(st, "bi_t", [128, 32])
                wa_t = sb(st, "wa_t", [128, 32, 128])
                wi_t = sb(st, "wi_t", [128, 32, 128])
                xpad = sb(st, "xpad", [128, TS + 3])
                xcv = sb(st, "xcv", [128, TS])
                rg = sb(st, "rg", [128, TS])
                ig = sb(st, "ig", [128, TS])
                a_t = sb(st, "a_t", [128, TS])
                mu = sb(st, "mu", [128, TS])
                hh = sb(st, "hh", [128, TS])
                acc = sb(st, "acc", [128, TS])
                gyt = sb(st, "gyt", [128, TS])
                lo = sb(st, "lo", [128, TS], BF16)
                stt = sb(st, "stt", [128, 2])
                S.dma(cw[:], convwT[:, :, :], [], ["cw"])
                S.dma(cbt[:], convbT[:, :], [], ["cbt"])
                S.dma(lam[:], lamT[:, :], [], ["lam"])
                S.dma(ba_t[:], baT[:, :], [], ["ba"])
                S.dma(bi_t[:], biT[:, :], [], ["bi"])
                S.dma(wa_t[:], lru_wa.rearrange("n c d -> c n d"), [], ["wa_t"])
                S.dma(wi_t[:], lru_wi.rearrange("n c d -> c n d"), [], ["wi_t"])
                S.act(cA[:], lam[:], AF.Exp, ["lam"], ["cA"], scale=-1.0)
                S.act(cA[:], cA[:], AF.Ln, ["cA"], ["cA"], bias=1.0)
                S.ts(cA[:], cA[:], -8.0, None, ALU.mult, None, ["cA"], ["cA"])
                S.ts(cA2[:], cA[:], 2.0, None, ALU.mult, None, ["cA"], ["cA2"])
                for n in range(16):
                    first = True
                    for dr in range(2):
                        gi = dr * 16 + n
                        S.op("pool", lambda dr=dr: nc.gpsimd.memset(stt[:, dr:dr + 1], 0.0), [], [("stt", dr)])
                        qs = range(nq) if dr == 0 else range(nq - 1, -1, -1)
                        for q in qs:
                            lo_t = q * TS - 2
                            hi_t = q * TS + TS + 1
                            a0 = max(lo_t, 0)
                            a1 = min(hi_t, T)
                            if lo_t < 0:
                                S.op("pool", lambda: nc.gpsimd.memset(xpad[:, 0:2], 0.0), [], ["xpad"])
                            if hi_t > T:
                                S.op("pool", lambda: nc.gpsimd.memset(xpad[:, TS + 2:TS + 3], 0.0), [], ["xpad"])
                            S.dma(xpad[:, a0 - lo_t:a1 - lo_t], xl_ap[n][:, a0:a1],
                                  [(tag_xl, "xl", n, bb) for bb in range(T // 512)], ["xpad"])
                            S.ts(xcv[:], xpad[:, 0:TS], cw[:, n, 0:1], cbt[:, n:n + 1], ALU.mult, ALU.add,
                                 ["xpad", "cw", "cbt"], ["xcv"])
                            for j in range(1, 4):
                                S.stt(xcv[:], xpad[:, j:j + TS], cw[:, n, j:j + 1], xcv[:], ALU.mult, ALU.add,
                                      ["xpad", "cw", "xcv"], ["xcv"])
                            for (wt_, bt_, gt_, gk, wkey, bkey) in ((wa_t, ba_t, rg, "rg", "wa_t", "ba"), (wi_t, bi_t, ig, "ig", "wi_t", "bi")):
                                for half in range(2):
                                    pb0 = 0 if gk == "rg" else 2
                                    for blk in range(2):
                                        c0 = half * 1024 + blk * 512
                                        S.mm(bank(pb0 + blk), wt_[:, gi, :], xcv[:, c0:c0 + 512], True, True,
                                             ["xcv", wkey], [("ps", pb0 + blk)])
                                    S.act(gt_[:, half * 1024:(half + 1) * 1024], ps[:, pb0 * 512:(pb0 + 2) * 512],
                                          AF.Sigmoid, [("ps", pb0), ("ps", pb0 + 1), bkey], [gk],
                                          bias=bt_[:, gi:gi + 1])
                            S.act(a_t[:], rg[:], AF.Exp, ["rg", "cA"], ["a_t"], scale=cA[:, gi:gi + 1])
                            S.act(mu[:], rg[:], AF.Exp, ["rg", "cA2"], ["mu"], scale=cA2[:, gi:gi + 1])
                            S.ts(mu[:], mu[:], -1.0, 1.0, ALU.mult, ALU.add, ["mu"], ["mu"])
                            S.ts(mu[:], mu[:], 0.0, None, ALU.max, None, ["mu"], ["mu"])
                            S.act(mu[:], mu[:], AF.Sqrt, ["mu"], ["mu"])
                            S.tt(ig[:], ig[:], xcv[:], ALU.mult, ["ig", "xcv"], ["ig"], eng="pool")
                            S.tt(ig[:], ig[:], mu[:], ALU.mult, ["ig", "mu"], ["ig"], eng="pool")
                            if dr == 0:
                                S.op("dve", lambda: nc.vector.tensor_tensor_scan(
                                    out=hh[:], data0=a_t[:], data1=ig[:], initial=stt[:, 0:1], op0=ALU.mult, op1=ALU.add),
                                    ["a_t", "ig", ("stt", 0)], ["hh"])
                                S.cp(stt[:, 0:1], hh[:, TS - 1:TS], ["hh"], [("stt", 0)])
                            else:
                                S.op("dve", lambda: nc.vector.tensor_tensor_scan(
                                    out=hh[:, ::-1], data0=a_t[:, ::-1], data1=ig[:, ::-1], initial=stt[:, 1:2],
                                    op0=ALU.mult, op1=ALU.add),
                                    ["a_t", "ig", ("stt", 1)], ["hh"])
                                S.cp(stt[:, 1:2], hh[:, 0:1], ["hh"], [("stt", 1)])
                            sel = msel_t[:, q:q + 1] if use_sel else 1.0
                            if first:
                                S.ts(acc[:], hh[:], sel, None, ALU.mult, None, ["hh", "msel"], ["acc"])
                                first = False
                            else:
                                S.stt(acc[:], hh[:], sel, acc[:], ALU.mult, ALU.add, ["hh", "msel", "acc"], ["acc"])
                    S.dma(gyt[:], gy[sidx, n], [(gy_tag, "yl", n, bb) for bb in range(4)], ["gyt"])
                    S.tt(lo[:], acc[:], gyt[:], ALU.mult, ["acc", "gyt"], ["lo"])
                    S.dma(lruT[sidx, n], lo[:], ["lo"], [("lruT", sidx, n)])
                    S.flush(barrier=False)
                S.flush()

        if stop == "C":
            return nc
        phase_d("S", xl_s, TS, 0, False, "S", "S")
        if stop == "DS":
            return nc
        phase_d("C", xl_c, TC, 1, True, "C", "O")

        def phase_e(sidx, qtag, ktag, KT_ap, V_ap, Tk):
            nkt = Tk // 128
            with contextlib.ExitStack() as st:
                kT = sb(st, "kT", [128, Tk], BF16)
                vt = sb(st, "vt", [128, nkt, 128], BF16)
                qT = [sb(st, f"qT{i}", [128, TS], BF16) for i in range(2)]
                pT = [sb(st, f"pT{i}", [128, 512], BF16) for i in range(3)]
                rden = sb(st, "rden", [128, 512])
                ot = [sb(st, f"ot{i}", [128, 512], BF16) for i in range(2)]
                sc = 128.0 ** -0.5
                hcount = 0
                ucount = 0
                for g in range(4):
                    S.dma(kT[:], KT_ap[g], [(ktag, "k", g, bb) for bb in range(Tk // 512)], ["kT"])
                    S.dma(vt[:], V_ap[:, g * 128:(g + 1) * 128].rearrange("(kt p) d -> p kt d", p=128),
                          [(ktag, "v", g, bb) for bb in range(Tk // 512)], ["vt"])
                    for hh_ in range(4):
                        h = g * 4 + hh_
                        qb_ = qT[hcount % 2]
                        qk = ("qT", hcount % 2)
                        hcount += 1
                        S.dma(qb_[:], QT[sidx, h], [(qtag, "q", h, bb) for bb in range(4)], [qk])
                        for qb in range(4):
                            po = bank(2 + ucount % 2)
                            pok = ("ps", 2 + ucount % 2)
                            pd = bank(4 + ucount % 2)
                            pdk = ("ps", 4 + ucount % 2)
                            ucount += 1
                            qsl = qb_[:, qb * 512:(qb + 1) * 512]

                            def qk_mm(kt):
                                S.mm(bank(kt % 2), kT[:, kt * 128:(kt + 1) * 128], qsl, True, True,
                                     ["kT", qk], [("ps", kt % 2)])
                            qk_mm(0)
                            for kt in range(nkt):
                                if kt + 1 < nkt:
                                    qk_mm(kt + 1)
                                p_ = pT[kt % 3]
                                pk_ = ("pT", kt % 3)
                                S.act(p_[:], bank(kt % 2), AF.Exp, [("ps", kt % 2)], [pk_], scale=sc)
                                S.mm(po, vt[:, kt, :], p_[:], kt == 0, kt == nkt - 1, ["vt", pk_], [pok])
                                S.mm(pd, onesb[:], p_[:], kt == 0, kt == nkt - 1, ["onesb", pk_], [pdk])
                            S.op("dve", lambda pd=pd: nc.vector.reciprocal(out=rden[:], in_=pd), [pdk], ["rden"])
                            o_ = ot[ucount % 2]
                            S.tt(o_[:], po, rden[:], ALU.mult, [pok, "rden"], [("ot", ucount % 2)])
                            S.dma(attnT[sidx, h][:, qb * 512:(qb + 1) * 512], o_[:], [("ot", ucount % 2)],
                                  [("attnT", sidx, h, qb)])
                        S.flush(barrier=False)
                S.flush()

        if stop == "D":
            return nc
        phase_e(0, "S", "S", KT_s, V_s, TS)
        if stop == "ES":
            return nc
        phase_e(1, "O", "C", KT_c, V_c, TC)

        if stop == "E":
            return nc
        with contextlib.ExitStack() as st:
            aT = sb(st, "aT", [128, 16, 512], BF16)
            lT = sb(st, "lT", [128, 16, 512], BF16)
            mg = sb(st, "mg", [128, 32, 512], BF16)
            wao_t = [sb(st, f"wao{i}", [128, 16, 128], BF16) for i in range(2)]
            wlo_t = [sb(st, f"wlo{i}", [128, 16, 128], BF16) for i in range(2)]
            ga_t = [sb(st, f"ga_t{i}", [128, 512]) for i in range(2)]
            gl_t = [sb(st, f"gl_t{i}", [128, 512]) for i in range(2)]
            f1 = sb(st, "f1", [128, 512])
            f2 = sb(st, "f2", [128, 512])
            wo_t = sb(st, "wo_t", [128, 32, 512], BF16)
            g1bc = sb(st, "g1bc", [128, D])
            xx = [sb(st, f"xx{i}", [128, 512]) for i in range(2)]
            xo_ = [sb(st, f"xo_{i}", [128, 512]) for i in range(2)]
            cnt = 0
            for sidx in range(2):
                x_ap = xs if sidx == 0 else xo
                S.dma(g1bc[:], modd[sidx, 0].partition_broadcast(128), [("modd", sidx, 0)], ["g1bc"])
                for b in range(4):
                    tsl = slice(b * 512, (b + 1) * 512)
                    S.dma(aT[:], attnT[sidx][:, :, tsl].rearrange("h p t -> p h t"),
                          [("attnT", sidx, h, b) for h in range(16)], ["aT"])
                    S.dma(lT[:], lruT[sidx][:, :, tsl].rearrange("h p t -> p h t"),
                          [("lruT", sidx, n) for n in range(16)], ["lT"])
                    for f in range(32):
                        i2 = f % 2
                        S.dma(wao_t[i2][:], waob[f], [("waob", f)], [("wao", i2)])
                        S.dma(wlo_t[i2][:], wlob[f], [("wlob", f)], [("wlo", i2)])
                        S.dma(ga_t[i2][:], ga[sidx, f][:, tsl], [("S" if sidx == 0 else "O", "ga", f, b)], [("ga_t", i2)])
                        S.dma(gl_t[i2][:], gl[sidx, f][:, tsl], [("S" if sidx == 0 else "O", "gl", f, b)], [("gl_t", i2)])
                        pB = bank(i2 * 2)
                        pA = bank(i2 * 2 + 1)
                        for kc in range(16):
                            S.mm(pB, wao_t[i2][:, kc, :], aT[:, kc, :], kc == 0, kc == 15, [("wao", i2), "aT"], [("ps", i2 * 2)])
                        for kc in range(16):
                            S.mm(pA, wlo_t[i2][:, kc, :], lT[:, kc, :], kc == 0, kc == 15, [("wlo", i2), "lT"], [("ps", i2 * 2 + 1)])
                        S.tt(f1[:], pB, ga_t[i2][:], ALU.mult, [("ps", i2 * 2), ("ga_t", i2)], ["f1"])
                        S.tt(f2[:], pA, gl_t[i2][:], ALU.mult, [("ps", i2 * 2 + 1), ("gl_t", i2)], ["f2"])
                        S.tt(mg[:, f, :], f1[:], f2[:], ALU.add, ["f1", "f2"], [("mg", f)], eng="pool")
                    mgk = [("mg", f) for f in range(32)]
                    for nch in range(8):
                        S.dma(wo_t[:], woutb[nch], [("woutb", nch)], ["wo_t"])
                        csl = slice(nch * 512, (nch + 1) * 512)
                        for t4 in range(4):
                            i2 = cnt % 2
                            cnt += 1
                            r0 = b * 512 + t4 * 128
                            S.dma(xx[i2][:], x_ap[r0:r0 + 128, csl], [], [("xx", i2)])
                            po = bank(4 + i2)
                            for kc in range(32):
                                S.mm(po, mg[:, kc, t4 * 128:(t4 + 1) * 128], wo_t[:, kc, :], kc == 0, kc == 31,
                                     mgk + ["wo_t"], [("ps", 4 + i2)])
                            S.tt(xo_[i2][:], po, g1bc[:, csl], ALU.mult, [("ps", 4 + i2), "g1bc"], [("xo_", i2)])
                            S.tt(xo_[i2][:], xo_[i2][:], xx[i2][:], ALU.add, [("xo_", i2), ("xx", i2)], [("xo_", i2)], eng="pool")
                            S.dma(x1[sidx * TS + r0: sidx * TS + r0 + 128, csl], xo_[i2][:], [("xo_", i2)],
                                  [("x1", sidx * 16 + b * 4 + t4, nch)])
                    S.flush(barrier=False)
            S.flush()

        if stop == "F":
            return nc
        with contextlib.ExitStack() as st:
            x1t = sb(st, "x1t", [128, D])
            h2 = sb(st, "h2", [128, D])
            accp = sb(st, "accp", [128, D])
            gb = [sb(st, f"gb{i}", [128, D]) for i in range(3)]
            h2b = sb(st, "h2b", [128, D], BF16)
            h2T = sb(st, "h2T", [128, 32, 128], BF16)
            wq_t = [sb(st, f"wq_t{i}", [128, 32, 128], BF16) for i in range(2)]
            qTp = sb(st, "qTp", [128, 16, 128])
            keys_t = sb(st, "keys_t", [128, 16, 128])
            scs = sb(st, "scs", [128, 16, 128])
            wk = sb(st, "wk", [128, 256])
            stop_ = sb(st, "stop_", [128, 16, 16])
            itop = sb(st, "itop", [128, 16, 16], U32)
            itf = sb(st, "itf", [128, 16, 16])
            i0s = sb(st, "i0s", [128, 8, 16])
            cand = sb(st, "cand", [128, 8, 256])
            cidx = sb(st, "cidx", [128, 8, 256])
            bs = sb(st, "bs", [128, 8, 16])
            posu = sb(st, "posu", [128, 8, 16], U32)
            posf = sb(st, "posf", [128, 8, 16])
            eidx = sb(st, "eidx", [128, 128])
            eidi = sb(st, "eidi", [128, 128], I32)
            gsum = sb(st, "gsum", [128, 8])
            gates = sb(st, "gates", [128, 128])
            dots = sb(st, "dots", [128, 128])
            coef = sb(st, "coef", [128, 128])
            iot = sb(st, "iot", [128, 256])
            ssq2 = sb(st, "ssq2", [128, 1])
            rstd2 = sb(st, "rstd2", [128, 1])
            S.dma(keys_t[:], keysT[:, :, :], [], ["keys_t"])
            S.dma(iot[:], iota_in[:, :], [], ["iot"])
            V = nc.vector
            gcount = 0
            gbv = [gb[i // 2][:].bitcast(BF16)[:, (i % 2) * D:(i % 2 + 1) * D] for i in range(6)]
            gbk = [("gb", i // 2, i % 2) for i in range(6)]
            ubk = [("ub16", i) for i in range(64)]
            vbk = [("vb16", i) for i in range(64)]

            def gfull(i):
                return [("gb", i, 0), ("gb", i, 1)]

            def top16(src_ap, n, vals_ap, idx_ap, rk):
                S.op("dve", lambda: V.max(out=vals_ap[:, 0:8], in_=src_ap), rk, ["tv"])
                S.op("dve", lambda: V.max_index(out=idx_ap[:, 0:8], in_max=vals_ap[:, 0:8], in_values=src_ap),
                     rk + ["tv"], ["ti"])
                S.op("dve", lambda: V.match_replace(out=wk[:, 0:n], in_to_replace=vals_ap[:, 0:8], in_values=src_ap,
                                                    imm_value=-1e30), rk + ["tv"], ["wk"])
                S.op("dve", lambda: V.max(out=vals_ap[:, 8:16], in_=wk[:, 0:n]), ["wk"], ["tv"])
                S.op("dve", lambda: V.max_index(out=idx_ap[:, 8:16], in_max=vals_ap[:, 8:16], in_values=wk[:, 0:n]),
                     ["wk", "tv"], ["ti"])

            for ti in range(32):
                sidx = ti // 16
                y_ap = ys if sidx == 0 else yo
                rloc = (ti % 16) * 128
                S.dma(x1t[:], x1[ti * 128:(ti + 1) * 128, :], [("x1", ti, nch) for nch in range(8)], ["x1t"])
                S.dma(gb[0][:], modd[sidx, 1].partition_broadcast(128), [("modd", sidx, 1)], gfull(0))
                S.dma(gb[1][:], modd[sidx, 2].partition_broadcast(128), [("modd", sidx, 2)], gfull(1))
                S.act(gb[2][:].bitcast(BF16)[:, 0:D], x1t[:], AF.Square, ["x1t"], gfull(2) + ["ssq2"], accum_out=ssq2[:])
                S.ts(rstd2[:], ssq2[:], 1.0 / D, EPS, ALU.mult, ALU.add, ["ssq2"], ["rstd2"])
                S.act(rstd2[:], rstd2[:], AF.Sqrt, ["rstd2"], ["rstd2"])
                S.op("dve", lambda: nc.vector.reciprocal(out=rstd2[:], in_=rstd2[:]), ["rstd2"], ["rstd2"])
                S.stt(h2[:], x1t[:], rstd2[:, 0:1], gb[0][:], ALU.mult, ALU.mult, ["x1t", "rstd2"] + gfull(0), ["h2"])
                S.tt(h2[:], h2[:], gb[1][:], ALU.add, ["h2"] + gfull(1), ["h2"])
                S.cp(h2b[:], h2[:], ["h2"], ["h2b"], eng="act")
                for g8 in range(4):
                    pb = g8 % 2
                    for k8 in range(8):
                        kc = g8 * 8 + k8
                        S.tr(psb[:, pb * 1024 + k8 * 128: pb * 1024 + (k8 + 1) * 128],
                             h2b[:, kc * 128:(kc + 1) * 128], identb[:], ["h2b", "identb"], [("psb", pb)])
                    S.cp(h2T[:, g8 * 8:(g8 + 1) * 8, :], psb[:, pb * 1024:(pb + 1) * 1024].rearrange("p (a t) -> p a t", a=8),
                         [("psb", pb)], [("h2T", g8)], eng=("dve" if g8 % 2 == 0 else "act"))
                h2Tk = [("h2T", g8) for g8 in range(4)]
                for f in range(16):
                    i2 = f % 2
                    S.dma(wq_t[i2][:], wqb[f], [("wqb", f)], [("wq_t", i2)])
                    pq = ps[:, i2 * 512:i2 * 512 + 128]
                    for kc in range(32):
                        S.mm(pq, wq_t[i2][:, kc, :], h2T[:, kc, :], kc == 0, kc == 31, [("wq_t", i2)] + h2Tk, [("ps", i2)])
                    S.cp(qTp[:, f, :], pq, [("ps", i2)], [("qTp", f)], eng=("dve" if f % 2 == 0 else "act"))
                for hp in range(16):
                    bk = 2 + hp // 4
                    S.mm(ps[:, bk * 512 + (hp % 4) * 128: bk * 512 + (hp % 4 + 1) * 128], qTp[:, hp, :], keys_t[:, hp, :],
                         True, True, [("qTp", hp), "keys_t"], [("ps", bk)])
                S.cp(scs[:].rearrange("p a n -> p (a n)"), ps[:, 1024:3072], [("ps", 2), ("ps", 3), ("ps", 4), ("ps", 5)], ["scs"])
                for hp in range(16):
                    top16(scs[:, hp, :], 128, stop_[:, hp, :], itop[:, hp, :], ["scs"])
                S.cp(itf[:], itop[:], ["ti"], ["itf"])
                st4 = stop_[:].rearrange("p (h two) k -> p h two k", two=2)
                it4 = itf[:].rearrange("p (h two) k -> p h two k", two=2)
                c4 = cand[:].rearrange("p h (i j) -> p h i j", i=16)
                x4 = cidx[:].rearrange("p h (i j) -> p h i j", i=16)
                S.tt(c4, st4[:, :, 0, :].unsqueeze(3).to_broadcast([128, 8, 16, 16]),
                     st4[:, :, 1, :].unsqueeze(2).to_broadcast([128, 8, 16, 16]), ALU.add, ["tv"], ["cand"])
                S.ts(i0s[:], it4[:, :, 0, :], 128.0, None, ALU.mult, None, ["itf"], ["i0s"])
                S.tt(x4, i0s[:].unsqueeze(3).to_broadcast([128, 8, 16, 16]),
                     it4[:, :, 1, :].unsqueeze(2).to_broadcast([128, 8, 16, 16]), ALU.add, ["i0s", "itf"], ["cidx"])
                oh = gb[2]
                oh3 = oh[:].rearrange("p (k n) -> p k n", k=16)
                for h in range(8):
                    top16(cand[:, h, :], 256, bs[:, h, :], posu[:, h, :], ["cand"])
                    S.cp(posf[:, h, :], posu[:, h, :], ["ti"], [("posf", h)])
                    S.tt(oh3, iot[:].unsqueeze(1).to_broadcast([128, 16, 256]),
                         posf[:, h, :].unsqueeze(2).to_broadcast([128, 16, 256]), ALU.is_equal,
                         ["iot", ("posf", h)], gfull(2))
                    S.tt(oh3, oh3, cidx[:, h, :].unsqueeze(1).to_broadcast([128, 16, 256]), ALU.mult,
                         gfull(2) + ["cidx"], gfull(2))
                    S.op("dve", lambda h=h: V.tensor_reduce(out=eidx[:, h * 16:(h + 1) * 16], in_=oh3, axis=AX.X, op=ALU.add),
                         gfull(2), [("eidx", h)])
                bsk = ["tv"]
                g3 = gates[:].rearrange("p (h k) -> p h k", h=8)
                S.tt(g3, bs[:], bs[:, :, 0:1].to_broadcast([128, 8, 16]), ALU.subtract, bsk, ["gates"])
                S.act(gates[:], gates[:], AF.Exp, ["gates"], ["gates"])
                S.op("dve", lambda: V.tensor_reduce(out=gsum[:], in_=g3, axis=AX.X, op=ALU.add), ["gates"], ["gsum"])
                S.op("dve", lambda: V.reciprocal(out=gsum[:], in_=gsum[:]), ["gsum"], ["gsum"])
                S.tt(g3, g3, gsum[:].unsqueeze(2).to_broadcast([128, 8, 16]), ALU.mult, ["gates", "gsum"], ["gates"])
                eik = [("eidx", h) for h in range(8)]
                S.ts(eidx[:], eidx[:], 0.0, 16383.0, ALU.max, ALU.min, eik, eik)
                S.cp(eidi[:], eidx[:], eik, ["eidi"])
                G = nc.gpsimd
                for e in range(128):
                    bi_ = gcount % 6
                    gcount += 1
                    S.op("pool", lambda bi_=bi_, e=e: G.indirect_dma_start(
                        out=gbv[bi_], out_offset=None, in_=ub16[:, :],
                        in_offset=bass.IndirectOffsetOnAxis(ap=eidi[:, e:e + 1], axis=0)),
                        ["eidi"] + ubk, [gbk[bi_]], dma=True)
                    S.op("dve", lambda bi_=bi_, e=e: V.scalar_tensor_tensor(
                        out=h2b[:], in0=gbv[bi_], scalar=1.0, in1=h2[:], op0=ALU.mult, op1=ALU.mult,
                        accum_out=dots[:, e:e + 1]), [gbk[bi_], "h2"], ["h2b", ("dots", e)])
                dk = [("dots", e) for e in range(128)]
                S.act(coef[:], dots[:], AF.Gelu, dk, ["coef"])
                S.tt(coef[:], coef[:], gates[:], ALU.mult, ["coef", "gates"], ["coef"])
                for e in range(128):
                    bi_ = gcount % 6
                    gcount += 1
                    S.op("pool", lambda bi_=bi_, e=e: G.indirect_dma_start(
                        out=gbv[bi_], out_offset=None, in_=vb16[:, :],
                        in_offset=bass.IndirectOffsetOnAxis(ap=eidi[:, e:e + 1], axis=0)),
                        ["eidi"] + vbk, [gbk[bi_]], dma=True)
                    if e == 0:
                        S.ts(accp[:], gbv[bi_], coef[:, 0:1], None, ALU.mult, None, [gbk[bi_], "coef"], ["accp"])
                    else:
                        S.stt(accp[:], gbv[bi_], coef[:, e:e + 1], accp[:], ALU.mult, ALU.add,
                              [gbk[bi_], "coef", "accp"], ["accp"])
                S.dma(gb[2][:], modd[sidx, 3].partition_broadcast(128), [("modd", sidx, 3)], gfull(2))
                S.tt(accp[:], accp[:], gb[2][:], ALU.mult, ["accp"] + gfull(2), ["accp"])
                S.tt(accp[:], accp[:], x1t[:], ALU.add, ["accp", "x1t"], ["accp"], eng="pool")
                S.dma(y_ap[rloc:rloc + 128, :], accp[:], ["accp"], [("y", ti)])
                S.flush(barrier=False)
            S.flush()
    return nc


def _rope_tables(T):
    pos = np.arange(T)
    rows = (pos // 64).astype(np.float32)
    cols = (pos % 64).astype(np.float32)
    inv = (10000.0 ** (-np.arange(0, 64, 2, dtype=np.float32) / 64.0)).astype(np.float32)
    ar = rows[None, :] * inv[:, None]
    ac = cols[None, :] * inv[:, None]
    C = np.concatenate([np.cos(ar), np.cos(ar), np.cos(ac), np.cos(ac)], axis=0).astype(np.float32)
    Sn = np.concatenate([np.sin(ar), np.sin(ar), np.sin(ac), np.sin(ac)], axis=0).astype(np.float32)
    return np.ascontiguousarray(C), np.ascontiguousarray(Sn)


def _consts():
    ident = np.eye(128, dtype=np.float32)
    R = np.zeros((128, 128), np.float32)
    for base in (0, 64):
        for i in range(32):
            R[base + i, base + 32 + i] = -1.0
            R[base + 32 + i, base + i] = 1.0
    rotT = np.ascontiguousarray(R.T)
    ones = np.ones((128, 128), np.float32)
    return np.ascontiguousarray(np.stack([ident, rotT, ones], axis=1))


def make_in_maps(inp):
    f = lambda a: np.ascontiguousarray(np.asarray(a, dtype=np.float32))
    C, Sn = _rope_tables(TC)
    cst = _consts()
    iota = np.ascontiguousarray(np.tile(np.arange(256, dtype=np.float32)[None, :], (128, 1)))
    fm = lambda v, nch: np.ascontiguousarray(f(v).reshape(nch, 128).T)
    shared = {
        "w_ada": f(inp["w_ada"][0]), "badaT": fm(inp["b_ada"][0], 192),
        "g1T": fm(inp["g_norm1"][0], 32), "g2T": fm(inp["g_norm2"][0], 32),
        "w_in": f(inp["w_in"][0]),
        "qkg": np.ascontiguousarray(np.stack([f(inp["q_gain"][0]), f(inp["k_gain"][0])], axis=1)),
        "convwT": np.ascontiguousarray(f(inp["conv_w"][0]).reshape(4, 16, 128).transpose(2, 1, 0)),
        "convbT": fm(inp["conv_b"][0], 16),
        "lamT": np.ascontiguousarray(f(inp["lru_lam"][0]).reshape(32, 128).T),
        "baT": np.ascontiguousarray(f(inp["lru_ba"][0]).reshape(32, 128).T),
        "biT": np.ascontiguousarray(f(inp["lru_bi"][0]).reshape(32, 128).T),
        "lru_wa": f(inp["lru_wa"][0]).reshape(32, 128, 128),
        "lru_wi": f(inp["lru_wi"][0]).reshape(32, 128, 128),
        "w_attn_o": f(inp["w_attn_o"][0]), "w_lru_o": f(inp["w_lru_o"][0]), "w_out": f(inp["w_out"][0]),
        "peer_wq": f(inp["peer_wq"][0]),
        "keysT": np.ascontiguousarray(f(inp["peer_keys"][0]).reshape(16, 128, 128).transpose(2, 0, 1)),
        "peer_u": f(inp["peer_u"][0]), "peer_v": f(inp["peer_v"][0]),
        "ropeC": C, "ropeS": Sn, "consts": cst, "iota256": iota,
    }
    xp = f(inp["x_prompt"])
    xsm = f(inp["x_sample"])
    cp = f(inp["c_prompt"])
    csm = f(inp["c_sample"])
    maps = []
    for c in range(8):
        b, j = c // 4, c % 4
        cc = np.stack([csm[c], cp[b]], axis=0)
        cTl = np.ascontiguousarray(cc.reshape(2, 32, 128).transpose(2, 1, 0))
        ms = np.zeros((128, 4), np.float32)
        ms[:, j] = 1.0
        m = dict(shared)
        m.update({
            "xs": xsm[c], "xo": np.ascontiguousarray(xp[b, j * TS:(j + 1) * TS]), "xc": xp[b],
            "cT": cTl, "msel": ms,
            "ropeCo": np.ascontiguousarray(C[:, j * TS:(j + 1) * TS]),
            "ropeSo": np.ascontiguousarray(Sn[:, j * TS:(j + 1) * TS]),
        })
        maps.append(m)
    return maps


def kernel(**inputs):
    nc = build()
    maps = make_in_maps(inputs)
    res = run_bass_kernel_spmd(nc, maps, core_ids=list(range(8)))
    y_prompt = np.zeros((2, 8192, D), np.float32)
    y_sample = np.zeros((8, TS, D), np.float32)
    for c in range(8):
        b, j = c // 4, c % 4
        y_sample[c] = res.results[c]["ys"]
        y_prompt[b, j * TS:(j + 1) * TS] = res.results[c]["yo"]
    return (y_prompt, y_sample)
```

```python
import contextlib
import numpy as np
import concourse.bass as bass
import concourse.mybir as mybir
from concourse.bass_utils import run_bass_kernel_spmd

F32 = mybir.dt.float32
BF16 = mybir.dt.bfloat16
U32 = mybir.dt.uint32
I32 = mybir.dt.int32
AF = mybir.ActivationFunctionType
ALU = mybir.AluOpType
AX = mybir.AxisListType

D = 4096
TS = 2048
TC = 8192
NCOL = 15360
EPS = 1e-6
SEM_LIMIT = 30000
DBG = {}


class Op:
    __slots__ = ("eng", "fn", "deps", "dma", "signal", "sem", "val")

    def __init__(self, eng, fn, dma):
        self.eng = eng
        self.fn = fn
        self.dma = dma
        self.deps = ()
        self.signal = False
        self.sem = None
        self.val = 0


class Sched:
    def __init__(self, nc, es):
        self.nc = nc
        self.es = es
        self.engs = {"pe": nc.tensor, "act": nc.scalar, "dve": nc.vector, "pool": nc.gpsimd, "sp": nc.sync}
        self.ops = []
        self.last_w = {}
        self.readers = {}
        self.nsem = 0
        self.cur_sem = {}
        self.cnt = {}
        for e in ("pe", "act", "dve", "pool"):
            self.cur_sem[e] = self._new_sem()
            self.cnt[e] = 0
        self.slots = {"sp": [[self._new_sem(), 0] for _ in range(24)],
                      "pool": [[self._new_sem(), 0] for _ in range(16)]}
        self.slot_i = {"sp": 0, "pool": 0}
        self.seen = {e: {} for e in self.engs}
        self.last_op = {}
        self.trace = {e: [] for e in self.engs} if DBG.get("trace") else None

    def _new_sem(self):
        self.nsem += 1
        return self.es.enter_context(self.nc.semaphore(f"sm{self.nsem}"))

    def op(self, eng, fn, r=(), w=(), dma=False):
        w = list(w) + [k for k in r if isinstance(k, tuple) and k[0] in ("ps", "psb")]
        o = Op(eng, fn, dma)
        deps = {}
        for k in r:
            lw = self.last_w.get(k)
            if lw is not None:
                deps[id(lw)] = lw
        for k in w:
            lw = self.last_w.get(k)
            if lw is not None:
                deps[id(lw)] = lw
            rd = self.readers.get(k)
            if rd:
                for x in rd.values():
                    deps[id(x)] = x
        rk = ("d", id(o)) if dma else eng
        for k in r:
            self.readers.setdefault(k, {})[rk] = o
        for k in w:
            self.last_w[k] = o
            self.readers[k] = {}
        dl = []
        for d in deps.values():
            if eng == "pe" and not dma and d.eng == "pe" and not d.dma:
                continue
            d.signal = True
            dl.append(d)
        o.deps = dl
        self.ops.append(o)
        return o

    def _wait(self, E, sem, val):
        sn = self.seen[E]
        if sn.get(id(sem), 0) < val:
            self.engs[E].wait_ge(sem, val)
            sn[id(sem)] = val
            if self.trace is not None:
                self.trace[E].append(("w", id(sem), val))

    def flush(self, barrier=True):
        for lw in self.last_w.values():
            lw.signal = True
        for rd in self.readers.values():
            for x in rd.values():
                x.signal = True
        lastc = {}
        for o in self.ops:
            if not o.dma:
                lastc[o.eng] = o
        for o in lastc.values():
            o.signal = True
        for o in self.ops:
            E = o.eng
            eng = self.engs[E]
            for d in o.deps:
                self._wait(E, d.sem, d.val)
            if o.dma:
                sl = self.slots[E]
                i = self.slot_i[E]
                self.slot_i[E] = (i + 1) % len(sl)
                sem, uses = sl[i]
                if uses > 0:
                    self._wait(E, sem, 16 * uses)
                if 16 * (uses + 1) > SEM_LIMIT:
                    sem = self._new_sem()
                    uses = 0
                    sl[i][0] = sem
                ins = o.fn()
                ins.then_inc(sem, 16)
                if self.trace is not None:
                    self.trace[E].append(("s", id(sem), 16))
                uses += 1
                sl[i][1] = uses
                o.sem = sem
                o.val = 16 * uses
            else:
                ins = o.fn()
                if o.signal:
                    if self.cnt[E] >= SEM_LIMIT:
                        self.cur_sem[E] = self._new_sem()
                        self.cnt[E] = 0
                    self.cnt[E] += 1
                    ins.then_inc(self.cur_sem[E], 1)
                    if self.trace is not None:
                        self.trace[E].append(("s", id(self.cur_sem[E]), 1))
                    o.sem = self.cur_sem[E]
                    o.val = self.cnt[E]
            o.fn = None
        self.ops = []
        if barrier:
            for E in self.engs:
                for E2 in ("pe", "act", "dve", "pool"):
                    if E2 != E and self.cnt[E2] > 0:
                        self._wait(E, self.cur_sem[E2], self.cnt[E2])
                for q in ("sp", "pool"):
                    for sem, uses in self.slots[q]:
                        if uses > 0:
                            self._wait(E, sem, 16 * uses)

    def dma(self, out, in_, r, w, q="sp"):
        eng = self.engs[q]
        return self.op(q, lambda: eng.dma_start(out=out, in_=in_), r, w, dma=True)

    def mm(self, out, lhsT, rhs, start, stop, r, w):
        t = self.nc.tensor
        return self.op("pe", lambda: t.matmul(out, lhsT=lhsT, rhs=rhs, start=start, stop=stop), r, w)

    def tr(self, out, in_, ident, r, w):
        t = self.nc.tensor
        return self.op("pe", lambda: t.transpose(out=out, in_=in_, identity=ident), r, w)

    def act(self, out, in_, func, r, w, bias=None, scale=None, accum_out=None):
        a = self.nc.scalar
        kw = {}
        if bias is not None:
            kw["bias"] = bias
        if scale is not None:
            kw["scale"] = scale
        if accum_out is not None:
            kw["accum_out"] = accum_out
        return self.op("act", lambda: a.activation(out=out, in_=in_, func=func, **kw), r, w)

    def ts(self, out, in0, s1, s2, op0, op1, r, w, eng="dve"):
        e = self.engs[eng]
        if op1 is None:
            return self.op(eng, lambda: e.tensor_scalar(out=out, in0=in0, scalar1=s1, scalar2=None, op0=op0), r, w)
        return self.op(eng, lambda: e.tensor_scalar(out=out, in0=in0, scalar1=s1, scalar2=s2, op0=op0, op1=op1), r, w)

    def tt(self, out, in0, in1, op, r, w, eng="dve"):
        e = self.engs[eng]
        return self.op(eng, lambda: e.tensor_tensor(out=out, in0=in0, in1=in1, op=op), r, w)

    def stt(self, out, in0, scalar, in1, op0, op1, r, w):
        e = self.nc.vector
        return self.op("dve", lambda: e.scalar_tensor_tensor(out=out, in0=in0, scalar=scalar, in1=in1, op0=op0, op1=op1), r, w)

    def cp(self, out, in_, r, w, eng="dve"):
        e = self.engs[eng]
        if eng == "act":
            return self.op(eng, lambda: e.copy(out=out, in_=in_), r, w)
        return self.op(eng, lambda: e.tensor_copy(out=out, in_=in_), r, w)


def build(debug=False, dbg_names=(), stop=None):
    nc = bass.Bass("TRN2", target_bir_lowering=False)

    def din(name, shape, dt=F32):
        return nc.dram_tensor(name, list(shape), dt, kind="ExternalInput").ap()

    def dscr(name, shape, dt=F32):
        kind = "ExternalOutput" if name in dbg_names else "Internal"
        return nc.dram_tensor(name, list(shape), dt, kind=kind).ap()

    xs = din("xs", [TS, D])
    xo = din("xo", [TS, D])
    xc = din("xc", [TC, D])
    cT = din("cT", [128, 32, 2])
    w_ada = din("w_ada", [D, 6 * D])
    badaT = din("badaT", [128, 192])
    g1T = din("g1T", [128, 32])
    g2T = din("g2T", [128, 32])
    w_in = din("w_in", [D, NCOL])
    qkg = din("qkg", [128, 2])
    convwT = din("convwT", [128, 16, 4])
    convbT = din("convbT", [128, 16])
    lamT = din("lamT", [128, 32])
    baT = din("baT", [128, 32])
    biT = din("biT", [128, 32])
    lru_wa = din("lru_wa", [32, 128, 128])
    lru_wi = din("lru_wi", [32, 128, 128])
    w_attn_o = din("w_attn_o", [2048, D])
    w_lru_o = din("w_lru_o", [2048, D])
    w_out = din("w_out", [D, D])
    peer_wq = din("peer_wq", [D, 2048])
    keysT = din("keysT", [128, 16, 128])
    peer_u = din("peer_u", [16384, D])
    peer_v = din("peer_v", [16384, D])
    ropeC = din("ropeC", [128, TC])
    ropeS = din("ropeS", [128, TC])
    ropeCo = din("ropeCo", [128, TS])
    ropeSo = din("ropeSo", [128, TS])
    consts = din("consts", [128, 3, 128])
    iota_in = din("iota256", [128, 256])
    msel = din("msel", [128, 4])

    ys = nc.dram_tensor("ys", [TS, D], F32, kind="ExternalOutput").ap()
    yo = nc.dram_tensor("yo", [TS, D], F32, kind="ExternalOutput").ap()

    winb = dscr("winb", [120, 128, 32, 128], BF16)
    waob = dscr("waob", [32, 128, 16, 128], BF16)
    wlob = dscr("wlob", [32, 128, 16, 128], BF16)
    woutb = dscr("woutb", [8, 128, 32, 512], BF16)
    wqb = dscr("wqb", [16, 128, 32, 128], BF16)
    modd = dscr("modd", [2, 4, D])
    QT = dscr("QT", [2, 16, 128, TS], BF16)
    KT_s = dscr("KT_s", [4, 128, TS], BF16)
    KT_c = dscr("KT_c", [4, 128, TC], BF16)
    V_s = dscr("V_s", [TS, 512], BF16)
    V_c = dscr("V_c", [TC, 512], BF16)
    xl_s = dscr("xl_s", [16, 128, TS])
    xl_c = dscr("xl_c", [16, 128, TC])
    gy = dscr("gy", [2, 16, 128, TS])
    ga = dscr("ga", [2, 32, 128, TS])
    gl = dscr("gl", [2, 32, 128, TS])
    attnT = dscr("attnT", [2, 16, 128, TS], BF16)
    lruT = dscr("lruT", [2, 16, 128, TS], BF16)
    x1 = dscr("x1", [2 * TS, D])
    ub16 = dscr("ub16", [16384, D], BF16)
    vb16 = dscr("vb16", [16384, D], BF16)
    dbg = {}
    if debug:
        dbg["modT"] = nc.dram_tensor("dbg_modT", [128, 192, 2], F32, kind="ExternalOutput").ap()

    es = contextlib.ExitStack()
    with es:
        S = Sched(nc, es)
        nc._sched = S

        nm = [0]

        def sb(st, name, shape, dt=F32):
            nm[0] += 1
            return st.enter_context(nc.sbuf_tensor(f"{name}_{nm[0]}", list(shape), dt))

        ps = es.enter_context(nc.psum_tensor("ps", [128, 3072], F32))
        psb = es.enter_context(nc.psum_tensor("psb", [128, 2048], BF16))
        cst = sb(es, "cst", [128, 3, 128])
        identb = sb(es, "identb", [128, 128], BF16)
        onesb = sb(es, "onesb", [128, 128], BF16)
        modT = sb(es, "modT", [128, 192, 2])
        G1 = sb(es, "G1", [128, 32, 2])
        G2 = sb(es, "G2", [128, 32, 2])
        qkg_t = sb(es, "qkg_t", [128, 2])
        msel_t = sb(es, "msel_t", [128, 4])
        identf = cst[:, 0, :]
        rotT = cst[:, 1, :]
        onesf = cst[:, 2, :]

        def bank(i):
            return ps[:, i * 512:(i + 1) * 512]

        S.dma(cst[:], consts[:, :, :], [], ["cst"])
        S.dma(qkg_t[:], qkg[:, :], [], ["qkg"])
        S.dma(msel_t[:], msel[:, :], [], ["msel"])
        S.cp(identb[:], cst[:, 0, :], ["cst"], ["identb"])
        S.cp(onesb[:], cst[:, 2, :], ["cst"], ["onesb"])

        def cast_w(dst, src, nf, kc, cw):
            for f in range(nf):
                S.dma(dst[f], src[:, f * cw:(f + 1) * cw].rearrange("(kc p) c -> p kc c", p=128),
                      [], [(dst.tensor.name, f)], q="pool")
        cast_w(winb, w_in, 120, 32, 128)
        cast_w(waob, w_attn_o, 32, 16, 128)
        cast_w(wlob, w_lru_o, 32, 16, 128)
        cast_w(woutb, w_out, 8, 32, 512)
        cast_w(wqb, peer_wq, 16, 32, 128)

        with contextlib.ExitStack() as st:
            cT_t = sb(st, "cT_t", [128, 32, 2])
            scT = sb(st, "scT", [128, 32, 2])
            bada_t = sb(st, "bada_t", [128, 192])
            g1_t = sb(st, "g1_t", [128, 32])
            g2_t = sb(st, "g2_t", [128, 32])
            wa = [sb(st, f"wa{i}", [128, 32, 256]) for i in range(2)]
            tmpc = sb(st, "tmpc", [128, 32])
            tmpr = sb(st, "tmpr", [32, 128])
            S.dma(cT_t[:], cT[:, :, :], [], ["cT"])
            S.dma(bada_t[:], badaT[:, :], [], ["bada"])
            S.dma(g1_t[:], g1T[:, :], [], ["g1"])
            S.dma(g2_t[:], g2T[:, :], [], ["g2"])
            S.act(scT[:], cT_t[:], AF.Silu, ["cT"], ["scT"])
            for j2 in range(96):
                buf = wa[j2 % 2]
                S.dma(buf[:], w_ada[:, j2 * 256:(j2 + 1) * 256].rearrange("(kc p) c -> p kc c", p=128),
                      [], [("wa", j2 % 2)])
                for half in range(2):
                    j = j2 * 2 + half
                    b = j % 6
                    pst = ps[:, b * 512:b * 512 + 2]
                    for kc in range(32):
                        S.mm(pst, buf[:, kc, half * 128:(half + 1) * 128], scT[:, kc, :], kc == 0, kc == 31,
                             [("wa", j2 % 2), "scT"], [("ps", b)])
                    S.ts(modT[:, j, :], pst, bada_t[:, j:j + 1], None, ALU.add, None,
                         [("ps", b), "bada"], [("modT", j)])
            allmod = [("modT", j) for j in range(192)]
            S.ts(G1[:], modT[:, 32:64, :], 1.0, None, ALU.add, None, allmod, ["G1"])
            S.tt(G1[:], G1[:], g1_t[:].unsqueeze(2).to_broadcast([128, 32, 2]), ALU.mult, ["G1", "g1"], ["G1"])
            S.ts(G2[:], modT[:, 128:160, :], 1.0, None, ALU.add, None, allmod, ["G2"])
            S.tt(G2[:], G2[:], g2_t[:].unsqueeze(2).to_broadcast([128, 32, 2]), ALU.mult, ["G2", "g2"], ["G2"])
            srcs = [(modT, 64), (G2, 0), (modT, 96), (modT, 160)]
            for m in range(2):
                for sl, (srct, off) in enumerate(srcs):
                    S.cp(tmpc[:], srct[:, off:off + 32, m], allmod + ["G2"], ["tmpc"])
                    S.tr(ps[0:32, 0:128], tmpc[:], identf, ["tmpc", "cst"], [("ps", 0)])
                    S.cp(tmpr[:], ps[0:32, 0:128], [("ps", 0)], ["tmpr"])
                    S.dma(modd[m, sl].rearrange("(kc p) -> kc p", p=128), tmpr[:], ["tmpr"], [("modd", m, sl)])
            if debug:
                S.dma(dbg["modT"][:, :, :], modT[:], allmod, ["dbg_modT"])
            S.flush()

        def phase_c(tag, x_ap, T, m, cols, rc, rs, oq, ok, ov, oxl, sidx):
            with contextlib.ExitStack() as st:
                NXB = DBG.get("nxb", 2)
                xt = [sb(st, f"xt{i}", [128, D]) for i in range(NXB)]
                NXN = DBG.get("nxn", NXB)
                xn = [sb(st, f"xn{i}", [128, D], BF16) for i in range(NXN)]
                hT = [sb(st, f"hT{i}", [128, 32, 512], BF16) for i in range(DBG.get("nhT", 2))]
                wt = [sb(st, f"wt{i}", [128, 32, 128], BF16) for i in range(3)]
                ssq = sb(st, "ssq", [128, 2])
                rstd = sb(st, "rstd", [128, 2])
                sqjunk = sb(st, "sqjunk", [128, D], BF16)
                ct = [sb(st, f"ct{i}", [128, 512]) for i in range(2)]
                stt_ = [sb(st, f"st{i}", [128, 512]) for i in range(2)]
                sqf = sb(st, "sqf", [128, 512])
                rsf = sb(st, "rsf", [128, 512])
                qn = sb(st, "qn", [128, 512])
                t1 = sb(st, "t1", [128, 512])
                t2 = sb(st, "t2", [128, 512])
                ob = [sb(st, f"ob{i}", [128, 512], BF16) for i in range(2)]
                of = [sb(st, f"of{i}", [128, 512]) for i in range(2)]
                vT = sb(st, "vT", [128, 512], BF16)
                vtile = [sb(st, f"vtile{i}", [128, 4, 128], BF16) for i in range(2)]
                nblk = DBG.get("nblk", T // 512)
                if "kinds" in DBG:
                    cols = [c_ for c_ in cols if c_[1] in DBG["kinds"]][:DBG.get("ncols", 1000)]
                has_rope = any(kind in ("q", "k") for (_, kind, _) in cols)
                wcount = 0
                pcount = 0
                ocount = 0
                for b in range(nblk):
                    hb = hT[b % len(hT)]
                    hk = ("hT", b % len(hT))
                    for tt_ in range(DBG.get("ntile", 4)):
                        ti = b * 4 + tt_
                        xb = xt[ti % NXB]
                        xk = ("xt", ti % NXB)
                        nb_ = xn[ti % NXN]
                        nk = ("xn", ti % NXN)
                        S.dma(xb[:], x_ap[ti * 128:(ti + 1) * 128, :], [], [xk])
                        S.act(sqjunk[:], xb[:], AF.Square, [xk], ["sqjunk", ("ssq", ti % 2)], accum_out=ssq[:, ti % 2:ti % 2 + 1])
                        S.ts(rstd[:, ti % 2:ti % 2 + 1], ssq[:, ti % 2:ti % 2 + 1], 1.0 / D, EPS, ALU.mult, ALU.add,
                             [("ssq", ti % 2)], [("rstd", ti % 2)])
                        S.act(rstd[:, ti % 2:ti % 2 + 1], rstd[:, ti % 2:ti % 2 + 1], AF.Sqrt,
                              [("rstd", ti % 2)], [("rstd", ti % 2)])
                        S.op("dve", lambda ti=ti: nc.vector.reciprocal(out=rstd[:, ti % 2:ti % 2 + 1], in_=rstd[:, ti % 2:ti % 2 + 1]),
                             [("rstd", ti % 2)], [("rstd", ti % 2)])
                        S.act(nb_[:], xb[:], AF.Identity, [xk, ("rstd", ti % 2)], [nk], scale=rstd[:, ti % 2:ti % 2 + 1])
                        fes = DBG.get("fe", 9)
                        if tt_ >= DBG.get("fe_tiles", 4):
                            fes = 1
                        for g8 in range(DBG.get("ng8", 4) if fes >= 2 else 0):
                            pb = g8 % 2
                            for k8 in range(8):
                                kc = g8 * 8 + k8
                                S.tr(psb[:, pb * 1024 + k8 * 128: pb * 1024 + (k8 + 1) * 128],
                                     nb_[:, kc * 128:(kc + 1) * 128], identb[:], [nk, "identb"], [("psb", pb)])
                            for k8 in range(8 if fes >= 3 else 0):
                                kc = g8 * 8 + k8
                                src = psb[:, pb * 1024 + k8 * 128: pb * 1024 + (k8 + 1) * 128]
                                dst = hb[:, kc, tt_ * 128:(tt_ + 1) * 128]
                                if (pb == 0 and fes != 4) or fes == 3:
                                    S.ts(dst, src, G1[:, kc, m:m + 1], modT[:, kc, m:m + 1], ALU.mult, ALU.add,
                                         [("psb", pb), "G1"], [(hk, kc, tt_)])
                                else:
                                    S.act(dst, src, AF.Identity, [("psb", pb), "G1"], [(hk, kc, tt_)],
                                          bias=modT[:, kc, m:m + 1], scale=G1[:, kc, m:m + 1])
                    hkeys = [(hk, kc, t4) for kc in range(32) for t4 in range(4)]
                    if has_rope:
                        cb = ct[b % 2]
                        sbb = stt_[b % 2]
                        S.dma(cb[:], rc[:, b * 512:(b + 1) * 512], [], [("ct", b % 2)])
                        S.dma(sbb[:], rs[:, b * 512:(b + 1) * 512], [], [("st", b % 2)])
                    tsl = slice(b * 512, (b + 1) * 512)
                    ncols_ = len(cols)
                    wbase = wcount

                    def issue_load(ci, wbase=wbase):
                        wi_ = (wbase + ci) % 3
                        S.dma(wt[wi_][:], winb[cols[ci][0]], [("winb", cols[ci][0])], [("wt", wi_)])
                    for ci in range(min(2, ncols_)):
                        issue_load(ci)
                    for ci, (f, kind, idx) in enumerate(cols):
                        if ci + 2 < ncols_:
                            issue_load(ci + 2)
                        wb = wt[wcount % 3]
                        wk = ("wt", wcount % 3)
                        wcount += 1
                        pbk = pcount % 4
                        pcount += 1
                        pq = bank(pbk)
                        pk = ("ps", pbk)
                        for kc in range(32):
                            S.mm(pq, wb[:, kc, :], hb[:, kc, :], kc == 0, kc == 31,
                                 [wk] + [(hk, kc, t4) for t4 in range(4)], [pk])
                        if kind in ("q", "k"):
                            S.act(sqf[:], pq, AF.Square, [pk], ["sqf"])
                            S.mm(bank(4), onesf, sqf[:], True, True, ["sqf", "cst"], [("ps", 4)])
                            S.ts(rsf[:], bank(4), 1.0 / 128, EPS, ALU.mult, ALU.add, [("ps", 4)], ["rsf"])
                            S.act(rsf[:], rsf[:], AF.Sqrt, ["rsf"], ["rsf"])
                            S.op("dve", lambda: nc.vector.reciprocal(out=rsf[:], in_=rsf[:]), ["rsf"], ["rsf"])
                            gcol = 0 if kind == "q" else 1
                            S.stt(qn[:], pq, qkg_t[:, gcol:gcol + 1], rsf[:], ALU.mult, ALU.mult,
                                  [pk, "rsf", "qkg"], ["qn"])
                            S.mm(bank(5), rotT, qn[:], True, True, ["qn", "cst"], [("ps", 5)])
                            S.tt(t1[:], qn[:], cb[:], ALU.mult, ["qn", ("ct", b % 2)], ["t1"])
                            S.tt(t2[:], bank(5), sbb[:], ALU.mult, [("ps", 5), ("st", b % 2)], ["t2"])
                            o = ob[ocount % 2]
                            okk = ("ob", ocount % 2)
                            ocount += 1
                            S.tt(o[:], t1[:], t2[:], ALU.add, ["t1", "t2"], [okk])
                            dst = oq[idx][:, tsl] if kind == "q" else ok[idx][:, tsl]
                            S.dma(dst, o[:], [okk], [(tag, kind, idx, b)])
                        elif kind == "v":
                            S.cp(vT[:], pq, [pk], ["vT"], eng="act")
                            vt_ = vtile[idx % 2]
                            for t4 in range(4):
                                S.tr(psb[:, t4 * 128:(t4 + 1) * 128], vT[:, t4 * 128:(t4 + 1) * 128], identb[:],
                                     ["vT", "identb"], [("psb", 0)])
                            S.cp(vt_[:], psb[:, 0:512].rearrange("p (a d) -> p a d", a=4),
                                 [("psb", 0)], [("vtile", idx % 2)])
                            S.dma(ov[b * 512:(b + 1) * 512, idx * 128:(idx + 1) * 128].rearrange("(a p) d -> p a d", p=128),
                                  vt_[:], [("vtile", idx % 2)], [(tag, "v", idx, b)])
                        else:
                            o = of[ocount % 2]
                            okk = ("of", ocount % 2)
                            ocount += 1
                            if kind == "xl":
                                S.cp(o[:], pq, [pk], [okk], eng="act")
                                dst = oxl[idx][:, tsl]
                            elif kind == "yl":
                                S.act(o[:], pq, AF.Gelu, [pk], [okk])
                                dst = gy[sidx, idx][:, tsl]
                            elif kind == "ga":
                                S.act(o[:], pq, AF.Sigmoid, [pk], [okk])
                                dst = ga[sidx, idx][:, tsl]
                            else:
                                S.act(o[:], pq, AF.Sigmoid, [pk], [okk])
                                dst = gl[sidx, idx][:, tsl]
                            S.dma(dst, o[:], [okk], [(tag, kind, idx, b)])
                    S.flush(barrier=False)
                S.flush()

        cols_q = [(h, "q", h) for h in range(16)]
        cols_k = [(16 + g, "k", g) for g in range(4)]
        cols_v = [(20 + g, "v", g) for g in range(4)]
        cols_xl = [(24 + n, "xl", n) for n in range(16)]
        cols_yl = [(40 + n, "yl", n) for n in range(16)]
        cols_ga = [(56 + n, "ga", n) for n in range(32)]
        cols_gl = [(88 + n, "gl", n) for n in range(32)]

        if stop == "A":
            return nc
        for i in range(64):
            S.dma(ub16[i * 256:(i + 1) * 256, :], peer_u[i * 256:(i + 1) * 256, :], [], [("ub16", i)], q="pool")
        for i in range(64):
            S.dma(vb16[i * 256:(i + 1) * 256, :], peer_v[i * 256:(i + 1) * 256, :], [], [("vb16", i)], q="pool")
        phase_c("S", xs, TS, 0, cols_q + cols_k + cols_v + cols_xl + cols_yl + cols_ga + cols_gl,
                ropeC, ropeS, QT[0], KT_s, V_s, xl_s, 0)
        if stop == "CS":
            return nc
        phase_c("O", xo, TS, 1, cols_q + cols_yl + cols_ga + cols_gl, ropeCo, ropeSo, QT[1], None, None, None, 1)
        phase_c("C", xc, TC, 1, cols_k + cols_v + cols_xl, ropeC, ropeS, None, KT_c, V_c, xl_c, 1)

        def phase_d(tag, xl_ap, T, sidx, use_sel, tag_xl, gy_tag):
            nq = T // TS
            with contextlib.ExitStack() as st:
                cw = sb(st, "cw", [128, 16, 4])
                cbt = sb(st, "cbt", [128, 16])
                lam = sb(st, "lam", [128, 32])
                cA = sb(st, "cA", [128, 32])
                cA2 = sb(st, "cA2", [128, 32])
                ba_t = sb(st, "ba_t", [128, 32])
                bi_t = sb(st, "bi_t", [128, 32])
                wa_t = sb(st, "wa_t", [128, 32, 128])
                wi_t = sb(st, "wi_t", [128, 32, 128])
                xpad = sb(st, "xpad", [128, TS + 3])
                xcv = sb(st, "xcv", [128, TS])
                rg = sb(st, "rg", [128, TS])
                ig = sb(st, "ig", [128, TS])
                a_t = sb(st, "a_t", [128, TS])
                mu = sb(st, "mu", [128, TS])
                hh = sb(st, "hh", [128, TS])
                acc = sb(st, "acc", [128, TS])
                gyt = sb(st, "gyt", [128, TS])
                lo = sb(st, "lo", [128, TS], BF16)
                stt = sb(st, "stt", [128, 2])
                S.dma(cw[:], convwT[:, :, :], [], ["cw"])
                S.dma(cbt[:], convbT[:, :], [], ["cbt"])
                S.dma(lam[:], lamT[:, :], [], ["lam"])
                S.dma(ba_t[:], baT[:, :], [], ["ba"])
                S.dma(bi_t[:], biT[:, :], [], ["bi"])
                S.dma(wa_t[:], lru_wa.rearrange("n c d -> c n d"), [], ["wa_t"])
                S.dma(wi_t[:], lru_wi.rearrange("n c d -> c n d"), [], ["wi_t"])
                S.act(cA[:], lam[:], AF.Exp, ["lam"], ["cA"], scale=-1.0)
                S.act(cA[:], cA[:], AF.Ln, ["cA"], ["cA"], bias=1.0)
                S.ts(cA[:], cA[:], -8.0, None, ALU.mult, None, ["cA"], ["cA"])
                S.ts(cA2[:], cA[:], 2.0, None, ALU.mult, None, ["cA"], ["cA2"])
                for n in range(16):
                    first = True
                    for dr in range(2):
                        gi = dr * 16 + n
                        S.op("pool", lambda dr=dr: nc.gpsimd.memset(stt[:, dr:dr + 1], 0.0), [], [("stt", dr)])
                        qs = range(nq) if dr == 0 else range(nq - 1, -1, -1)
                        for q in qs:
                            lo_t = q * TS - 2
                            hi_t = q * TS + TS + 1
                            a0 = max(lo_t, 0)
                            a1 = min(hi_t, T)
                            if lo_t < 0:
                                S.op("pool", lambda: nc.gpsimd.memset(xpad[:, 0:2], 0.0), [], ["xpad"])
                            if hi_t > T:
                                S.op("pool", lambda: nc.gpsimd.memset(xpad[:, TS + 2:TS + 3], 0.0), [], ["xpad"])
                            S.dma(xpad[:, a0 - lo_t:a1 - lo_t], xl_ap[n][:, a0:a1],
                                  [(tag_xl, "xl", n, bb) for bb in range(T // 512)], ["xpad"])
                            S.ts(xcv[:], xpad[:, 0:TS], cw[:, n, 0:1], cbt[:, n:n + 1], ALU.mult, ALU.add,
                                 ["xpad", "cw", "cbt"], ["xcv"])
                            for j in range(1, 4):
                                S.stt(xcv[:], xpad[:, j:j + TS], cw[:, n, j:j + 1], xcv[:], ALU.mult, ALU.add,
                                      ["xpad", "cw", "xcv"], ["xcv"])
                            for (wt_, bt_, gt_, gk, wkey, bkey) in ((wa_t, ba_t, rg, "rg", "wa_t", "ba"), (wi_t, bi_t, ig, "ig", "wi_t", "bi")):
                                for half in range(2):
                                    pb0 = 0 if gk == "rg" else 2
                                    for blk in range(2):
                                        c0 = half * 1024 + blk * 512
                                        S.mm(bank(pb0 + blk), wt_[:, gi, :], xcv[:, c0:c0 + 512], True, True,
                                             ["xcv", wkey], [("ps", pb0 + blk)])
                                    S.act(gt_[:, half * 1024:(half + 1) * 1024], ps[:, pb0 * 512:(pb0 + 2) * 512],
                                          AF.Sigmoid, [("ps", pb0), ("ps", pb0 + 1), bkey], [gk],
                                          bias=bt_[:, gi:gi + 1])
                            S.act(a_t[:], rg[:], AF.Exp, ["rg", "cA"], ["a_t"], scale=cA[:, gi:gi + 1])
                            S.act(mu[:], rg[:], AF.Exp, ["rg", "cA2"], ["mu"], scale=cA2[:, gi:gi + 1])
                            S.ts(mu[:], mu[:], -1.0, 1.0, ALU.mult, ALU.add, ["mu"], ["mu"])
                            S.ts(mu[:], mu[:], 0.0, None, ALU.max, None, ["mu"], ["mu"])
                            S.act(mu[:], mu[:], AF.Sqrt, ["mu"], ["mu"])
                            S.tt(ig[:], ig[:], xcv[:], ALU.mult, ["ig", "xcv"], ["ig"], eng="pool")
                            S.tt(ig[:], ig[:], mu[:], ALU.mult, ["ig", "mu"], ["ig"], eng="pool")
                            if dr == 0:
                                S.op("dve", lambda: nc.vector.tensor_tensor_scan(
                                    out=hh[:], data0=a_t[:], data1=ig[:], initial=stt[:, 0:1], op0=ALU.mult, op1=ALU.add),
                                    ["a_t", "ig", ("stt", 0)], ["hh"])
                                S.cp(stt[:, 0:1], hh[:, TS - 1:TS], ["hh"], [("stt", 0)])
                            else:
                                S.op("dve", lambda: nc.vector.tensor_tensor_scan(
                                    out=hh[:, ::-1], data0=a_t[:, ::-1], data1=ig[:, ::-1], initial=stt[:, 1:2],
                                    op0=ALU.mult, op1=ALU.add),
                                    ["a_t", "ig", ("stt", 1)], ["hh"])
                                S.cp(stt[:, 1:2], hh[:, 0:1], ["hh"], [("stt", 1)])
                            sel = msel_t[:, q:q + 1] if use_sel else 1.0
                            if first:
                                S.ts(acc[:], hh[:], sel, None, ALU.mult, None, ["hh", "msel"], ["acc"])
                                first = False
                            else:
                                S.stt(acc[:], hh[:], sel, acc[:], ALU.mult, ALU.add, ["hh", "msel", "acc"], ["acc"])
                    S.dma(gyt[:], gy[sidx, n], [(gy_tag, "yl", n, bb) for bb in range(4)], ["gyt"])
                    S.tt(lo[:], acc[:], gyt[:], ALU.mult, ["acc", "gyt"], ["lo"])
                    S.dma(lruT[sidx, n], lo[:], ["lo"], [("lruT", sidx, n)])
                    S.flush(barrier=False)
                S.flush()

        if stop == "C":
            return nc
        phase_d("S", xl_s, TS, 0, False, "S", "S")
        if stop == "DS":
            return nc
        phase_d("C", xl_c, TC, 1, True, "C", "O")

        def phase_e(sidx, qtag, ktag, KT_ap, V_ap, Tk):
            nkt = Tk // 128
            with contextlib.ExitStack() as st:
                kT = sb(st, "kT", [128, Tk], BF16)
                vt = sb(st, "vt", [128, nkt, 128], BF16)
                qT = [sb(st, f"qT{i}", [128, TS], BF16) for i in range(2)]
                pT = [sb(st, f"pT{i}", [128, 512], BF16) for i in range(3)]
                rden = sb(st, "rden", [128, 512])
                ot = [sb(st, f"ot{i}", [128, 512], BF16) for i in range(2)]
                sc = 128.0 ** -0.5
                hcount = 0
                ucount = 0
                for g in range(4):
                    S.dma(kT[:], KT_ap[g], [(ktag, "k", g, bb) for bb in range(Tk // 512)], ["kT"])
                    S.dma(vt[:], V_ap[:, g * 128:(g + 1) * 128].rearrange("(kt p) d -> p kt d", p=128),
                          [(ktag, "v", g, bb) for bb in range(Tk // 512)], ["vt"])
                    for hh_ in range(4):
                        h = g * 4 + hh_
                        qb_ = qT[hcount % 2]
                        qk = ("qT", hcount % 2)
                        hcount += 1
                        S.dma(qb_[:], QT[sidx, h], [(qtag, "q", h, bb) for bb in range(4)], [qk])
                        for qb in range(4):
                            po = bank(2 + ucount % 2)
                            pok = ("ps", 2 + ucount % 2)
                            pd = bank(4 + ucount % 2)
                            pdk = ("ps", 4 + ucount % 2)
                            ucount += 1
                            qsl = qb_[:, qb * 512:(qb + 1) * 512]

                            def qk_mm(kt):
                                S.mm(bank(kt % 2), kT[:, kt * 128:(kt + 1) * 128], qsl, True, True,
                                     ["kT", qk], [("ps", kt % 2)])
                            qk_mm(0)
                            for kt in range(nkt):
                                if kt + 1 < nkt:
                                    qk_mm(kt + 1)
                                p_ = pT[kt % 3]
                                pk_ = ("pT", kt % 3)
                                S.act(p_[:], bank(kt % 2), AF.Exp, [("ps", kt % 2)], [pk_], scale=sc)
                                S.mm(po, vt[:, kt, :], p_[:], kt == 0, kt == nkt - 1, ["vt", pk_], [pok])
                                S.mm(pd, onesb[:], p_[:], kt == 0, kt == nkt - 1, ["onesb", pk_], [pdk])
                            S.op("dve", lambda pd=pd: nc.vector.reciprocal(out=rden[:], in_=pd), [pdk], ["rden"])
                            o_ = ot[ucount % 2]
                            S.tt(o_[:], po, rden[:], ALU.mult, [pok, "rden"], [("ot", ucount % 2)])
                            S.dma(attnT[sidx, h][:, qb * 512:(qb + 1) * 512], o_[:], [("ot", ucount % 2)],
                                  [("attnT", sidx, h, qb)])
                        S.flush(barrier=False)
                S.flush()

        if stop == "D":
            return nc
        phase_e(0, "S", "S", KT_s, V_s, TS)
        if stop == "ES":
            return nc
        phase_e(1, "O", "C", KT_c, V_c, TC)

        if stop == "E":
            return nc
        with contextlib.ExitStack() as st:
            aT = sb(st, "aT", [128, 16, 512], BF16)
            lT = sb(st, "lT", [128, 16, 512], BF16)
            mg = sb(st, "mg", [128, 32, 512], BF16)
            wao_t = [sb(st, f"wao{i}", [128, 16, 128], BF16) for i in range(2)]
            wlo_t = [sb(st, f"wlo{i}", [128, 16, 128], BF16) for i in range(2)]
            ga_t = [sb(st, f"ga_t{i}", [128, 512]) for i in range(2)]
            gl_t = [sb(st, f"gl_t{i}", [128, 512]) for i in range(2)]
            f1 = sb(st, "f1", [128, 512])
            f2 = sb(st, "f2", [128, 512])
            wo_t2 = [sb(st, f"wo_t{i}", [128, 32, 512], BF16) for i in range(2)]
            g1bc = sb(st, "g1bc", [128, D])
            xx = [sb(st, f"xx{i}", [128, 512]) for i in range(2)]
            xo_ = [sb(st, f"xo_{i}", [128, 512]) for i in range(2)]
            cnt = 0
            for sidx in range(2):
                x_ap = xs if sidx == 0 else xo
                S.dma(g1bc[:], modd[sidx, 0].partition_broadcast(128), [("modd", sidx, 0)], ["g1bc"])
                for b in range(4):
                    tsl = slice(b * 512, (b + 1) * 512)
                    S.dma(aT[:], attnT[sidx][:, :, tsl].rearrange("h p t -> p h t"),
                          [("attnT", sidx, h, b) for h in range(16)], ["aT"])
                    S.dma(lT[:], lruT[sidx][:, :, tsl].rearrange("h p t -> p h t"),
                          [("lruT", sidx, n) for n in range(16)], ["lT"])
                    for f in range(32):
                        i2 = f % 2
                        S.dma(wao_t[i2][:], waob[f], [("waob", f)], [("wao", i2)])
                        S.dma(wlo_t[i2][:], wlob[f], [("wlob", f)], [("wlo", i2)])
                        S.dma(ga_t[i2][:], ga[sidx, f][:, tsl], [("S" if sidx == 0 else "O", "ga", f, b)], [("ga_t", i2)])
                        S.dma(gl_t[i2][:], gl[sidx, f][:, tsl], [("S" if sidx == 0 else "O", "gl", f, b)], [("gl_t", i2)])
                        pB = bank(i2 * 2)
                        pA = bank(i2 * 2 + 1)
                        for kc in range(16):
                            S.mm(pB, wao_t[i2][:, kc, :], aT[:, kc, :], kc == 0, kc == 15, [("wao", i2), "aT"], [("ps", i2 * 2)])
                        for kc in range(16):
                            S.mm(pA, wlo_t[i2][:, kc, :], lT[:, kc, :], kc == 0, kc == 15, [("wlo", i2), "lT"], [("ps", i2 * 2 + 1)])
                        S.tt(f1[:], pB, ga_t[i2][:], ALU.mult, [("ps", i2 * 2), ("ga_t", i2)], ["f1"])
                        S.tt(f2[:], pA, gl_t[i2][:], ALU.mult, [("ps", i2 * 2 + 1), ("gl_t", i2)], ["f2"])
                        S.tt(mg[:, f, :], f1[:], f2[:], ALU.add, ["f1", "f2"], [("mg", f)], eng="pool")
                    mgk = [("mg", f) for f in range(32)]
                    S.dma(wo_t2[0][:], woutb[0], [("woutb", 0)], [("wo_t", 0)])
                    for nch in range(8):
                        if nch + 1 < 8:
                            S.dma(wo_t2[(nch + 1) % 2][:], woutb[nch + 1], [("woutb", nch + 1)], [("wo_t", (nch + 1) % 2)])
                        wo_t = wo_t2[nch % 2]
                        wok = ("wo_t", nch % 2)
                        csl = slice(nch * 512, (nch + 1) * 512)
                        for t4 in range(4):
                            i2 = cnt % 2
                            cnt += 1
                            r0 = b * 512 + t4 * 128
                            S.dma(xx[i2][:], x_ap[r0:r0 + 128, csl], [], [("xx", i2)])
                            po = bank(4 + i2)
                            for kc in range(32):
                                S.mm(po, mg[:, kc, t4 * 128:(t4 + 1) * 128], wo_t[:, kc, :], kc == 0, kc == 31,
                                     mgk + [wok], [("ps", 4 + i2)])
                            S.tt(xo_[i2][:], po, g1bc[:, csl], ALU.mult, [("ps", 4 + i2), "g1bc"], [("xo_", i2)])
                            S.tt(xo_[i2][:], xo_[i2][:], xx[i2][:], ALU.add, [("xo_", i2), ("xx", i2)], [("xo_", i2)], eng="pool")
                            S.dma(x1[sidx * TS + r0: sidx * TS + r0 + 128, csl], xo_[i2][:], [("xo_", i2)],
                                  [("x1", sidx * 16 + b * 4 + t4, nch)])
                    S.flush(barrier=False)
            S.flush()

        if stop == "F":
            return nc
        with contextlib.ExitStack() as st:
            x1t = sb(st, "x1t", [128, D])
            h2 = sb(st, "h2", [128, D])
            accp = sb(st, "accp", [128, D])
            gb = [sb(st, f"gb{i}", [128, D]) for i in range(3)]
            h2b = sb(st, "h2b", [128, D], BF16)
            h2T = sb(st, "h2T", [128, 32, 128], BF16)
            wq_t = [sb(st, f"wq_t{i}", [128, 32, 128], BF16) for i in range(2)]
            qTp = sb(st, "qTp", [128, 16, 128])
            keys_t = sb(st, "keys_t", [128, 16, 128])
            scs = sb(st, "scs", [128, 16, 128])
            wk = sb(st, "wk", [128, 256])
            stop_ = sb(st, "stop_", [128, 16, 16])
            itop = sb(st, "itop", [128, 16, 16], U32)
            itf = sb(st, "itf", [128, 16, 16])
            i0s = sb(st, "i0s", [128, 8, 16])
            cand = sb(st, "cand", [128, 8, 256])
            cidx = sb(st, "cidx", [128, 8, 256])
            bs = sb(st, "bs", [128, 8, 16])
            posu = sb(st, "posu", [128, 8, 16], U32)
            posf = sb(st, "posf", [128, 8, 16])
            eidx = sb(st, "eidx", [128, 128])
            eidi = sb(st, "eidi", [128, 128], I32)
            gsum = sb(st, "gsum", [128, 8])
            gates = sb(st, "gates", [128, 128])
            dots = sb(st, "dots", [128, 128])
            coef = sb(st, "coef", [128, 128])
            iot = sb(st, "iot", [128, 256])
            ssq2 = sb(st, "ssq2", [128, 1])
            rstd2 = sb(st, "rstd2", [128, 1])
            S.dma(keys_t[:], keysT[:, :, :], [], ["keys_t"])
            S.dma(iot[:], iota_in[:, :], [], ["iot"])
            V = nc.vector
            gcount = 0
            gbv = [gb[i // 2][:].bitcast(BF16)[:, (i % 2) * D:(i % 2 + 1) * D] for i in range(6)]
            gbk = [("gb", i // 2, i % 2) for i in range(6)]
            ubk = [("ub16", i) for i in range(64)]
            vbk = [("vb16", i) for i in range(64)]

            def gfull(i):
                return [("gb", i, 0), ("gb", i, 1)]

            def top16(src_ap, n, vals_ap, idx_ap, rk):
                S.op("dve", lambda: V.max(out=vals_ap[:, 0:8], in_=src_ap), rk, ["tv"])
                S.op("dve", lambda: V.max_index(out=idx_ap[:, 0:8], in_max=vals_ap[:, 0:8], in_values=src_ap),
                     rk + ["tv"], ["ti"])
                S.op("dve", lambda: V.match_replace(out=wk[:, 0:n], in_to_replace=vals_ap[:, 0:8], in_values=src_ap,
                                                    imm_value=-1e30), rk + ["tv"], ["wk"])
                S.op("dve", lambda: V.max(out=vals_ap[:, 8:16], in_=wk[:, 0:n]), ["wk"], ["tv"])
                S.op("dve", lambda: V.max_index(out=idx_ap[:, 8:16], in_max=vals_ap[:, 8:16], in_values=wk[:, 0:n]),
                     ["wk", "tv"], ["ti"])

            for ti in range(32):
                sidx = ti // 16
                y_ap = ys if sidx == 0 else yo
                rloc = (ti % 16) * 128
                S.dma(x1t[:], x1[ti * 128:(ti + 1) * 128, :], [("x1", ti, nch) for nch in range(8)], ["x1t"])
                S.dma(gb[0][:], modd[sidx, 1].partition_broadcast(128), [("modd", sidx, 1)], gfull(0))
                S.dma(gb[1][:], modd[sidx, 2].partition_broadcast(128), [("modd", sidx, 2)], gfull(1))
                S.act(gb[2][:].bitcast(BF16)[:, 0:D], x1t[:], AF.Square, ["x1t"], gfull(2) + ["ssq2"], accum_out=ssq2[:])
                S.ts(rstd2[:], ssq2[:], 1.0 / D, EPS, ALU.mult, ALU.add, ["ssq2"], ["rstd2"])
                S.act(rstd2[:], rstd2[:], AF.Sqrt, ["rstd2"], ["rstd2"])
                S.op("dve", lambda: nc.vector.reciprocal(out=rstd2[:], in_=rstd2[:]), ["rstd2"], ["rstd2"])
                S.stt(h2[:], x1t[:], rstd2[:, 0:1], gb[0][:], ALU.mult, ALU.mult, ["x1t", "rstd2"] + gfull(0), ["h2"])
                S.tt(h2[:], h2[:], gb[1][:], ALU.add, ["h2"] + gfull(1), ["h2"])
                S.cp(h2b[:], h2[:], ["h2"], ["h2b"], eng="act")
                for g8 in range(4):
                    pb = g8 % 2
                    for k8 in range(8):
                        kc = g8 * 8 + k8
                        S.tr(psb[:, pb * 1024 + k8 * 128: pb * 1024 + (k8 + 1) * 128],
                             h2b[:, kc * 128:(kc + 1) * 128], identb[:], ["h2b", "identb"], [("psb", pb)])
                    S.cp(h2T[:, g8 * 8:(g8 + 1) * 8, :], psb[:, pb * 1024:(pb + 1) * 1024].rearrange("p (a t) -> p a t", a=8),
                         [("psb", pb)], [("h2T", g8)], eng=("dve" if g8 % 2 == 0 else "act"))
                h2Tk = [("h2T", g8) for g8 in range(4)]
                for f in range(16):
                    i2 = f % 2
                    S.dma(wq_t[i2][:], wqb[f], [("wqb", f)], [("wq_t", i2)])
                    pq = ps[:, i2 * 512:i2 * 512 + 128]
                    for kc in range(32):
                        S.mm(pq, wq_t[i2][:, kc, :], h2T[:, kc, :], kc == 0, kc == 31, [("wq_t", i2)] + h2Tk, [("ps", i2)])
                    S.cp(qTp[:, f, :], pq, [("ps", i2)], [("qTp", f)], eng=("dve" if f % 2 == 0 else "act"))
                for hp in range(16):
                    bk = 2 + hp // 4
                    S.mm(ps[:, bk * 512 + (hp % 4) * 128: bk * 512 + (hp % 4 + 1) * 128], qTp[:, hp, :], keys_t[:, hp, :],
                         True, True, [("qTp", hp), "keys_t"], [("ps", bk)])
                S.cp(scs[:].rearrange("p a n -> p (a n)"), ps[:, 1024:3072], [("ps", 2), ("ps", 3), ("ps", 4), ("ps", 5)], ["scs"])
                for hp in range(16):
                    top16(scs[:, hp, :], 128, stop_[:, hp, :], itop[:, hp, :], ["scs"])
                S.cp(itf[:], itop[:], ["ti"], ["itf"])
                st4 = stop_[:].rearrange("p (h two) k -> p h two k", two=2)
                it4 = itf[:].rearrange("p (h two) k -> p h two k", two=2)
                c4 = cand[:].rearrange("p h (i j) -> p h i j", i=16)
                x4 = cidx[:].rearrange("p h (i j) -> p h i j", i=16)
                S.tt(c4, st4[:, :, 0, :].unsqueeze(3).to_broadcast([128, 8, 16, 16]),
                     st4[:, :, 1, :].unsqueeze(2).to_broadcast([128, 8, 16, 16]), ALU.add, ["tv"], ["cand"])
                S.ts(i0s[:], it4[:, :, 0, :], 128.0, None, ALU.mult, None, ["itf"], ["i0s"])
                S.tt(x4, i0s[:].unsqueeze(3).to_broadcast([128, 8, 16, 16]),
                     it4[:, :, 1, :].unsqueeze(2).to_broadcast([128, 8, 16, 16]), ALU.add, ["i0s", "itf"], ["cidx"])
                oh = gb[2]
                oh3 = oh[:].rearrange("p (k n) -> p k n", k=16)
                for h in range(8):
                    top16(cand[:, h, :], 256, bs[:, h, :], posu[:, h, :], ["cand"])
                    S.cp(posf[:, h, :], posu[:, h, :], ["ti"], [("posf", h)])
                    S.tt(oh3, iot[:].unsqueeze(1).to_broadcast([128, 16, 256]),
                         posf[:, h, :].unsqueeze(2).to_broadcast([128, 16, 256]), ALU.is_equal,
                         ["iot", ("posf", h)], gfull(2))
                    S.tt(oh3, oh3, cidx[:, h, :].unsqueeze(1).to_broadcast([128, 16, 256]), ALU.mult,
                         gfull(2) + ["cidx"], gfull(2))
                    S.op("dve", lambda h=h: V.tensor_reduce(out=eidx[:, h * 16:(h + 1) * 16], in_=oh3, axis=AX.X, op=ALU.add),
                         gfull(2), [("eidx", h)])
                bsk = ["tv"]
                g3 = gates[:].rearrange("p (h k) -> p h k", h=8)
                S.tt(g3, bs[:], bs[:, :, 0:1].to_broadcast([128, 8, 16]), ALU.subtract, bsk, ["gates"])
                S.act(gates[:], gates[:], AF.Exp, ["gates"], ["gates"])
                S.op("dve", lambda: V.tensor_reduce(out=gsum[:], in_=g3, axis=AX.X, op=ALU.add), ["gates"], ["gsum"])
                S.op("dve", lambda: V.reciprocal(out=gsum[:], in_=gsum[:]), ["gsum"], ["gsum"])
                S.tt(g3, g3, gsum[:].unsqueeze(2).to_broadcast([128, 8, 16]), ALU.mult, ["gates", "gsum"], ["gates"])
                eik = [("eidx", h) for h in range(8)]
                S.ts(eidx[:], eidx[:], 0.0, 16383.0, ALU.max, ALU.min, eik, eik)
                S.cp(eidi[:], eidx[:], eik, ["eidi"])
                G = nc.gpsimd
                for e in range(128):
                    bi_ = gcount % 6
                    gcount += 1
                    S.op("pool", lambda bi_=bi_, e=e: G.indirect_dma_start(
                        out=gbv[bi_], out_offset=None, in_=ub16[:, :],
                        in_offset=bass.IndirectOffsetOnAxis(ap=eidi[:, e:e + 1], axis=0)),
                        ["eidi"] + ubk, [gbk[bi_]], dma=True)
                    S.op("dve", lambda bi_=bi_, e=e: V.scalar_tensor_tensor(
                        out=h2b[:], in0=gbv[bi_], scalar=1.0, in1=h2[:], op0=ALU.mult, op1=ALU.mult,
                        accum_out=dots[:, e:e + 1]), [gbk[bi_], "h2"], ["h2b", ("dots", e)])
                dk = [("dots", e) for e in range(128)]
                S.act(coef[:], dots[:], AF.Gelu, dk, ["coef"])
                S.tt(coef[:], coef[:], gates[:], ALU.mult, ["coef", "gates"], ["coef"])
                for e in range(128):
                    bi_ = gcount % 6
                    gcount += 1
                    S.op("pool", lambda bi_=bi_, e=e: G.indirect_dma_start(
                        out=gbv[bi_], out_offset=None, in_=vb16[:, :],
                        in_offset=bass.IndirectOffsetOnAxis(ap=eidi[:, e:e + 1], axis=0)),
                        ["eidi"] + vbk, [gbk[bi_]], dma=True)
                    if e == 0:
                        S.ts(accp[:], gbv[bi_], coef[:, 0:1], None, ALU.mult, None, [gbk[bi_], "coef"], ["accp"])
                    else:
                        S.stt(accp[:], gbv[bi_], coef[:, e:e + 1], accp[:], ALU.mult, ALU.add,
                              [gbk[bi_], "coef", "accp"], ["accp"])
                S.dma(gb[2][:], modd[sidx, 3].partition_broadcast(128), [("modd", sidx, 3)], gfull(2))
                S.tt(accp[:], accp[:], gb[2][:], ALU.mult, ["accp"] + gfull(2), ["accp"])
                S.tt(accp[:], accp[:], x1t[:], ALU.add, ["accp", "x1t"], ["accp"], eng="pool")
                S.dma(y_ap[rloc:rloc + 128, :], accp[:], ["accp"], [("y", ti)])
                S.flush(barrier=False)
            S.flush()
    return nc


def _rope_tables(T):
    pos = np.arange(T)
    rows = (pos // 64).astype(np.float32)
    cols = (pos % 64).astype(np.float32)
    inv = (10000.0 ** (-np.arange(0, 64, 2, dtype=np.float32) / 64.0)).astype(np.float32)
    ar = rows[None, :] * inv[:, None]
    ac = cols[None, :] * inv[:, None]
    C = np.concatenate([np.cos(ar), np.cos(ar), np.cos(ac), np.cos(ac)], axis=0).astype(np.float32)
    Sn = np.concatenate([np.sin(ar), np.sin(ar), np.sin(ac), np.sin(ac)], axis=0).astype(np.float32)
    return np.ascontiguousarray(C), np.ascontiguousarray(Sn)


def _consts():
    ident = np.eye(128, dtype=np.float32)
    R = np.zeros((128, 128), np.float32)
    for base in (0, 64):
        for i in range(32):
            R[base + i, base + 32 + i] = -1.0
            R[base + 32 + i, base + i] = 1.0
    rotT = np.ascontiguousarray(R.T)
    ones = np.ones((128, 128), np.float32)
    return np.ascontiguousarray(np.stack([ident, rotT, ones], axis=1))


def make_in_maps(inp):
    f = lambda a: np.ascontiguousarray(np.asarray(a, dtype=np.float32))
    C, Sn = _rope_tables(TC)
    cst = _consts()
    iota = np.ascontiguousarray(np.tile(np.arange(256, dtype=np.float32)[None, :], (128, 1)))
    fm = lambda v, nch: np.ascontiguousarray(f(v).reshape(nch, 128).T)
    shared = {
        "w_ada": f(inp["w_ada"][0]), "badaT": fm(inp["b_ada"][0], 192),
        "g1T": fm(inp["g_norm1"][0], 32), "g2T": fm(inp["g_norm2"][0], 32),
        "w_in": f(inp["w_in"][0]),
        "qkg": np.ascontiguousarray(np.stack([f(inp["q_gain"][0]), f(inp["k_gain"][0])], axis=1)),
        "convwT": np.ascontiguousarray(f(inp["conv_w"][0]).reshape(4, 16, 128).transpose(2, 1, 0)),
        "convbT": fm(inp["conv_b"][0], 16),
        "lamT": np.ascontiguousarray(f(inp["lru_lam"][0]).reshape(32, 128).T),
        "baT": np.ascontiguousarray(f(inp["lru_ba"][0]).reshape(32, 128).T),
        "biT": np.ascontiguousarray(f(inp["lru_bi"][0]).reshape(32, 128).T),
        "lru_wa": f(inp["lru_wa"][0]).reshape(32, 128, 128),
        "lru_wi": f(inp["lru_wi"][0]).reshape(32, 128, 128),
        "w_attn_o": f(inp["w_attn_o"][0]), "w_lru_o": f(inp["w_lru_o"][0]), "w_out": f(inp["w_out"][0]),
        "peer_wq": f(inp["peer_wq"][0]),
        "keysT": np.ascontiguousarray(f(inp["peer_keys"][0]).reshape(16, 128, 128).transpose(2, 0, 1)),
        "peer_u": f(inp["peer_u"][0]), "peer_v": f(inp["peer_v"][0]),
        "ropeC": C, "ropeS": Sn, "consts": cst, "iota256": iota,
    }
    xp = f(inp["x_prompt"])
    xsm = f(inp["x_sample"])
    cp = f(inp["c_prompt"])
    csm = f(inp["c_sample"])
    maps = []
    for c in range(8):
        b, j = c // 4, c % 4
        cc = np.stack([csm[c], cp[b]], axis=0)
        cTl = np.ascontiguousarray(cc.reshape(2, 32, 128).transpose(2, 1, 0))
        ms = np.zeros((128, 4), np.float32)
        ms[:, j] = 1.0
        m = dict(shared)
        m.update({
            "xs": xsm[c], "xo": np.ascontiguousarray(xp[b, j * TS:(j + 1) * TS]), "xc": xp[b],
            "cT": cTl, "msel": ms,
            "ropeCo": np.ascontiguousarray(C[:, j * TS:(j + 1) * TS]),
            "ropeSo": np.ascontiguousarray(Sn[:, j * TS:(j + 1) * TS]),
        })
        maps.append(m)
    return maps


def kernel(**inputs):
    nc = build()
    maps = make_in_maps(inputs)
    res = run_bass_kernel_spmd(nc, maps, core_ids=list(range(8)))
    y_prompt = np.zeros((2, 8192, D), np.float32)
    y_sample = np.zeros((8, TS, D), np.float32)
    for c in range(8):
        b, j = c // 4, c % 4
        y_sample[c] = res.results[c]["ys"]
        y_prompt[b, j * TS:(j + 1) * TS] = res.results[c]["yo"]
    return (y_prompt, y_sample)
```

```python
import contextlib
import numpy as np
import concourse.bass as bass
import concourse.mybir as mybir
from concourse.bass_utils import run_bass_kernel_spmd

F32 = mybir.dt.float32
BF16 = mybir.dt.bfloat16
U32 = mybir.dt.uint32
I32 = mybir.dt.int32
AF = mybir.ActivationFunctionType
ALU = mybir.AluOpType
AX = mybir.AxisListType

D = 4096
TS = 2048
TC = 8192
NCOL = 15360
EPS = 1e-6
SEM_LIMIT = 30000
DBG = {}


class Op:
    __slots__ = ("eng", "fn", "deps", "dma", "signal", "sem", "val")

    def __init__(self, eng, fn, dma):
        self.eng = eng
        self.fn = fn
        self.dma = dma
        self.deps = ()
        self.signal = False
        self.sem = None
        self.val = 0


class Sched:
    def __init__(self, nc, es):
        self.nc = nc
        self.es = es
        self.engs = {"pe": nc.tensor, "act": nc.scalar, "dve": nc.vector, "pool": nc.gpsimd, "sp": nc.sync}
        self.ops = []
        self.last_w = {}
        self.readers = {}
        self.nsem = 0
        self.cur_sem = {}
        self.cnt = {}
        for e in ("pe", "act", "dve", "pool"):
            self.cur_sem[e] = self._new_sem()
            self.cnt[e] = 0
        self.slots = {"sp": [[self._new_sem(), 0] for _ in range(24)],
                      "pool": [[self._new_sem(), 0] for _ in range(16)]}
        self.slot_i = {"sp": 0, "pool": 0}
        self.seen = {e: {} for e in self.engs}
        self.last_op = {}
        self.trace = {e: [] for e in self.engs} if DBG.get("trace") else None

    def _new_sem(self):
        self.nsem += 1
        return self.es.enter_context(self.nc.semaphore(f"sm{self.nsem}"))

    def op(self, eng, fn, r=(), w=(), dma=False):
        w = list(w) + [k for k in r if isinstance(k, tuple) and k[0] in ("ps", "psb")]
        o = Op(eng, fn, dma)
        deps = {}
        for k in r:
            lw = self.last_w.get(k)
            if lw is not None:
                deps[id(lw)] = lw
        for k in w:
            lw = self.last_w.get(k)
            if lw is not None:
                deps[id(lw)] = lw
            rd = self.readers.get(k)
            if rd:
                for x in rd.values():
                    deps[id(x)] = x
        rk = ("d", id(o)) if dma else eng
        for k in r:
            self.readers.setdefault(k, {})[rk] = o
        for k in w:
            self.last_w[k] = o
            self.readers[k] = {}
        dl = []
        for d in deps.values():
            if eng == "pe" and not dma and d.eng == "pe" and not d.dma:
                continue
            d.signal = True
            dl.append(d)
        o.deps = dl
        self.ops.append(o)
        return o

    def _wait(self, E, sem, val):
        sn = self.seen[E]
        if sn.get(id(sem), 0) < val:
            self.engs[E].wait_ge(sem, val)
            sn[id(sem)] = val
            if self.trace is not None:
                self.trace[E].append(("w", id(sem), val))

    def flush(self, barrier=True):
        for lw in self.last_w.values():
            lw.signal = True
        for rd in self.readers.values():
            for x in rd.values():
                x.signal = True
        lastc = {}
        for o in self.ops:
            if not o.dma:
                lastc[o.eng] = o
        for o in lastc.values():
            o.signal = True
        for o in self.ops:
            E = o.eng
            eng = self.engs[E]
            for d in o.deps:
                self._wait(E, d.sem, d.val)
            if o.dma:
                sl = self.slots[E]
                i = self.slot_i[E]
                self.slot_i[E] = (i + 1) % len(sl)
                sem, uses = sl[i]
                if uses > 0:
                    self._wait(E, sem, 16 * uses)
                if 16 * (uses + 1) > SEM_LIMIT:
                    sem = self._new_sem()
                    uses = 0
                    sl[i][0] = sem
                ins = o.fn()
                ins.then_inc(sem, 16)
                if self.trace is not None:
                    self.trace[E].append(("s", id(sem), 16))
                uses += 1
                sl[i][1] = uses
                o.sem = sem
                o.val = 16 * uses
            else:
                ins = o.fn()
                if o.signal:
                    if self.cnt[E] >= SEM_LIMIT:
                        self.cur_sem[E] = self._new_sem()
                        self.cnt[E] = 0
                    self.cnt[E] += 1
                    ins.then_inc(self.cur_sem[E], 1)
                    if self.trace is not None:
                        self.trace[E].append(("s", id(self.cur_sem[E]), 1))
                    o.sem = self.cur_sem[E]
                    o.val = self.cnt[E]
            o.fn = None
        self.ops = []
        if barrier:
            for E in self.engs:
                for E2 in ("pe", "act", "dve", "pool"):
                    if E2 != E and self.cnt[E2] > 0:
                        self._wait(E, self.cur_sem[E2], self.cnt[E2])
                for q in ("sp", "pool"):
                    for sem, uses in self.slots[q]:
                        if uses > 0:
                            self._wait(E, sem, 16 * uses)

    def dma(self, out, in_, r, w, q="sp"):
        eng = self.engs[q]
        return self.op(q, lambda: eng.dma_start(out=out, in_=in_), r, w, dma=True)

    def mm(self, out, lhsT, rhs, start, stop, r, w):
        t = self.nc.tensor
        return self.op("pe", lambda: t.matmul(out, lhsT=lhsT, rhs=rhs, start=start, stop=stop), r, w)

    def tr(self, out, in_, ident, r, w):
        t = self.nc.tensor
        return self.op("pe", lambda: t.transpose(out=out, in_=in_, identity=ident), r, w)

    def act(self, out, in_, func, r, w, bias=None, scale=None, accum_out=None):
        a = self.nc.scalar
        kw = {}
        if bias is not None:
            kw["bias"] = bias
        if scale is not None:
            kw["scale"] = scale
        if accum_out is not None:
            kw["accum_out"] = accum_out
        return self.op("act", lambda: a.activation(out=out, in_=in_, func=func, **kw), r, w)

    def ts(self, out, in0, s1, s2, op0, op1, r, w, eng="dve"):
        e = self.engs[eng]
        if op1 is None:
            return self.op(eng, lambda: e.tensor_scalar(out=out, in0=in0, scalar1=s1, scalar2=None, op0=op0), r, w)
        return self.op(eng, lambda: e.tensor_scalar(out=out, in0=in0, scalar1=s1, scalar2=s2, op0=op0, op1=op1), r, w)

    def tt(self, out, in0, in1, op, r, w, eng="dve"):
        e = self.engs[eng]
        return self.op(eng, lambda: e.tensor_tensor(out=out, in0=in0, in1=in1, op=op), r, w)

    def stt(self, out, in0, scalar, in1, op0, op1, r, w):
        e = self.nc.vector
        return self.op("dve", lambda: e.scalar_tensor_tensor(out=out, in0=in0, scalar=scalar, in1=in1, op0=op0, op1=op1), r, w)

    def cp(self, out, in_, r, w, eng="dve"):
        e = self.engs[eng]
        if eng == "act":
            return self.op(eng, lambda: e.copy(out=out, in_=in_), r, w)
        return self.op(eng, lambda: e.tensor_copy(out=out, in_=in_), r, w)


def build(debug=False, dbg_names=(), stop=None):
    nc = bass.Bass("TRN2", target_bir_lowering=False)

    def din(name, shape, dt=F32):
        return nc.dram_tensor(name, list(shape), dt, kind="ExternalInput").ap()

    def dscr(name, shape, dt=F32):
        kind = "ExternalOutput" if name in dbg_names else "Internal"
        return nc.dram_tensor(name, list(shape), dt, kind=kind).ap()

    xs = din("xs", [TS, D])
    xo = din("xo", [TS, D])
    xc = din("xc", [TC, D])
    cT = din("cT", [128, 32, 2])
    w_ada = din("w_ada", [D, 6 * D])
    badaT = din("badaT", [128, 192])
    g1T = din("g1T", [128, 32])
    g2T = din("g2T", [128, 32])
    w_in = din("w_in", [D, NCOL])
    qkg = din("qkg", [128, 2])
    convwT = din("convwT", [128, 16, 4])
    convbT = din("convbT", [128, 16])
    lamT = din("lamT", [128, 32])
    baT = din("baT", [128, 32])
    biT = din("biT", [128, 32])
    lru_wa = din("lru_wa", [32, 128, 128])
    lru_wi = din("lru_wi", [32, 128, 128])
    w_attn_o = din("w_attn_o", [2048, D])
    w_lru_o = din("w_lru_o", [2048, D])
    w_out = din("w_out", [D, D])
    peer_wq = din("peer_wq", [D, 2048])
    keysT = din("keysT", [128, 16, 128])
    peer_u = din("peer_u", [16384, D])
    peer_v = din("peer_v", [16384, D])
    ropeC = din("ropeC", [128, TC])
    ropeS = din("ropeS", [128, TC])
    ropeCo = din("ropeCo", [128, TS])
    ropeSo = din("ropeSo", [128, TS])
    consts = din("consts", [128, 3, 128])
    iota_in = din("iota256", [128, 256])
    msel = din("msel", [128, 4])

    ys = nc.dram_tensor("ys", [TS, D], F32, kind="ExternalOutput").ap()
    yo = nc.dram_tensor("yo", [TS, D], F32, kind="ExternalOutput").ap()

    winb = dscr("winb", [120, 128, 32, 128], BF16)
    waob = dscr("waob", [32, 128, 16, 128], BF16)
    wlob = dscr("wlob", [32, 128, 16, 128], BF16)
    woutb = dscr("woutb", [8, 128, 32, 512], BF16)
    wqb = dscr("wqb", [16, 128, 32, 128], BF16)
    modd = dscr("modd", [2, 4, D])
    QT = dscr("QT", [2, 16, 128, TS], BF16)
    KT_s = dscr("KT_s", [4, 128, TS], BF16)
    KT_c = dscr("KT_c", [4, 128, TC], BF16)
    V_s = dscr("V_s", [TS, 512], BF16)
    V_c = dscr("V_c", [TC, 512], BF16)
    xl_s = dscr("xl_s", [16, 128, TS])
    xl_c = dscr("xl_c", [16, 128, TC])
    gy = dscr("gy", [2, 16, 128, TS])
    ga = dscr("ga", [2, 32, 128, TS])
    gl = dscr("gl", [2, 32, 128, TS])
    attnT = dscr("attnT", [2, 16, 128, TS], BF16)
    lruT = dscr("lruT", [2, 16, 128, TS], BF16)
    x1 = dscr("x1", [2 * TS, D])
    ub16 = dscr("ub16", [16384, D], BF16)
    vb16 = dscr("vb16", [16384, D], BF16)
    dbg = {}
    if debug:
        dbg["modT"] = nc.dram_tensor("dbg_modT", [128, 192, 2], F32, kind="ExternalOutput").ap()

    es = contextlib.ExitStack()
    with es:
        S = Sched(nc, es)
        nc._sched = S

        nm = [0]

        def sb(st, name, shape, dt=F32):
            nm[0] += 1
            return st.enter_context(nc.sbuf_tensor(f"{name}_{nm[0]}", list(shape), dt))

        ps = es.enter_context(nc.psum_tensor("ps", [128, 3072], F32))
        psb = es.enter_context(nc.psum_tensor("psb", [128, 2048], BF16))
        cst = sb(es, "cst", [128, 3, 128])
        identb = sb(es, "identb", [128, 128], BF16)
        onesb = sb(es, "onesb", [128, 128], BF16)
        modT = sb(es, "modT", [128, 192, 2])
        G1 = sb(es, "G1", [128, 32, 2])
        G2 = sb(es, "G2", [128, 32, 2])
        qkg_t = sb(es, "qkg_t", [128, 2])
        msel_t = sb(es, "msel_t", [128, 4])
        identf = cst[:, 0, :]
        rotT = cst[:, 1, :]
        onesf = cst[:, 2, :]

        def bank(i):
            return ps[:, i * 512:(i + 1) * 512]

        S.dma(cst[:], consts[:, :, :], [], ["cst"])
        S.dma(qkg_t[:], qkg[:, :], [], ["qkg"])
        S.dma(msel_t[:], msel[:, :], [], ["msel"])
        S.cp(identb[:], cst[:, 0, :], ["cst"], ["identb"])
        S.cp(onesb[:], cst[:, 2, :], ["cst"], ["onesb"])

        def cast_w(dst, src, nf, kc, cw):
            for f in range(nf):
                S.dma(dst[f], src[:, f * cw:(f + 1) * cw].rearrange("(kc p) c -> p kc c", p=128),
                      [], [(dst.tensor.name, f)], q="pool")
        cast_w(winb, w_in, 120, 32, 128)
        cast_w(waob, w_attn_o, 32, 16, 128)
        cast_w(wlob, w_lru_o, 32, 16, 128)
        cast_w(woutb, w_out, 8, 32, 512)
        cast_w(wqb, peer_wq, 16, 32, 128)

        with contextlib.ExitStack() as st:
            cT_t = sb(st, "cT_t", [128, 32, 2])
            scT = sb(st, "scT", [128, 32, 2])
            bada_t = sb(st, "bada_t", [128, 192])
            g1_t = sb(st, "g1_t", [128, 32])
            g2_t = sb(st, "g2_t", [128, 32])
            wa = [sb(st, f"wa{i}", [128, 32, 256]) for i in range(2)]
            tmpc = sb(st, "tmpc", [128, 32])
            tmpr = sb(st, "tmpr", [32, 128])
            S.dma(cT_t[:], cT[:, :, :], [], ["cT"])
            S.dma(bada_t[:], badaT[:, :], [], ["bada"])
            S.dma(g1_t[:], g1T[:, :], [], ["g1"])
            S.dma(g2_t[:], g2T[:, :], [], ["g2"])
            S.act(scT[:], cT_t[:], AF.Silu, ["cT"], ["scT"])
            for j2 in range(96):
                buf = wa[j2 % 2]
                S.dma(buf[:], w_ada[:, j2 * 256:(j2 + 1) * 256].rearrange("(kc p) c -> p kc c", p=128),
                      [], [("wa", j2 % 2)])
                for half in range(2):
                    j = j2 * 2 + half
                    b = j % 6
                    pst = ps[:, b * 512:b * 512 + 2]
                    for kc in range(32):
                        S.mm(pst, buf[:, kc, half * 128:(half + 1) * 128], scT[:, kc, :], kc == 0, kc == 31,
                             [("wa", j2 % 2), "scT"], [("ps", b)])
                    S.ts(modT[:, j, :], pst, bada_t[:, j:j + 1], None, ALU.add, None,
                         [("ps", b), "bada"], [("modT", j)])
            allmod = [("modT", j) for j in range(192)]
            S.ts(G1[:], modT[:, 32:64, :], 1.0, None, ALU.add, None, allmod, ["G1"])
            S.tt(G1[:], G1[:], g1_t[:].unsqueeze(2).to_broadcast([128, 32, 2]), ALU.mult, ["G1", "g1"], ["G1"])
            S.ts(G2[:], modT[:, 128:160, :], 1.0, None, ALU.add, None, allmod, ["G2"])
            S.tt(G2[:], G2[:], g2_t[:].unsqueeze(2).to_broadcast([128, 32, 2]), ALU.mult, ["G2", "g2"], ["G2"])
            srcs = [(modT, 64), (G2, 0), (modT, 96), (modT, 160)]
            for m in range(2):
                for sl, (srct, off) in enumerate(srcs):
                    S.cp(tmpc[:], srct[:, off:off + 32, m], allmod + ["G2"], ["tmpc"])
                    S.tr(ps[0:32, 0:128], tmpc[:], identf, ["tmpc", "cst"], [("ps", 0)])
                    S.cp(tmpr[:], ps[0:32, 0:128], [("ps", 0)], ["tmpr"])
                    S.dma(modd[m, sl].rearrange("(kc p) -> kc p", p=128), tmpr[:], ["tmpr"], [("modd", m, sl)])
            if debug:
                S.dma(dbg["modT"][:, :, :], modT[:], allmod, ["dbg_modT"])
            S.flush()

        def phase_c(tag, x_ap, T, m, cols, rc, rs, oq, ok, ov, oxl, sidx):
            with contextlib.ExitStack() as st:
                NXB = DBG.get("nxb", 2)
                xt = [sb(st, f"xt{i}", [128, D]) for i in range(NXB)]
                NXN = DBG.get("nxn", NXB)
                xn = [sb(st, f"xn{i}", [128, D], BF16) for i in range(NXN)]
                hT = [sb(st, f"hT{i}", [128, 32, 512], BF16) for i in range(DBG.get("nhT", 2))]
                wt = [sb(st, f"wt{i}", [128, 32, 128], BF16) for i in range(3)]
                ssq = sb(st, "ssq", [128, 2])
                rstd = sb(st, "rstd", [128, 2])
                sqjunk = sb(st, "sqjunk", [128, D], BF16)
                ct = [sb(st, f"ct{i}", [128, 512]) for i in range(2)]
                stt_ = [sb(st, f"st{i}", [128, 512]) for i in range(2)]
                sqf2 = [sb(st, f"sqf{i}", [128, 512]) for i in range(2)]
                rsf2 = [sb(st, f"rsf{i}", [128, 512]) for i in range(2)]
                qn2 = [sb(st, f"qn{i}", [128, 512]) for i in range(2)]
                t12 = [sb(st, f"t1{i}", [128, 512]) for i in range(2)]
                t22 = [sb(st, f"t2{i}", [128, 512]) for i in range(2)]
                ecount = 0
                ob = [sb(st, f"ob{i}", [128, 512], BF16) for i in range(2)]
                of = [sb(st, f"of{i}", [128, 512]) for i in range(2)]
                vT = sb(st, "vT", [128, 512], BF16)
                vtile = [sb(st, f"vtile{i}", [128, 4, 128], BF16) for i in range(2)]
                nblk = DBG.get("nblk", T // 512)
                if "kinds" in DBG:
                    cols = [c_ for c_ in cols if c_[1] in DBG["kinds"]][:DBG.get("ncols", 1000)]
                has_rope = any(kind in ("q", "k") for (_, kind, _) in cols)
                wcount = 0
                pcount = 0
                ocount = 0
                for b in range(nblk):
                    hb = hT[b % len(hT)]
                    hk = ("hT", b % len(hT))
                    for tt_ in range(DBG.get("ntile", 4)):
                        ti = b * 4 + tt_
                        xb = xt[ti % NXB]
                        xk = ("xt", ti % NXB)
                        nb_ = xn[ti % NXN]
                        nk = ("xn", ti % NXN)
                        S.dma(xb[:], x_ap[ti * 128:(ti + 1) * 128, :], [], [xk])
                        S.act(sqjunk[:], xb[:], AF.Square, [xk], ["sqjunk", ("ssq", ti % 2)], accum_out=ssq[:, ti % 2:ti % 2 + 1])
                        S.ts(rstd[:, ti % 2:ti % 2 + 1], ssq[:, ti % 2:ti % 2 + 1], 1.0 / D, EPS, ALU.mult, ALU.add,
                             [("ssq", ti % 2)], [("rstd", ti % 2)])
                        S.act(rstd[:, ti % 2:ti % 2 + 1], rstd[:, ti % 2:ti % 2 + 1], AF.Sqrt,
                              [("rstd", ti % 2)], [("rstd", ti % 2)])
                        S.op("dve", lambda ti=ti: nc.vector.reciprocal(out=rstd[:, ti % 2:ti % 2 + 1], in_=rstd[:, ti % 2:ti % 2 + 1]),
                             [("rstd", ti % 2)], [("rstd", ti % 2)])
                        S.act(nb_[:], xb[:], AF.Identity, [xk, ("rstd", ti % 2)], [nk], scale=rstd[:, ti % 2:ti % 2 + 1])
                        fes = DBG.get("fe", 9)
                        if tt_ >= DBG.get("fe_tiles", 4):
                            fes = 1
                        for g8 in range(DBG.get("ng8", 4) if fes >= 2 else 0):
                            pb = g8 % 2
                            for k8 in range(8):
                                kc = g8 * 8 + k8
                                S.tr(psb[:, pb * 1024 + k8 * 128: pb * 1024 + (k8 + 1) * 128],
                                     nb_[:, kc * 128:(kc + 1) * 128], identb[:], [nk, "identb"], [("psb", pb)])
                            for k8 in range(8 if fes >= 3 else 0):
                                kc = g8 * 8 + k8
                                src = psb[:, pb * 1024 + k8 * 128: pb * 1024 + (k8 + 1) * 128]
                                dst = hb[:, kc, tt_ * 128:(tt_ + 1) * 128]
                                if (pb == 0 and fes != 4) or fes == 3:
                                    S.ts(dst, src, G1[:, kc, m:m + 1], modT[:, kc, m:m + 1], ALU.mult, ALU.add,
                                         [("psb", pb), "G1"], [(hk, kc, tt_)])
                                else:
                                    S.act(dst, src, AF.Identity, [("psb", pb), "G1"], [(hk, kc, tt_)],
                                          bias=modT[:, kc, m:m + 1], scale=G1[:, kc, m:m + 1])
                    hkeys = [(hk, kc, t4) for kc in range(32) for t4 in range(4)]
                    if has_rope:
                        cb = ct[b % 2]
                        sbb = stt_[b % 2]
                        S.dma(cb[:], rc[:, b * 512:(b + 1) * 512], [], [("ct", b % 2)])
                        S.dma(sbb[:], rs[:, b * 512:(b + 1) * 512], [], [("st", b % 2)])
                    tsl = slice(b * 512, (b + 1) * 512)
                    ncols_ = len(cols)
                    wbase = wcount

                    def issue_load(ci, wbase=wbase):
                        wi_ = (wbase + ci) % 3
                        S.dma(wt[wi_][:], winb[cols[ci][0]], [("winb", cols[ci][0])], [("wt", wi_)])
                    for ci in range(min(2, ncols_)):
                        issue_load(ci)
                    for ci, (f, kind, idx) in enumerate(cols):
                        if ci + 2 < ncols_:
                            issue_load(ci + 2)
                        wb = wt[wcount % 3]
                        wk = ("wt", wcount % 3)
                        wcount += 1
                        pbk = pcount % 4
                        pcount += 1
                        pq = bank(pbk)
                        pk = ("ps", pbk)
                        for kc in range(32):
                            S.mm(pq, wb[:, kc, :], hb[:, kc, :], kc == 0, kc == 31,
                                 [wk] + [(hk, kc, t4) for t4 in range(4)], [pk])
                        if kind in ("q", "k"):
                            ec = ecount % 2
                            ecount += 1
                            sqf, rsf, qn, t1, t2 = sqf2[ec], rsf2[ec], qn2[ec], t12[ec], t22[ec]
                            ksq, krs, kqn, kt1, kt2 = ("sqf", ec), ("rsf", ec), ("qn", ec), ("t1", ec), ("t2", ec)
                            S.act(sqf[:], pq, AF.Square, [pk], [ksq])
                            S.mm(bank(4), onesf, sqf[:], True, True, [ksq, "cst"], [("ps", 4)])
                            S.ts(rsf[:], bank(4), 1.0 / 128, EPS, ALU.mult, ALU.add, [("ps", 4)], [krs])
                            S.act(rsf[:], rsf[:], AF.Sqrt, [krs], [krs])
                            S.op("dve", lambda rsf=rsf: nc.vector.reciprocal(out=rsf[:], in_=rsf[:]), [krs], [krs])
                            gcol = 0 if kind == "q" else 1
                            S.stt(qn[:], pq, qkg_t[:, gcol:gcol + 1], rsf[:], ALU.mult, ALU.mult,
                                  [pk, krs, "qkg"], [kqn])
                            S.mm(bank(5), rotT, qn[:], True, True, [kqn, "cst"], [("ps", 5)])
                            S.tt(t1[:], qn[:], cb[:], ALU.mult, [kqn, ("ct", b % 2)], [kt1])
                            S.tt(t2[:], bank(5), sbb[:], ALU.mult, [("ps", 5), ("st", b % 2)], [kt2])
                            o = ob[ocount % 2]
                            okk = ("ob", ocount % 2)
                            ocount += 1
                            S.tt(o[:], t1[:], t2[:], ALU.add, [kt1, kt2], [okk])
                            dst = oq[idx][:, tsl] if kind == "q" else ok[idx][:, tsl]
                            S.dma(dst, o[:], [okk], [(tag, kind, idx, b)])
                        elif kind == "v":
                            S.cp(vT[:], pq, [pk], ["vT"], eng="act")
                            vt_ = vtile[idx % 2]
                            for t4 in range(4):
                                S.tr(psb[:, t4 * 128:(t4 + 1) * 128], vT[:, t4 * 128:(t4 + 1) * 128], identb[:],
                                     ["vT", "identb"], [("psb", 0)])
                            S.cp(vt_[:], psb[:, 0:512].rearrange("p (a d) -> p a d", a=4),
                                 [("psb", 0)], [("vtile", idx % 2)])
                            S.dma(ov[b * 512:(b + 1) * 512, idx * 128:(idx + 1) * 128].rearrange("(a p) d -> p a d", p=128),
                                  vt_[:], [("vtile", idx % 2)], [(tag, "v", idx, b)])
                        else:
                            o = of[ocount % 2]
                            okk = ("of", ocount % 2)
                            ocount += 1
                            if kind == "xl":
                                S.cp(o[:], pq, [pk], [okk], eng="act")
                                dst = oxl[idx][:, tsl]
                            elif kind == "yl":
                                S.act(o[:], pq, AF.Gelu, [pk], [okk])
                                dst = gy[sidx, idx][:, tsl]
                            elif kind == "ga":
                                S.act(o[:], pq, AF.Sigmoid, [pk], [okk])
                                dst = ga[sidx, idx][:, tsl]
                            else:
                                S.act(o[:], pq, AF.Sigmoid, [pk], [okk])
                                dst = gl[sidx, idx][:, tsl]
                            S.dma(dst, o[:], [okk], [(tag, kind, idx, b)])
                    S.flush(barrier=False)
                S.flush()

        cols_q = [(h, "q", h) for h in range(16)]
        cols_k = [(16 + g, "k", g) for g in range(4)]
        cols_v = [(20 + g, "v", g) for g in range(4)]
        cols_xl = [(24 + n, "xl", n) for n in range(16)]
        cols_yl = [(40 + n, "yl", n) for n in range(16)]
        cols_ga = [(56 + n, "ga", n) for n in range(32)]
        cols_gl = [(88 + n, "gl", n) for n in range(32)]

        if stop == "A":
            return nc
        for i in range(64):
            S.dma(ub16[i * 256:(i + 1) * 256, :], peer_u[i * 256:(i + 1) * 256, :], [], [("ub16", i)], q="pool")
        for i in range(64):
            S.dma(vb16[i * 256:(i + 1) * 256, :], peer_v[i * 256:(i + 1) * 256, :], [], [("vb16", i)], q="pool")
        phase_c("S", xs, TS, 0, cols_q + cols_k + cols_v + cols_xl + cols_yl + cols_ga + cols_gl,
                ropeC, ropeS, QT[0], KT_s, V_s, xl_s, 0)
        if stop == "CS":
            return nc
        phase_c("O", xo, TS, 1, cols_q + cols_yl + cols_ga + cols_gl, ropeCo, ropeSo, QT[1], None, None, None, 1)
        phase_c("C", xc, TC, 1, cols_k + cols_v + cols_xl, ropeC, ropeS, None, KT_c, V_c, xl_c, 1)

        def phase_d(tag, xl_ap, T, sidx, use_sel, tag_xl, gy_tag):
            nq = T // TS
            with contextlib.ExitStack() as st:
                cw = sb(st, "cw", [128, 16, 4])
                cbt = sb(st, "cbt", [128, 16])
                lam = sb(st, "lam", [128, 32])
                cA = sb(st, "cA", [128, 32])
                cA2 = sb(st, "cA2", [128, 32])
                ba_t = sb(st, "ba_t", [128, 32])
                bi_t = sb(st, "bi_t", [128, 32])
                wa_t = sb(st, "wa_t", [128, 32, 128])
                wi_t = sb(st, "wi_t", [128, 32, 128])
                xpad_2 = [sb(st, f"xpad{i}", [128, TS + 3]) for i in range(2)]
                xcv_2 = [sb(st, f"xcv{i}", [128, TS]) for i in range(2)]
                rg_2 = [sb(st, f"rg{i}", [128, TS]) for i in range(2)]
                ig_2 = [sb(st, f"ig{i}", [128, TS]) for i in range(2)]
                a_t_2 = [sb(st, f"a_t{i}", [128, TS]) for i in range(2)]
                mu_2 = [sb(st, f"mu{i}", [128, TS]) for i in range(2)]
                hh_2 = [sb(st, f"hh{i}", [128, TS]) for i in range(2)]
                ucnt = 0
                acc = sb(st, "acc", [128, TS])
                gyt = sb(st, "gyt", [128, TS])
                lo = sb(st, "lo", [128, TS], BF16)
                stt = sb(st, "stt", [128, 2])
                S.dma(cw[:], convwT[:, :, :], [], ["cw"])
                S.dma(cbt[:], convbT[:, :], [], ["cbt"])
                S.dma(lam[:], lamT[:, :], [], ["lam"])
                S.dma(ba_t[:], baT[:, :], [], ["ba"])
                S.dma(bi_t[:], biT[:, :], [], ["bi"])
                S.dma(wa_t[:], lru_wa.rearrange("n c d -> c n d"), [], ["wa_t"])
                S.dma(wi_t[:], lru_wi.rearrange("n c d -> c n d"), [], ["wi_t"])
                S.act(cA[:], lam[:], AF.Exp, ["lam"], ["cA"], scale=-1.0)
                S.act(cA[:], cA[:], AF.Ln, ["cA"], ["cA"], bias=1.0)
                S.ts(cA[:], cA[:], -8.0, None, ALU.mult, None, ["cA"], ["cA"])
                S.ts(cA2[:], cA[:], 2.0, None, ALU.mult, None, ["cA"], ["cA2"])
                for n in range(16):
                    first = True
                    for dr in range(2):
                        gi = dr * 16 + n
                        S.op("pool", lambda dr=dr: nc.gpsimd.memset(stt[:, dr:dr + 1], 0.0), [], [("stt", dr)])
                        qs = range(nq) if dr == 0 else range(nq - 1, -1, -1)
                        for q in qs:
                            up = ucnt % 2
                            ucnt += 1
                            xpad, xcv, rg, ig, a_t, mu, hh = xpad_2[up], xcv_2[up], rg_2[up], ig_2[up], a_t_2[up], mu_2[up], hh_2[up]
                            lo_t = q * TS - 2
                            hi_t = q * TS + TS + 1
                            a0 = max(lo_t, 0)
                            a1 = min(hi_t, T)
                            if lo_t < 0:
                                S.op("pool", lambda xpad=xpad: nc.gpsimd.memset(xpad[:, 0:2], 0.0), [], [("xpad", up)])
                            if hi_t > T:
                                S.op("pool", lambda xpad=xpad: nc.gpsimd.memset(xpad[:, TS + 2:TS + 3], 0.0), [], [("xpad", up)])
                            S.dma(xpad[:, a0 - lo_t:a1 - lo_t], xl_ap[n][:, a0:a1],
                                  [(tag_xl, "xl", n, bb) for bb in range(T // 512)], [("xpad", up)])
                            S.ts(xcv[:], xpad[:, 0:TS], cw[:, n, 0:1], cbt[:, n:n + 1], ALU.mult, ALU.add,
                                 [("xpad", up), "cw", "cbt"], [("xcv", up)])
                            for j in range(1, 4):
                                S.stt(xcv[:], xpad[:, j:j + TS], cw[:, n, j:j + 1], xcv[:], ALU.mult, ALU.add,
                                      [("xpad", up), "cw", ("xcv", up)], [("xcv", up)])
                            for (wt_, bt_, gt_, gk, wkey, bkey) in ((wa_t, ba_t, rg, ("rg", up), "wa_t", "ba"), (wi_t, bi_t, ig, ("ig", up), "wi_t", "bi")):
                                for half in range(2):
                                    pb0 = 0 if gk == ("rg", up) else 2
                                    for blk in range(2):
                                        c0 = half * 1024 + blk * 512
                                        S.mm(bank(pb0 + blk), wt_[:, gi, :], xcv[:, c0:c0 + 512], True, True,
                                             [("xcv", up), wkey], [("ps", pb0 + blk)])
                                    S.act(gt_[:, half * 1024:(half + 1) * 1024], ps[:, pb0 * 512:(pb0 + 2) * 512],
                                          AF.Sigmoid, [("ps", pb0), ("ps", pb0 + 1), bkey], [gk],
                                          bias=bt_[:, gi:gi + 1])
                            S.act(a_t[:], rg[:], AF.Exp, [("rg", up), "cA"], [("a_t", up)], scale=cA[:, gi:gi + 1])
                            S.act(mu[:], rg[:], AF.Exp, [("rg", up), "cA2"], [("mu", up)], scale=cA2[:, gi:gi + 1])
                            S.ts(mu[:], mu[:], -1.0, 1.0, ALU.mult, ALU.add, [("mu", up)], [("mu", up)])
                            S.ts(mu[:], mu[:], 0.0, None, ALU.max, None, [("mu", up)], [("mu", up)])
                            S.act(mu[:], mu[:], AF.Sqrt, [("mu", up)], [("mu", up)])
                            S.tt(ig[:], ig[:], xcv[:], ALU.mult, [("ig", up), ("xcv", up)], [("ig", up)], eng="pool")
                            S.tt(ig[:], ig[:], mu[:], ALU.mult, [("ig", up), ("mu", up)], [("ig", up)], eng="pool")
                            if dr == 0:
                                S.op("dve", lambda hh=hh, a_t=a_t, ig=ig: nc.vector.tensor_tensor_scan(
                                    out=hh[:], data0=a_t[:], data1=ig[:], initial=stt[:, 0:1], op0=ALU.mult, op1=ALU.add),
                                    [("a_t", up), ("ig", up), ("stt", 0)], [("hh", up)])
                                S.cp(stt[:, 0:1], hh[:, TS - 1:TS], [("hh", up)], [("stt", 0)])
                            else:
                                S.op("dve", lambda hh=hh, a_t=a_t, ig=ig: nc.vector.tensor_tensor_scan(
                                    out=hh[:, ::-1], data0=a_t[:, ::-1], data1=ig[:, ::-1], initial=stt[:, 1:2],
                                    op0=ALU.mult, op1=ALU.add),
                                    [("a_t", up), ("ig", up), ("stt", 1)], [("hh", up)])
                                S.cp(stt[:, 1:2], hh[:, 0:1], [("hh", up)], [("stt", 1)])
                            sel = msel_t[:, q:q + 1] if use_sel else 1.0
                            if first:
                                S.ts(acc[:], hh[:], sel, None, ALU.mult, None, [("hh", up), "msel"], ["acc"])
                                first = False
                            else:
                                S.stt(acc[:], hh[:], sel, acc[:], ALU.mult, ALU.add, [("hh", up), "msel", "acc"], ["acc"])
                    S.dma(gyt[:], gy[sidx, n], [(gy_tag, "yl", n, bb) for bb in range(4)], ["gyt"])
                    S.tt(lo[:], acc[:], gyt[:], ALU.mult, ["acc", "gyt"], ["lo"])
                    S.dma(lruT[sidx, n], lo[:], ["lo"], [("lruT", sidx, n)])
                    S.flush(barrier=False)
                S.flush()

        if stop == "C":
            return nc
        phase_d("S", xl_s, TS, 0, False, "S", "S")
        if stop == "DS":
            return nc
        phase_d("C", xl_c, TC, 1, True, "C", "O")

        def phase_e(sidx, qtag, ktag, KT_ap, V_ap, Tk):
            nkt = Tk // 128
            with contextlib.ExitStack() as st:
                kT = sb(st, "kT", [128, Tk], BF16)
                vt = sb(st, "vt", [128, nkt, 128], BF16)
                qT = [sb(st, f"qT{i}", [128, TS], BF16) for i in range(2)]
                pT = [sb(st, f"pT{i}", [128, 512], BF16) for i in range(3)]
                rden = sb(st, "rden", [128, 512])
                ot = [sb(st, f"ot{i}", [128, 512], BF16) for i in range(2)]
                sc = 128.0 ** -0.5
                hcount = 0
                ucount = 0
                for g in range(4):
                    S.dma(kT[:], KT_ap[g], [(ktag, "k", g, bb) for bb in range(Tk // 512)], ["kT"])
                    S.dma(vt[:], V_ap[:, g * 128:(g + 1) * 128].rearrange("(kt p) d -> p kt d", p=128),
                          [(ktag, "v", g, bb) for bb in range(Tk // 512)], ["vt"])
                    for hh_ in range(4):
                        h = g * 4 + hh_
                        qb_ = qT[hcount % 2]
                        qk = ("qT", hcount % 2)
                        hcount += 1
                        S.dma(qb_[:], QT[sidx, h], [(qtag, "q", h, bb) for bb in range(4)], [qk])
                        for qb in range(4):
                            po = bank(2 + ucount % 2)
                            pok = ("ps", 2 + ucount % 2)
                            pd = bank(4 + ucount % 2)
                            pdk = ("ps", 4 + ucount % 2)
                            ucount += 1
                            qsl = qb_[:, qb * 512:(qb + 1) * 512]

                            def qk_mm(kt):
                                S.mm(bank(kt % 2), kT[:, kt * 128:(kt + 1) * 128], qsl, True, True,
                                     ["kT", qk], [("ps", kt % 2)])
                            qk_mm(0)
                            for kt in range(nkt):
                                if kt + 1 < nkt:
                                    qk_mm(kt + 1)
                                p_ = pT[kt % 3]
                                pk_ = ("pT", kt % 3)
                                S.act(p_[:], bank(kt % 2), AF.Exp, [("ps", kt % 2)], [pk_], scale=sc)
                                S.mm(po, vt[:, kt, :], p_[:], kt == 0, kt == nkt - 1, ["vt", pk_], [pok])
                                S.mm(pd, onesb[:], p_[:], kt == 0, kt == nkt - 1, ["onesb", pk_], [pdk])
                            S.op("dve", lambda pd=pd: nc.vector.reciprocal(out=rden[:], in_=pd), [pdk], ["rden"])
                            o_ = ot[ucount % 2]
                            S.tt(o_[:], po, rden[:], ALU.mult, [pok, "rden"], [("ot", ucount % 2)])
                            S.dma(attnT[sidx, h][:, qb * 512:(qb + 1) * 512], o_[:], [("ot", ucount % 2)],
                                  [("attnT", sidx, h, qb)])
                        S.flush(barrier=False)
                S.flush()

        if stop == "D":
            return nc
        phase_e(0, "S", "S", KT_s, V_s, TS)
        if stop == "ES":
            return nc
        phase_e(1, "O", "C", KT_c, V_c, TC)

        if stop == "E":
            return nc
        with contextlib.ExitStack() as st:
            aT = sb(st, "aT", [128, 16, 512], BF16)
            lT = sb(st, "lT", [128, 16, 512], BF16)
            mg = sb(st, "mg", [128, 32, 512], BF16)
            wao_t = [sb(st, f"wao{i}", [128, 16, 128], BF16) for i in range(2)]
            wlo_t = [sb(st, f"wlo{i}", [128, 16, 128], BF16) for i in range(2)]
            ga_t = [sb(st, f"ga_t{i}", [128, 512]) for i in range(2)]
            gl_t = [sb(st, f"gl_t{i}", [128, 512]) for i in range(2)]
            f1 = sb(st, "f1", [128, 512])
            f2 = sb(st, "f2", [128, 512])
            wo_t2 = [sb(st, f"wo_t{i}", [128, 32, 512], BF16) for i in range(2)]
            g1bc = sb(st, "g1bc", [128, D])
            xx = [sb(st, f"xx{i}", [128, 512]) for i in range(2)]
            xo_ = [sb(st, f"xo_{i}", [128, 512]) for i in range(2)]
            cnt = 0
            for sidx in range(2):
                x_ap = xs if sidx == 0 else xo
                S.dma(g1bc[:], modd[sidx, 0].partition_broadcast(128), [("modd", sidx, 0)], ["g1bc"])
                for b in range(4):
                    tsl = slice(b * 512, (b + 1) * 512)
                    S.dma(aT[:], attnT[sidx][:, :, tsl].rearrange("h p t -> p h t"),
                          [("attnT", sidx, h, b) for h in range(16)], ["aT"])
                    S.dma(lT[:], lruT[sidx][:, :, tsl].rearrange("h p t -> p h t"),
                          [("lruT", sidx, n) for n in range(16)], ["lT"])
                    for f in range(32):
                        i2 = f % 2
                        S.dma(wao_t[i2][:], waob[f], [("waob", f)], [("wao", i2)])
                        S.dma(wlo_t[i2][:], wlob[f], [("wlob", f)], [("wlo", i2)])
                        S.dma(ga_t[i2][:], ga[sidx, f][:, tsl], [("S" if sidx == 0 else "O", "ga", f, b)], [("ga_t", i2)])
                        S.dma(gl_t[i2][:], gl[sidx, f][:, tsl], [("S" if sidx == 0 else "O", "gl", f, b)], [("gl_t", i2)])
                        pB = bank(i2 * 2)
                        pA = bank(i2 * 2 + 1)
                        for kc in range(16):
                            S.mm(pB, wao_t[i2][:, kc, :], aT[:, kc, :], kc == 0, kc == 15, [("wao", i2), "aT"], [("ps", i2 * 2)])
                        for kc in range(16):
                            S.mm(pA, wlo_t[i2][:, kc, :], lT[:, kc, :], kc == 0, kc == 15, [("wlo", i2), "lT"], [("ps", i2 * 2 + 1)])
                        S.tt(f1[:], pB, ga_t[i2][:], ALU.mult, [("ps", i2 * 2), ("ga_t", i2)], ["f1"])
                        S.tt(f2[:], pA, gl_t[i2][:], ALU.mult, [("ps", i2 * 2 + 1), ("gl_t", i2)], ["f2"])
                        S.tt(mg[:, f, :], f1[:], f2[:], ALU.add, ["f1", "f2"], [("mg", f)], eng="pool")
                    mgk = [("mg", f) for f in range(32)]
                    S.dma(wo_t2[0][:], woutb[0], [("woutb", 0)], [("wo_t", 0)])
                    for nch in range(8):
                        if nch + 1 < 8:
                            S.dma(wo_t2[(nch + 1) % 2][:], woutb[nch + 1], [("woutb", nch + 1)], [("wo_t", (nch + 1) % 2)])
                        wo_t = wo_t2[nch % 2]
                        wok = ("wo_t", nch % 2)
                        csl = slice(nch * 512, (nch + 1) * 512)
                        for t4 in range(4):
                            i2 = cnt % 2
                            cnt += 1
                            r0 = b * 512 + t4 * 128
                            S.dma(xx[i2][:], x_ap[r0:r0 + 128, csl], [], [("xx", i2)])
                            po = bank(4 + i2)
                            for kc in range(32):
                                S.mm(po, mg[:, kc, t4 * 128:(t4 + 1) * 128], wo_t[:, kc, :], kc == 0, kc == 31,
                                     mgk + [wok], [("ps", 4 + i2)])
                            S.tt(xo_[i2][:], po, g1bc[:, csl], ALU.mult, [("ps", 4 + i2), "g1bc"], [("xo_", i2)])
                            S.tt(xo_[i2][:], xo_[i2][:], xx[i2][:], ALU.add, [("xo_", i2), ("xx", i2)], [("xo_", i2)], eng="pool")
                            S.dma(x1[sidx * TS + r0: sidx * TS + r0 + 128, csl], xo_[i2][:], [("xo_", i2)],
                                  [("x1", sidx * 16 + b * 4 + t4, nch)])
                    S.flush(barrier=False)
            S.flush()

        if stop == "F":
            return nc
        with contextlib.ExitStack() as st:
            x1t = sb(st, "x1t", [128, D])
            h2 = sb(st, "h2", [128, D])
            accp = sb(st, "accp", [128, D])
            gb = [sb(st, f"gb{i}", [128, D]) for i in range(3)]
            h2b = sb(st, "h2b", [128, D], BF16)
            h2T = sb(st, "h2T", [128, 32, 128], BF16)
            wq_t = [sb(st, f"wq_t{i}", [128, 32, 128], BF16) for i in range(2)]
            qTp = sb(st, "qTp", [128, 16, 128])
            keys_t = sb(st, "keys_t", [128, 16, 128])
            scs = sb(st, "scs", [128, 16, 128])
            wk = sb(st, "wk", [128, 256])
            stop_ = sb(st, "stop_", [128, 16, 16])
            itop = sb(st, "itop", [128, 16, 16], U32)
            itf = sb(st, "itf", [128, 16, 16])
            i0s = sb(st, "i0s", [128, 8, 16])
            cand = sb(st, "cand", [128, 8, 256])
            cidx = sb(st, "cidx", [128, 8, 256])
            bs = sb(st, "bs", [128, 8, 16])
            posu = sb(st, "posu", [128, 8, 16], U32)
            posf = sb(st, "posf", [128, 8, 16])
            eidx = sb(st, "eidx", [128, 128])
            eidi = sb(st, "eidi", [128, 128], I32)
            gsum = sb(st, "gsum", [128, 8])
            gates = sb(st, "gates", [128, 128])
            dots = sb(st, "dots", [128, 128])
            coef = sb(st, "coef", [128, 128])
            iot = sb(st, "iot", [128, 256])
            ssq2 = sb(st, "ssq2", [128, 1])
            rstd2 = sb(st, "rstd2", [128, 1])
            S.dma(keys_t[:], keysT[:, :, :], [], ["keys_t"])
            S.dma(iot[:], iota_in[:, :], [], ["iot"])
            V = nc.vector
            gcount = 0
            gbv = [gb[i // 2][:].bitcast(BF16)[:, (i % 2) * D:(i % 2 + 1) * D] for i in range(6)]
            gbk = [("gb", i // 2, i % 2) for i in range(6)]
            ubk = [("ub16", i) for i in range(64)]
            vbk = [("vb16", i) for i in range(64)]

            def gfull(i):
                return [("gb", i, 0), ("gb", i, 1)]

            def top16(src_ap, n, vals_ap, idx_ap, rk):
                S.op("dve", lambda: V.max(out=vals_ap[:, 0:8], in_=src_ap), rk, ["tv"])
                S.op("dve", lambda: V.max_index(out=idx_ap[:, 0:8], in_max=vals_ap[:, 0:8], in_values=src_ap),
                     rk + ["tv"], ["ti"])
                S.op("dve", lambda: V.match_replace(out=wk[:, 0:n], in_to_replace=vals_ap[:, 0:8], in_values=src_ap,
                                                    imm_value=-1e30), rk + ["tv"], ["wk"])
                S.op("dve", lambda: V.max(out=vals_ap[:, 8:16], in_=wk[:, 0:n]), ["wk"], ["tv"])
                S.op("dve", lambda: V.max_index(out=idx_ap[:, 8:16], in_max=vals_ap[:, 8:16], in_values=wk[:, 0:n]),
                     ["wk", "tv"], ["ti"])

            for ti in range(32):
                sidx = ti // 16
                y_ap = ys if sidx == 0 else yo
                rloc = (ti % 16) * 128
                S.dma(x1t[:], x1[ti * 128:(ti + 1) * 128, :], [("x1", ti, nch) for nch in range(8)], ["x1t"])
                S.dma(gb[0][:], modd[sidx, 1].partition_broadcast(128), [("modd", sidx, 1)], gfull(0))
                S.dma(gb[1][:], modd[sidx, 2].partition_broadcast(128), [("modd", sidx, 2)], gfull(1))
                S.act(gb[2][:].bitcast(BF16)[:, 0:D], x1t[:], AF.Square, ["x1t"], gfull(2) + ["ssq2"], accum_out=ssq2[:])
                S.ts(rstd2[:], ssq2[:], 1.0 / D, EPS, ALU.mult, ALU.add, ["ssq2"], ["rstd2"])
                S.act(rstd2[:], rstd2[:], AF.Sqrt, ["rstd2"], ["rstd2"])
                S.op("dve", lambda: nc.vector.reciprocal(out=rstd2[:], in_=rstd2[:]), ["rstd2"], ["rstd2"])
                S.stt(h2[:], x1t[:], rstd2[:, 0:1], gb[0][:], ALU.mult, ALU.mult, ["x1t", "rstd2"] + gfull(0), ["h2"])
                S.tt(h2[:], h2[:], gb[1][:], ALU.add, ["h2"] + gfull(1), ["h2"])
                S.cp(h2b[:], h2[:], ["h2"], ["h2b"], eng="act")
                for g8 in range(4):
                    pb = g8 % 2
                    for k8 in range(8):
                        kc = g8 * 8 + k8
                        S.tr(psb[:, pb * 1024 + k8 * 128: pb * 1024 + (k8 + 1) * 128],
                             h2b[:, kc * 128:(kc + 1) * 128], identb[:], ["h2b", "identb"], [("psb", pb)])
                    S.cp(h2T[:, g8 * 8:(g8 + 1) * 8, :], psb[:, pb * 1024:(pb + 1) * 1024].rearrange("p (a t) -> p a t", a=8),
                         [("psb", pb)], [("h2T", g8)], eng=("dve" if g8 % 2 == 0 else "act"))
                h2Tk = [("h2T", g8) for g8 in range(4)]
                for f in range(16):
                    i2 = f % 2
                    S.dma(wq_t[i2][:], wqb[f], [("wqb", f)], [("wq_t", i2)])
                    pq = ps[:, i2 * 512:i2 * 512 + 128]
                    for kc in range(32):
                        S.mm(pq, wq_t[i2][:, kc, :], h2T[:, kc, :], kc == 0, kc == 31, [("wq_t", i2)] + h2Tk, [("ps", i2)])
                    S.cp(qTp[:, f, :], pq, [("ps", i2)], [("qTp", f)], eng=("dve" if f % 2 == 0 else "act"))
                for hp in range(16):
                    bk = 2 + hp // 4
                    S.mm(ps[:, bk * 512 + (hp % 4) * 128: bk * 512 + (hp % 4 + 1) * 128], qTp[:, hp, :], keys_t[:, hp, :],
                         True, True, [("qTp", hp), "keys_t"], [("ps", bk)])
                S.cp(scs[:].rearrange("p a n -> p (a n)"), ps[:, 1024:3072], [("ps", 2), ("ps", 3), ("ps", 4), ("ps", 5)], ["scs"])
                for hp in range(16):
                    top16(scs[:, hp, :], 128, stop_[:, hp, :], itop[:, hp, :], ["scs"])
                S.cp(itf[:], itop[:], ["ti"], ["itf"])
                st4 = stop_[:].rearrange("p (h two) k -> p h two k", two=2)
                it4 = itf[:].rearrange("p (h two) k -> p h two k", two=2)
                c4 = cand[:].rearrange("p h (i j) -> p h i j", i=16)
                x4 = cidx[:].rearrange("p h (i j) -> p h i j", i=16)
                S.tt(c4, st4[:, :, 0, :].unsqueeze(3).to_broadcast([128, 8, 16, 16]),
                     st4[:, :, 1, :].unsqueeze(2).to_broadcast([128, 8, 16, 16]), ALU.add, ["tv"], ["cand"])
                S.ts(i0s[:], it4[:, :, 0, :], 128.0, None, ALU.mult, None, ["itf"], ["i0s"])
                S.tt(x4, i0s[:].unsqueeze(3).to_broadcast([128, 8, 16, 16]),
                     it4[:, :, 1, :].unsqueeze(2).to_broadcast([128, 8, 16, 16]), ALU.add, ["i0s", "itf"], ["cidx"])
                oh = gb[2]
                oh3 = oh[:].rearrange("p (k n) -> p k n", k=16)
                for h in range(8):
                    top16(cand[:, h, :], 256, bs[:, h, :], posu[:, h, :], ["cand"])
                    S.cp(posf[:, h, :], posu[:, h, :], ["ti"], [("posf", h)])
                    S.tt(oh3, iot[:].unsqueeze(1).to_broadcast([128, 16, 256]),
                         posf[:, h, :].unsqueeze(2).to_broadcast([128, 16, 256]), ALU.is_equal,
                         ["iot", ("posf", h)], gfull(2))
                    S.tt(oh3, oh3, cidx[:, h, :].unsqueeze(1).to_broadcast([128, 16, 256]), ALU.mult,
                         gfull(2) + ["cidx"], gfull(2))
                    S.op("dve", lambda h=h: V.tensor_reduce(out=eidx[:, h * 16:(h + 1) * 16], in_=oh3, axis=AX.X, op=ALU.add),
                         gfull(2), [("eidx", h)])
                bsk = ["tv"]
                g3 = gates[:].rearrange("p (h k) -> p h k", h=8)
                S.tt(g3, bs[:], bs[:, :, 0:1].to_broadcast([128, 8, 16]), ALU.subtract, bsk, ["gates"])
                S.act(gates[:], gates[:], AF.Exp, ["gates"], ["gates"])
                S.op("dve", lambda: V.tensor_reduce(out=gsum[:], in_=g3, axis=AX.X, op=ALU.add), ["gates"], ["gsum"])
                S.op("dve", lambda: V.reciprocal(out=gsum[:], in_=gsum[:]), ["gsum"], ["gsum"])
                S.tt(g3, g3, gsum[:].unsqueeze(2).to_broadcast([128, 8, 16]), ALU.mult, ["gates", "gsum"], ["gates"])
                eik = [("eidx", h) for h in range(8)]
                S.ts(eidx[:], eidx[:], 0.0, 16383.0, ALU.max, ALU.min, eik, eik)
                S.cp(eidi[:], eidx[:], eik, ["eidi"])
                G = nc.gpsimd
                for e in range(128):
                    bi_ = gcount % 6
                    gcount += 1
                    S.op("pool", lambda bi_=bi_, e=e: G.indirect_dma_start(
                        out=gbv[bi_], out_offset=None, in_=ub16[:, :],
                        in_offset=bass.IndirectOffsetOnAxis(ap=eidi[:, e:e + 1], axis=0)),
                        ["eidi"] + ubk, [gbk[bi_]], dma=True)
                    S.op("dve", lambda bi_=bi_, e=e: V.scalar_tensor_tensor(
                        out=h2b[:], in0=gbv[bi_], scalar=1.0, in1=h2[:], op0=ALU.mult, op1=ALU.mult,
                        accum_out=dots[:, e:e + 1]), [gbk[bi_], "h2"], ["h2b", ("dots", e)])
                dk = [("dots", e) for e in range(128)]
                S.act(coef[:], dots[:], AF.Gelu, dk, ["coef"])
                S.tt(coef[:], coef[:], gates[:], ALU.mult, ["coef", "gates"], ["coef"])
                for e in range(128):
                    bi_ = gcount % 6
                    gcount += 1
                    S.op("pool", lambda bi_=bi_, e=e: G.indirect_dma_start(
                        out=gbv[bi_], out_offset=None, in_=vb16[:, :],
                        in_offset=bass.IndirectOffsetOnAxis(ap=eidi[:, e:e + 1], axis=0)),
                        ["eidi"] + vbk, [gbk[bi_]], dma=True)
                    if e == 0:
                        S.ts(accp[:], gbv[bi_], coef[:, 0:1], None, ALU.mult, None, [gbk[bi_], "coef"], ["accp"])
                    else:
                        S.stt(accp[:], gbv[bi_], coef[:, e:e + 1], accp[:], ALU.mult, ALU.add,
                              [gbk[bi_], "coef", "accp"], ["accp"])
                S.dma(gb[2][:], modd[sidx, 3].partition_broadcast(128), [("modd", sidx, 3)], gfull(2))
                S.tt(accp[:], accp[:], gb[2][:], ALU.mult, ["accp"] + gfull(2), ["accp"])
                S.tt(accp[:], accp[:], x1t[:], ALU.add, ["accp", "x1t"], ["accp"], eng="pool")
                S.dma(y_ap[rloc:rloc + 128, :], accp[:], ["accp"], [("y", ti)])
                S.flush(barrier=False)
            S.flush()
    return nc


def _rope_tables(T):
    pos = np.arange(T)
    rows = (pos // 64).astype(np.float32)
    cols = (pos % 64).astype(np.float32)
    inv = (10000.0 ** (-np.arange(0, 64, 2, dtype=np.float32) / 64.0)).astype(np.float32)
    ar = rows[None, :] * inv[:, None]
    ac = cols[None, :] * inv[:, None]
    C = np.concatenate([np.cos(ar), np.cos(ar), np.cos(ac), np.cos(ac)], axis=0).astype(np.float32)
    Sn = np.concatenate([np.sin(ar), np.sin(ar), np.sin(ac), np.sin(ac)], axis=0).astype(np.float32)
    return np.ascontiguousarray(C), np.ascontiguousarray(Sn)


def _consts():
    ident = np.eye(128, dtype=np.float32)
    R = np.zeros((128, 128), np.float32)
    for base in (0, 64):
        for i in range(32):
            R[base + i, base + 32 + i] = -1.0
            R[base + 32 + i, base + i] = 1.0
    rotT = np.ascontiguousarray(R.T)
    ones = np.ones((128, 128), np.float32)
    return np.ascontiguousarray(np.stack([ident, rotT, ones], axis=1))


def make_in_maps(inp):
    f = lambda a: np.ascontiguousarray(np.asarray(a, dtype=np.float32))
    C, Sn = _rope_tables(TC)
    cst = _consts()
    iota = np.ascontiguousarray(np.tile(np.arange(256, dtype=np.float32)[None, :], (128, 1)))
    fm = lambda v, nch: np.ascontiguousarray(f(v).reshape(nch, 128).T)
    shared = {
        "w_ada": f(inp["w_ada"][0]), "badaT": fm(inp["b_ada"][0], 192),
        "g1T": fm(inp["g_norm1"][0], 32), "g2T": fm(inp["g_norm2"][0], 32),
        "w_in": f(inp["w_in"][0]),
        "qkg": np.ascontiguousarray(np.stack([f(inp["q_gain"][0]), f(inp["k_gain"][0])], axis=1)),
        "convwT": np.ascontiguousarray(f(inp["conv_w"][0]).reshape(4, 16, 128).transpose(2, 1, 0)),
        "convbT": fm(inp["conv_b"][0], 16),
        "lamT": np.ascontiguousarray(f(inp["lru_lam"][0]).reshape(32, 128).T),
        "baT": np.ascontiguousarray(f(inp["lru_ba"][0]).reshape(32, 128).T),
        "biT": np.ascontiguousarray(f(inp["lru_bi"][0]).reshape(32, 128).T),
        "lru_wa": f(inp["lru_wa"][0]).reshape(32, 128, 128),
        "lru_wi": f(inp["lru_wi"][0]).reshape(32, 128, 128),
        "w_attn_o": f(inp["w_attn_o"][0]), "w_lru_o": f(inp["w_lru_o"][0]), "w_out": f(inp["w_out"][0]),
        "peer_wq": f(inp["peer_wq"][0]),
        "keysT": np.ascontiguousarray(f(inp["peer_keys"][0]).reshape(16, 128, 128).transpose(2, 0, 1)),
        "peer_u": f(inp["peer_u"][0]), "peer_v": f(inp["peer_v"][0]),
        "ropeC": C, "ropeS": Sn, "consts": cst, "iota256": iota,
    }
    xp = f(inp["x_prompt"])
    xsm = f(inp["x_sample"])
    cp = f(inp["c_prompt"])
    csm = f(inp["c_sample"])
    maps = []
    for c in range(8):
        b, j = c // 4, c % 4
        cc = np.stack([csm[c], cp[b]], axis=0)
        cTl = np.ascontiguousarray(cc.reshape(2, 32, 128).transpose(2, 1, 0))
        ms = np.zeros((128, 4), np.float32)
        ms[:, j] = 1.0
        m = dict(shared)
        m.update({
            "xs": xsm[c], "xo": np.ascontiguousarray(xp[b, j * TS:(j + 1) * TS]), "xc": xp[b],
            "cT": cTl, "msel": ms,
            "ropeCo": np.ascontiguousarray(C[:, j * TS:(j + 1) * TS]),
            "ropeSo": np.ascontiguousarray(Sn[:, j * TS:(j + 1) * TS]),
        })
        maps.append(m)
    return maps


def kernel(**inputs):
    nc = build()
    maps = make_in_maps(inputs)
    res = run_bass_kernel_spmd(nc, maps, core_ids=list(range(8)))
    y_prompt = np.zeros((2, 8192, D), np.float32)
    y_sample = np.zeros((8, TS, D), np.float32)
    for c in range(8):
        b, j = c // 4, c % 4
        y_sample[c] = res.results[c]["ys"]
        y_prompt[b, j * TS:(j + 1) * TS] = res.results[c]["yo"]
    return (y_prompt, y_sample)
```

```python
import contextlib
import numpy as np
import concourse.bass as bass
import concourse.mybir as mybir
from concourse.bass_utils import run_bass_kernel_spmd

F32 = mybir.dt.float32
BF16 = mybir.dt.bfloat16
U32 = mybir.dt.uint32
I32 = mybir.dt.int32
AF = mybir.ActivationFunctionType
ALU = mybir.AluOpType
AX = mybir.AxisListType

D = 4096
TS = 2048
TC = 8192
NCOL = 15360
EPS = 1e-6
SEM_LIMIT = 30000
DBG = {}


class Op:
    __slots__ = ("eng", "fn", "deps", "dma", "signal", "sem", "val")

    def __init__(self, eng, fn, dma):
        self.eng = eng
        self.fn = fn
        self.dma = dma
        self.deps = ()
        self.signal = False
        self.sem = None
        self.val = 0


class Sched:
    def __init__(self, nc, es):
        self.nc = nc
        self.es = es
        self.engs = {"pe": nc.tensor, "act": nc.scalar, "dve": nc.vector, "pool": nc.gpsimd, "sp": nc.sync}
        self.ops = []
        self.last_w = {}
        self.readers = {}
        self.nsem = 0
        self.cur_sem = {}
        self.cnt = {}
        for e in ("pe", "act", "dve", "pool"):
            self.cur_sem[e] = self._new_sem()
            self.cnt[e] = 0
        self.slots = {"sp": [[self._new_sem(), 0] for _ in range(24)],
                      "pool": [[self._new_sem(), 0] for _ in range(16)]}
        self.slot_i = {"sp": 0, "pool": 0}
        self.seen = {e: {} for e in self.engs}
        self.last_op = {}
        self.trace = {e: [] for e in self.engs} if DBG.get("trace") else None

    def _new_sem(self):
        self.nsem += 1
        return self.es.enter_context(self.nc.semaphore(f"sm{self.nsem}"))

    def op(self, eng, fn, r=(), w=(), dma=False):
        w = list(w) + [k for k in r if isinstance(k, tuple) and k[0] in ("ps", "psb")]
        o = Op(eng, fn, dma)
        deps = {}
        for k in r:
            lw = self.last_w.get(k)
            if lw is not None:
                deps[id(lw)] = lw
        for k in w:
            lw = self.last_w.get(k)
            if lw is not None:
                deps[id(lw)] = lw
            rd = self.readers.get(k)
            if rd:
                for x in rd.values():
                    deps[id(x)] = x
        rk = ("d", id(o)) if dma else eng
        for k in r:
            self.readers.setdefault(k, {})[rk] = o
        for k in w:
            self.last_w[k] = o
            self.readers[k] = {}
        dl = []
        for d in deps.values():
            if eng == "pe" and not dma and d.eng == "pe" and not d.dma:
                continue
            d.signal = True
            dl.append(d)
        o.deps = dl
        self.ops.append(o)
        return o

    def _wait(self, E, sem, val):
        sn = self.seen[E]
        if sn.get(id(sem), 0) < val:
            self.engs[E].wait_ge(sem, val)
            sn[id(sem)] = val
            if self.trace is not None:
                self.trace[E].append(("w", id(sem), val))

    def flush(self, barrier=True):
        for lw in self.last_w.values():
            lw.signal = True
        for rd in self.readers.values():
            for x in rd.values():
                x.signal = True
        lastc = {}
        for o in self.ops:
            if not o.dma:
                lastc[o.eng] = o
        for o in lastc.values():
            o.signal = True
        for o in self.ops:
            E = o.eng
            eng = self.engs[E]
            for d in o.deps:
                self._wait(E, d.sem, d.val)
            if o.dma:
                sl = self.slots[E]
                i = self.slot_i[E]
                self.slot_i[E] = (i + 1) % len(sl)
                sem, uses = sl[i]
                if uses > 0:
                    self._wait(E, sem, 16 * uses)
                if 16 * (uses + 1) > SEM_LIMIT:
                    sem = self._new_sem()
                    uses = 0
                    sl[i][0] = sem
                ins = o.fn()
                ins.then_inc(sem, 16)
                if self.trace is not None:
                    self.trace[E].append(("s", id(sem), 16))
                uses += 1
                sl[i][1] = uses
                o.sem = sem
                o.val = 16 * uses
            else:
                ins = o.fn()
                if o.signal:
                    if self.cnt[E] >= SEM_LIMIT:
                        self.cur_sem[E] = self._new_sem()
                        self.cnt[E] = 0
                    self.cnt[E] += 1
                    ins.then_inc(self.cur_sem[E], 1)
                    if self.trace is not None:
                        self.trace[E].append(("s", id(self.cur_sem[E]), 1))
                    o.sem = self.cur_sem[E]
                    o.val = self.cnt[E]
            o.fn = None
        self.ops = []
        if barrier:
            for E in self.engs:
                for E2 in ("pe", "act", "dve", "pool"):
                    if E2 != E and self.cnt[E2] > 0:
                        self._wait(E, self.cur_sem[E2], self.cnt[E2])
                for q in ("sp", "pool"):
                    for sem, uses in self.slots[q]:
                        if uses > 0:
                            self._wait(E, sem, 16 * uses)

    def dma(self, out, in_, r, w, q="sp"):
        eng = self.engs[q]
        return self.op(q, lambda: eng.dma_start(out=out, in_=in_), r, w, dma=True)

    def mm(self, out, lhsT, rhs, start, stop, r, w):
        t = self.nc.tensor
        return self.op("pe", lambda: t.matmul(out, lhsT=lhsT, rhs=rhs, start=start, stop=stop), r, w)

    def tr(self, out, in_, ident, r, w):
        t = self.nc.tensor
        return self.op("pe", lambda: t.transpose(out=out, in_=in_, identity=ident), r, w)

    def act(self, out, in_, func, r, w, bias=None, scale=None, accum_out=None):
        a = self.nc.scalar
        kw = {}
        if bias is not None:
            kw["bias"] = bias
        if scale is not None:
            kw["scale"] = scale
        if accum_out is not None:
            kw["accum_out"] = accum_out
        return self.op("act", lambda: a.activation(out=out, in_=in_, func=func, **kw), r, w)

    def ts(self, out, in0, s1, s2, op0, op1, r, w, eng="dve"):
        e = self.engs[eng]
        if op1 is None:
            return self.op(eng, lambda: e.tensor_scalar(out=out, in0=in0, scalar1=s1, scalar2=None, op0=op0), r, w)
        return self.op(eng, lambda: e.tensor_scalar(out=out, in0=in0, scalar1=s1, scalar2=s2, op0=op0, op1=op1), r, w)

    def tt(self, out, in0, in1, op, r, w, eng="dve"):
        e = self.engs[eng]
        return self.op(eng, lambda: e.tensor_tensor(out=out, in0=in0, in1=in1, op=op), r, w)

    def stt(self, out, in0, scalar, in1, op0, op1, r, w):
        e = self.nc.vector
        return self.op("dve", lambda: e.scalar_tensor_tensor(out=out, in0=in0, scalar=scalar, in1=in1, op0=op0, op1=op1), r, w)

    def cp(self, out, in_, r, w, eng="dve"):
        e = self.engs[eng]
        if eng == "act":
            return self.op(eng, lambda: e.copy(out=out, in_=in_), r, w)
        return self.op(eng, lambda: e.tensor_copy(out=out, in_=in_), r, w)


def build(debug=False, dbg_names=(), stop=None):
    nc = bass.Bass("TRN2", target_bir_lowering=False)

    def din(name, shape, dt=F32):
        return nc.dram_tensor(name, list(shape), dt, kind="ExternalInput").ap()

    def dscr(name, shape, dt=F32):
        kind = "ExternalOutput" if name in dbg_names else "Internal"
        return nc.dram_tensor(name, list(shape), dt, kind=kind).ap()

    xs = din("xs", [TS, D])
    xo = din("xo", [TS, D])
    xc = din("xc", [TC, D])
    cT = din("cT", [128, 32, 2])
    w_ada = din("w_ada", [D, 6 * D])
    badaT = din("badaT", [128, 192])
    g1T = din("g1T", [128, 32])
    g2T = din("g2T", [128, 32])
    w_in = din("w_in", [D, NCOL])
    qkg = din("qkg", [128, 2])
    convwT = din("convwT", [128, 16, 4])
    convbT = din("convbT", [128, 16])
    lamT = din("lamT", [128, 32])
    baT = din("baT", [128, 32])
    biT = din("biT", [128, 32])
    lru_wa = din("lru_wa", [32, 128, 128])
    lru_wi = din("lru_wi", [32, 128, 128])
    w_attn_o = din("w_attn_o", [2048, D])
    w_lru_o = din("w_lru_o", [2048, D])
    w_out = din("w_out", [D, D])
    peer_wq = din("peer_wq", [D, 2048])
    keysT = din("keysT", [128, 16, 128])
    peer_u = din("peer_u", [16384, D])
    peer_v = din("peer_v", [16384, D])
    ropeC = din("ropeC", [128, TC])
    ropeS = din("ropeS", [128, TC])
    ropeCo = din("ropeCo", [128, TS])
    ropeSo = din("ropeSo", [128, TS])
    consts = din("consts", [128, 3, 128])
    iota_in = din("iota256", [128, 256])
    msel = din("msel", [128, 4])

    ys = nc.dram_tensor("ys", [TS, D], F32, kind="ExternalOutput").ap()
    yo = nc.dram_tensor("yo", [TS, D], F32, kind="ExternalOutput").ap()

    winb = dscr("winb", [120, 128, 32, 128], BF16)
    waob = dscr("waob", [32, 128, 16, 128], BF16)
    wlob = dscr("wlob", [32, 128, 16, 128], BF16)
    woutb = dscr("woutb", [8, 128, 32, 512], BF16)
    wqb = dscr("wqb", [16, 128, 32, 128], BF16)
    modd = dscr("modd", [2, 4, D])
    QT = dscr("QT", [2, 16, 128, TS], BF16)
    KT_s = dscr("KT_s", [4, 128, TS], BF16)
    KT_c = dscr("KT_c", [4, 128, TC], BF16)
    V_s = dscr("V_s", [TS, 512], BF16)
    V_c = dscr("V_c", [TC, 512], BF16)
    xl_s = dscr("xl_s", [16, 128, TS])
    xl_c = dscr("xl_c", [16, 128, TC])
    gy = dscr("gy", [2, 16, 128, TS])
    ga = dscr("ga", [2, 32, 128, TS])
    gl = dscr("gl", [2, 32, 128, TS])
    attnT = dscr("attnT", [2, 16, 128, TS], BF16)
    lruT = dscr("lruT", [2, 16, 128, TS], BF16)
    x1 = dscr("x1", [2 * TS, D])
    ub16 = dscr("ub16", [16384, D], BF16)
    vb16 = dscr("vb16", [16384, D], BF16)
    dbg = {}
    if debug:
        dbg["modT"] = nc.dram_tensor("dbg_modT", [128, 192, 2], F32, kind="ExternalOutput").ap()

    es = contextlib.ExitStack()
    with es:
        S = Sched(nc, es)
        nc._sched = S

        nm = [0]

        def sb(st, name, shape, dt=F32):
            nm[0] += 1
            return st.enter_context(nc.sbuf_tensor(f"{name}_{nm[0]}", list(shape), dt))

        ps = es.enter_context(nc.psum_tensor("ps", [128, 3072], F32))
        psb = es.enter_context(nc.psum_tensor("psb", [128, 2048], BF16))
        cst = sb(es, "cst", [128, 3, 128])
        identb = sb(es, "identb", [128, 128], BF16)
        onesb = sb(es, "onesb", [128, 128], BF16)
        modT = sb(es, "modT", [128, 192, 2])
        G1 = sb(es, "G1", [128, 32, 2])
        G2 = sb(es, "G2", [128, 32, 2])
        qkg_t = sb(es, "qkg_t", [128, 2])
        msel_t = sb(es, "msel_t", [128, 4])
        identf = cst[:, 0, :]
        rotT = cst[:, 1, :]
        onesf = cst[:, 2, :]

        def bank(i):
            return ps[:, i * 512:(i + 1) * 512]

        S.dma(cst[:], consts[:, :, :], [], ["cst"])
        S.dma(qkg_t[:], qkg[:, :], [], ["qkg"])
        S.dma(msel_t[:], msel[:, :], [], ["msel"])
        S.cp(identb[:], cst[:, 0, :], ["cst"], ["identb"])
        S.cp(onesb[:], cst[:, 2, :], ["cst"], ["onesb"])

        def cast_w(dst, src, nf, kc, cw):
            for f in range(nf):
                S.dma(dst[f], src[:, f * cw:(f + 1) * cw].rearrange("(kc p) c -> p kc c", p=128),
                      [], [(dst.tensor.name, f)], q="pool")
        cast_w(winb, w_in, 120, 32, 128)
        cast_w(waob, w_attn_o, 32, 16, 128)
        cast_w(wlob, w_lru_o, 32, 16, 128)
        cast_w(woutb, w_out, 8, 32, 512)
        cast_w(wqb, peer_wq, 16, 32, 128)

        with contextlib.ExitStack() as st:
            cT_t = sb(st, "cT_t", [128, 32, 2])
            scT = sb(st, "scT", [128, 32, 2])
            bada_t = sb(st, "bada_t", [128, 192])
            g1_t = sb(st, "g1_t", [128, 32])
            g2_t = sb(st, "g2_t", [128, 32])
            wa = [sb(st, f"wa{i}", [128, 32, 256]) for i in range(2)]
            tmpc = sb(st, "tmpc", [128, 32])
            tmpr = sb(st, "tmpr", [32, 128])
            S.dma(cT_t[:], cT[:, :, :], [], ["cT"])
            S.dma(bada_t[:], badaT[:, :], [], ["bada"])
            S.dma(g1_t[:], g1T[:, :], [], ["g1"])
            S.dma(g2_t[:], g2T[:, :], [], ["g2"])
            S.act(scT[:], cT_t[:], AF.Silu, ["cT"], ["scT"])
            for j2 in range(96):
                buf = wa[j2 % 2]
                S.dma(buf[:], w_ada[:, j2 * 256:(j2 + 1) * 256].rearrange("(kc p) c -> p kc c", p=128),
                      [], [("wa", j2 % 2)])
                for half in range(2):
                    j = j2 * 2 + half
                    b = j % 6
                    pst = ps[:, b * 512:b * 512 + 2]
                    for kc in range(32):
                        S.mm(pst, buf[:, kc, half * 128:(half + 1) * 128], scT[:, kc, :], kc == 0, kc == 31,
                             [("wa", j2 % 2), "scT"], [("ps", b)])
                    S.ts(modT[:, j, :], pst, bada_t[:, j:j + 1], None, ALU.add, None,
                         [("ps", b), "bada"], [("modT", j)])
            allmod = [("modT", j) for j in range(192)]
            S.ts(G1[:], modT[:, 32:64, :], 1.0, None, ALU.add, None, allmod, ["G1"])
            S.tt(G1[:], G1[:], g1_t[:].unsqueeze(2).to_broadcast([128, 32, 2]), ALU.mult, ["G1", "g1"], ["G1"])
            S.ts(G2[:], modT[:, 128:160, :], 1.0, None, ALU.add, None, allmod, ["G2"])
            S.tt(G2[:], G2[:], g2_t[:].unsqueeze(2).to_broadcast([128, 32, 2]), ALU.mult, ["G2", "g2"], ["G2"])
            srcs = [(modT, 64), (G2, 0), (modT, 96), (modT, 160)]
            for m in range(2):
                for sl, (srct, off) in enumerate(srcs):
                    S.cp(tmpc[:], srct[:, off:off + 32, m], allmod + ["G2"], ["tmpc"])
                    S.tr(ps[0:32, 0:128], tmpc[:], identf, ["tmpc", "cst"], [("ps", 0)])
                    S.cp(tmpr[:], ps[0:32, 0:128], [("ps", 0)], ["tmpr"])
                    S.dma(modd[m, sl].rearrange("(kc p) -> kc p", p=128), tmpr[:], ["tmpr"], [("modd", m, sl)])
            if debug:
                S.dma(dbg["modT"][:, :, :], modT[:], allmod, ["dbg_modT"])
            S.flush()

        def phase_c(tag, x_ap, T, m, cols, rc, rs, oq, ok, ov, oxl, sidx):
            with contextlib.ExitStack() as st:
                NXB = DBG.get("nxb", 2)
                xt = [sb(st, f"xt{i}", [128, D]) for i in range(NXB)]
                NXN = DBG.get("nxn", NXB)
                xn = [sb(st, f"xn{i}", [128, D], BF16) for i in range(NXN)]
                hT = [sb(st, f"hT{i}", [128, 32, 512], BF16) for i in range(DBG.get("nhT", 2))]
                wt = [sb(st, f"wt{i}", [128, 32, 128], BF16) for i in range(3)]
                ssq = sb(st, "ssq", [128, 2])
                rstd = sb(st, "rstd", [128, 2])
                sqjunk = sb(st, "sqjunk", [128, D], BF16)
                ct = [sb(st, f"ct{i}", [128, 512]) for i in range(2)]
                stt_ = [sb(st, f"st{i}", [128, 512]) for i in range(2)]
                sqf = sb(st, "sqf", [128, 512])
                rsf = sb(st, "rsf", [128, 512])
                qn = sb(st, "qn", [128, 512])
                t1 = sb(st, "t1", [128, 512])
                t2 = sb(st, "t2", [128, 512])
                ob = [sb(st, f"ob{i}", [128, 512], BF16) for i in range(2)]
                of = [sb(st, f"of{i}", [128, 512]) for i in range(2)]
                vT = sb(st, "vT", [128, 512], BF16)
                vtile = [sb(st, f"vtile{i}", [128, 4, 128], BF16) for i in range(2)]
                nblk = DBG.get("nblk", T // 512)
                if "kinds" in DBG:
                    cols = [c_ for c_ in cols if c_[1] in DBG["kinds"]][:DBG.get("ncols", 1000)]
                has_rope = any(kind in ("q", "k") for (_, kind, _) in cols)
                wcount = 0
                pcount = 0
                ocount = 0
                for b in range(nblk):
                    hb = hT[b % len(hT)]
                    hk = ("hT", b % len(hT))
                    for tt_ in range(DBG.get("ntile", 4)):
                        ti = b * 4 + tt_
                        xb = xt[ti % NXB]
                        xk = ("xt", ti % NXB)
                        nb_ = xn[ti % NXN]
                        nk = ("xn", ti % NXN)
                        S.dma(xb[:], x_ap[ti * 128:(ti + 1) * 128, :], [], [xk])
                        S.act(sqjunk[:], xb[:], AF.Square, [xk], ["sqjunk", ("ssq", ti % 2)], accum_out=ssq[:, ti % 2:ti % 2 + 1])
                        S.ts(rstd[:, ti % 2:ti % 2 + 1], ssq[:, ti % 2:ti % 2 + 1], 1.0 / D, EPS, ALU.mult, ALU.add,
                             [("ssq", ti % 2)], [("rstd", ti % 2)])
                        S.act(rstd[:, ti % 2:ti % 2 + 1], rstd[:, ti % 2:ti % 2 + 1], AF.Sqrt,
                              [("rstd", ti % 2)], [("rstd", ti % 2)])
                        S.op("dve", lambda ti=ti: nc.vector.reciprocal(out=rstd[:, ti % 2:ti % 2 + 1], in_=rstd[:, ti % 2:ti % 2 + 1]),
                             [("rstd", ti % 2)], [("rstd", ti % 2)])
                        S.act(nb_[:], xb[:], AF.Identity, [xk, ("rstd", ti % 2)], [nk], scale=rstd[:, ti % 2:ti % 2 + 1])
                        fes = DBG.get("fe", 9)
                        if tt_ >= DBG.get("fe_tiles", 4):
                            fes = 1
                        for g8 in range(DBG.get("ng8", 4) if fes >= 2 else 0):
                            pb = g8 % 2
                            for k8 in range(8):
                                kc = g8 * 8 + k8
                                S.tr(psb[:, pb * 1024 + k8 * 128: pb * 1024 + (k8 + 1) * 128],
                                     nb_[:, kc * 128:(kc + 1) * 128], identb[:], [nk, "identb"], [("psb", pb)])
                            for k8 in range(8 if fes >= 3 else 0):
                                kc = g8 * 8 + k8
                                src = psb[:, pb * 1024 + k8 * 128: pb * 1024 + (k8 + 1) * 128]
                                dst = hb[:, kc, tt_ * 128:(tt_ + 1) * 128]
                                if (pb == 0 and fes != 4) or fes == 3:
                                    S.ts(dst, src, G1[:, kc, m:m + 1], modT[:, kc, m:m + 1], ALU.mult, ALU.add,
                                         [("psb", pb), "G1"], [(hk, kc, tt_)])
                                else:
                                    S.act(dst, src, AF.Identity, [("psb", pb), "G1"], [(hk, kc, tt_)],
                                          bias=modT[:, kc, m:m + 1], scale=G1[:, kc, m:m + 1])
                    hkeys = [(hk, kc, t4) for kc in range(32) for t4 in range(4)]
                    if has_rope:
                        cb = ct[b % 2]
                        sbb = stt_[b % 2]
                        S.dma(cb[:], rc[:, b * 512:(b + 1) * 512], [], [("ct", b % 2)])
                        S.dma(sbb[:], rs[:, b * 512:(b + 1) * 512], [], [("st", b % 2)])
                    tsl = slice(b * 512, (b + 1) * 512)
                    ncols_ = len(cols)
                    wbase = wcount

                    def issue_load(ci, wbase=wbase):
                        wi_ = (wbase + ci) % 3
                        S.dma(wt[wi_][:], winb[cols[ci][0]], [("winb", cols[ci][0])], [("wt", wi_)])
                    for ci in range(min(2, ncols_)):
                        issue_load(ci)
                    for ci, (f, kind, idx) in enumerate(cols):
                        if ci + 2 < ncols_:
                            issue_load(ci + 2)
                        wb = wt[wcount % 3]
                        wk = ("wt", wcount % 3)
                        wcount += 1
                        pbk = pcount % 4
                        pcount += 1
                        pq = bank(pbk)
                        pk = ("ps", pbk)
                        for kc in range(32):
                            S.mm(pq, wb[:, kc, :], hb[:, kc, :], kc == 0, kc == 31,
                                 [wk] + [(hk, kc, t4) for t4 in range(4)], [pk])
                        if kind in ("q", "k"):
                            S.act(sqf[:], pq, AF.Square, [pk], ["sqf"])
                            S.mm(bank(4), onesf, sqf[:], True, True, ["sqf", "cst"], [("ps", 4)])
                            S.ts(rsf[:], bank(4), 1.0 / 128, EPS, ALU.mult, ALU.add, [("ps", 4)], ["rsf"])
                            S.act(rsf[:], rsf[:], AF.Sqrt, ["rsf"], ["rsf"])
                            S.op("dve", lambda: nc.vector.reciprocal(out=rsf[:], in_=rsf[:]), ["rsf"], ["rsf"])
                            gcol = 0 if kind == "q" else 1
                            S.stt(qn[:], pq, qkg_t[:, gcol:gcol + 1], rsf[:], ALU.mult, ALU.mult,
                                  [pk, "rsf", "qkg"], ["qn"])
                            S.mm(bank(5), rotT, qn[:], True, True, ["qn", "cst"], [("ps", 5)])
                            S.tt(t1[:], qn[:], cb[:], ALU.mult, ["qn", ("ct", b % 2)], ["t1"])
                            S.tt(t2[:], bank(5), sbb[:], ALU.mult, [("ps", 5), ("st", b % 2)], ["t2"])
                            o = ob[ocount % 2]
                            okk = ("ob", ocount % 2)
                            ocount += 1
                            S.tt(o[:], t1[:], t2[:], ALU.add, ["t1", "t2"], [okk])
                            dst = oq[idx][:, tsl] if kind == "q" else ok[idx][:, tsl]
                            S.dma(dst, o[:], [okk], [(tag, kind, idx, b)])
                        elif kind == "v":
                            S.cp(vT[:], pq, [pk], ["vT"], eng="act")
                            vt_ = vtile[idx % 2]
                            for t4 in range(4):
                                S.tr(psb[:, t4 * 128:(t4 + 1) * 128], vT[:, t4 * 128:(t4 + 1) * 128], identb[:],
                                     ["vT", "identb"], [("psb", 0)])
                            S.cp(vt_[:], psb[:, 0:512].rearrange("p (a d) -> p a d", a=4),
                                 [("psb", 0)], [("vtile", idx % 2)])
                            S.dma(ov[b * 512:(b + 1) * 512, idx * 128:(idx + 1) * 128].rearrange("(a p) d -> p a d", p=128),
                                  vt_[:], [("vtile", idx % 2)], [(tag, "v", idx, b)])
                        else:
                            o = of[ocount % 2]
                            okk = ("of", ocount % 2)
                            ocount += 1
                            if kind == "xl":
                                S.cp(o[:], pq, [pk], [okk], eng="act")
                                dst = oxl[idx][:, tsl]
                            elif kind == "yl":
                                S.act(o[:], pq, AF.Gelu, [pk], [okk])
                                dst = gy[sidx, idx][:, tsl]
                            elif kind == "ga":
                                S.act(o[:], pq, AF.Sigmoid, [pk], [okk])
                                dst = ga[sidx, idx][:, tsl]
                            else:
                                S.act(o[:], pq, AF.Sigmoid, [pk], [okk])
                                dst = gl[sidx, idx][:, tsl]
                            S.dma(dst, o[:], [okk], [(tag, kind, idx, b)])
                    S.flush(barrier=False)
                S.flush()

        cols_q = [(h, "q", h) for h in range(16)]
        cols_k = [(16 + g, "k", g) for g in range(4)]
        cols_v = [(20 + g, "v", g) for g in range(4)]
        cols_xl = [(24 + n, "xl", n) for n in range(16)]
        cols_yl = [(40 + n, "yl", n) for n in range(16)]
        cols_ga = [(56 + n, "ga", n) for n in range(32)]
        cols_gl = [(88 + n, "gl", n) for n in range(32)]

        if stop == "A":
            return nc
        for i in range(64):
            S.dma(ub16[i * 256:(i + 1) * 256, :], peer_u[i * 256:(i + 1) * 256, :], [], [("ub16", i)], q="pool")
        for i in range(64):
            S.dma(vb16[i * 256:(i + 1) * 256, :], peer_v[i * 256:(i + 1) * 256, :], [], [("vb16", i)], q="pool")
        phase_c("S", xs, TS, 0, cols_q + cols_k + cols_v + cols_xl + cols_yl + cols_ga + cols_gl,
                ropeC, ropeS, QT[0], KT_s, V_s, xl_s, 0)
        if stop == "CS":
            return nc
        phase_c("O", xo, TS, 1, cols_q + cols_yl + cols_ga + cols_gl, ropeCo, ropeSo, QT[1], None, None, None, 1)
        phase_c("C", xc, TC, 1, cols_k + cols_v + cols_xl, ropeC, ropeS, None, KT_c, V_c, xl_c, 1)

        def phase_d(tag, xl_ap, T, sidx, use_sel, tag_xl, gy_tag):
            nq = T // TS
            with contextlib.ExitStack() as st:
                cw = sb(st, "cw", [128, 16, 4])
                cbt = sb(st, "cbt", [128, 16])
                lam = sb(st, "lam", [128, 32])
                cA = sb(st, "cA", [128, 32])
                cA2 = sb(st, "cA2", [128, 32])
                ba_t = sb(st, "ba_t", [128, 32])
                bi_t = sb(st, "bi_t", [128, 32])
                wa_t = sb(st, "wa_t", [128, 32, 128])
                wi_t = sb(st, "wi_t", [128, 32, 128])
                xpad = sb(st, "xpad", [128, TS + 3])
                xcv = sb(st, "xcv", [128, TS])
                rg = sb(st, "rg", [128, TS])
                ig = sb(st, "ig", [128, TS])
                a_t = sb(st, "a_t", [128, TS])
                mu = sb(st, "mu", [128, TS])
                hh = sb(st, "hh", [128, TS])
                acc = sb(st, "acc", [128, TS])
                gyt = sb(st, "gyt", [128, TS])
                lo = sb(st, "lo", [128, TS], BF16)
                stt = sb(st, "stt", [128, 2])
                S.dma(cw[:], convwT[:, :, :], [], ["cw"])
                S.dma(cbt[:], convbT[:, :], [], ["cbt"])
                S.dma(lam[:], lamT[:, :], [], ["lam"])
                S.dma(ba_t[:], baT[:, :], [], ["ba"])
                S.dma(bi_t[:], biT[:, :], [], ["bi"])
                S.dma(wa_t[:], lru_wa.rearrange("n c d -> c n d"), [], ["wa_t"])
                S.dma(wi_t[:], lru_wi.rearrange("n c d -> c n d"), [], ["wi_t"])
                S.act(cA[:], lam[:], AF.Exp, ["lam"], ["cA"], scale=-1.0)
                S.act(cA[:], cA[:], AF.Ln, ["cA"], ["cA"], bias=1.0)
                S.ts(cA[:], cA[:], -8.0, None, ALU.mult, None, ["cA"], ["cA"])
                S.ts(cA2[:], cA[:], 2.0, None, ALU.mult, None, ["cA"], ["cA2"])
                for n in range(16):
                    first = True
                    for dr in range(2):
                        gi = dr * 16 + n
                        S.op("pool", lambda dr=dr: nc.gpsimd.memset(stt[:, dr:dr + 1], 0.0), [], [("stt", dr)])
                        qs = range(nq) if dr == 0 else range(nq - 1, -1, -1)
                        for q in qs:
                            lo_t = q * TS - 2
                            hi_t = q * TS + TS + 1
                            a0 = max(lo_t, 0)
                            a1 = min(hi_t, T)
                            if lo_t < 0:
                                S.op("pool", lambda: nc.gpsimd.memset(xpad[:, 0:2], 0.0), [], ["xpad"])
                            if hi_t > T:
                                S.op("pool", lambda: nc.gpsimd.memset(xpad[:, TS + 2:TS + 3], 0.0), [], ["xpad"])
                            S.dma(xpad[:, a0 - lo_t:a1 - lo_t], xl_ap[n][:, a0:a1],
                                  [(tag_xl, "xl", n, bb) for bb in range(T // 512)], ["xpad"])
                            S.ts(xcv[:], xpad[:, 0:TS], cw[:, n, 0:1], cbt[:, n:n + 1], ALU.mult, ALU.add,
                                 ["xpad", "cw", "cbt"], ["xcv"])
                            for j in range(1, 4):
                                S.stt(xcv[:], xpad[:, j:j + TS], cw[:, n, j:j + 1], xcv[:], ALU.mult, ALU.add,
                                      ["xpad", "cw", "xcv"], ["xcv"])
                            for (wt_, bt_, gt_, gk, wkey, bkey) in ((wa_t, ba_t, rg, "rg", "wa_t", "ba"), (wi_t, bi_t, ig, "ig", "wi_t", "bi")):
                                for half in range(2):
                                    pb0 = 0 if gk == "rg" else 2
                                    for blk in range(2):
                                        c0 = half * 1024 + blk * 512
                                        S.mm(bank(pb0 + blk), wt_[:, gi, :], xcv[:, c0:c0 + 512], True, True,
                                             ["xcv", wkey], [("ps", pb0 + blk)])
                                    S.act(gt_[:, half * 1024:(half + 1) * 1024], ps[:, pb0 * 512:(pb0 + 2) * 512],
                                          AF.Sigmoid, [("ps", pb0), ("ps", pb0 + 1), bkey], [gk],
                                          bias=bt_[:, gi:gi + 1])
                            S.act(a_t[:], rg[:], AF.Exp, ["rg", "cA"], ["a_t"], scale=cA[:, gi:gi + 1])
                            S.act(mu[:], rg[:], AF.Exp, ["rg", "cA2"], ["mu"], scale=cA2[:, gi:gi + 1])
                            S.ts(mu[:], mu[:], -1.0, 1.0, ALU.mult, ALU.add, ["mu"], ["mu"])
                            S.ts(mu[:], mu[:], 0.0, None, ALU.max, None, ["mu"], ["mu"])
                            S.act(mu[:], mu[:], AF.Sqrt, ["mu"], ["mu"])
                            S.tt(ig[:], ig[:], xcv[:], ALU.mult, ["ig", "xcv"], ["ig"], eng="pool")
                            S.tt(ig[:], ig[:], mu[:], ALU.mult, ["ig", "mu"], ["ig"], eng="pool")
                            if dr == 0:
                                S.op("dve", lambda: nc.vector.tensor_tensor_scan(
                                    out=hh[:], data0=a_t[:], data1=ig[:], initial=stt[:, 0:1], op0=ALU.mult, op1=ALU.add),
                                    ["a_t", "ig", ("stt", 0)], ["hh"])
                                S.cp(stt[:, 0:1], hh[:, TS - 1:TS], ["hh"], [("stt", 0)])
                            else:
                                S.op("dve", lambda: nc.vector.tensor_tensor_scan(
                                    out=hh[:, ::-1], data0=a_t[:, ::-1], data1=ig[:, ::-1], initial=stt[:, 1:2],
                                    op0=ALU.mult, op1=ALU.add),
                                    ["a_t", "ig", ("stt", 1)], ["hh"])
                                S.cp(stt[:, 1:2], hh[:, 0:1], ["hh"], [("stt", 1)])
                            sel = msel_t[:, q:q + 1] if use_sel else 1.0
                            if first:
                                S.ts(acc[:], hh[:], sel, None, ALU.mult, None, ["hh", "msel"], ["acc"])
                                first = False
                            else:
                                S.stt(acc[:], hh[:], sel, acc[:], ALU.mult, ALU.add, ["hh", "msel", "acc"], ["acc"])
                    S.dma(gyt[:], gy[sidx, n], [(gy_tag, "yl", n, bb) for bb in range(4)], ["gyt"])
                    S.tt(lo[:], acc[:], gyt[:], ALU.mult, ["acc", "gyt"], ["lo"])
                    S.dma(lruT[sidx, n], lo[:], ["lo"], [("lruT", sidx, n)])
                    S.flush(barrier=False)
                S.flush()

        if stop == "C":
            return nc
        phase_d("S", xl_s, TS, 0, False, "S", "S")
        if stop == "DS":
            return nc
        phase_d("C", xl_c, TC, 1, True, "C", "O")

        def phase_e(sidx, qtag, ktag, KT_ap, V_ap, Tk):
            nkt = Tk // 128
            with contextlib.ExitStack() as st:
                kT = sb(st, "kT", [128, Tk], BF16)
                vt = sb(st, "vt", [128, nkt, 128], BF16)
                qT = [sb(st, f"qT{i}", [128, TS], BF16) for i in range(2)]
                pT = [sb(st, f"pT{i}", [128, 512], BF16) for i in range(3)]
                rden = sb(st, "rden", [128, 512])
                ot = [sb(st, f"ot{i}", [128, 512], BF16) for i in range(2)]
                sc = 128.0 ** -0.5
                hcount = 0
                ucount = 0
                for g in range(4):
                    S.dma(kT[:], KT_ap[g], [(ktag, "k", g, bb) for bb in range(Tk // 512)], ["kT"])
                    S.dma(vt[:], V_ap[:, g * 128:(g + 1) * 128].rearrange("(kt p) d -> p kt d", p=128),
                          [(ktag, "v", g, bb) for bb in range(Tk // 512)], ["vt"])
                    for hh_ in range(4):
                        h = g * 4 + hh_
                        qb_ = qT[hcount % 2]
                        qk = ("qT", hcount % 2)
                        hcount += 1
                        S.dma(qb_[:], QT[sidx, h], [(qtag, "q", h, bb) for bb in range(4)], [qk])
                        for qb in range(4):
                            po = bank(2 + ucount % 2)
                            pok = ("ps", 2 + ucount % 2)
                            pd = bank(4 + ucount % 2)
                            pdk = ("ps", 4 + ucount % 2)
                            ucount += 1
                            qsl = qb_[:, qb * 512:(qb + 1) * 512]

                            def qk_mm(kt):
                                S.mm(bank(kt % 2), kT[:, kt * 128:(kt + 1) * 128], qsl, True, True,
                                     ["kT", qk], [("ps", kt % 2)])
                            qk_mm(0)
                            for kt in range(nkt):
                                if kt + 1 < nkt:
                                    qk_mm(kt + 1)
                                p_ = pT[kt % 3]
                                pk_ = ("pT", kt % 3)
                                S.act(p_[:], bank(kt % 2), AF.Exp, [("ps", kt % 2)], [pk_], scale=sc)
                                S.mm(po, vt[:, kt, :], p_[:], kt == 0, kt == nkt - 1, ["vt", pk_], [pok])
                                S.mm(pd, onesb[:], p_[:], kt == 0, kt == nkt - 1, ["onesb", pk_], [pdk])
                            S.op("dve", lambda pd=pd: nc.vector.reciprocal(out=rden[:], in_=pd), [pdk], ["rden"])
                            o_ = ot[ucount % 2]
                            S.tt(o_[:], po, rden[:], ALU.mult, [pok, "rden"], [("ot", ucount % 2)])
                            S.dma(attnT[sidx, h][:, qb * 512:(qb + 1) * 512], o_[:], [("ot", ucount % 2)],
                                  [("attnT", sidx, h, qb)])
                        S.flush(barrier=False)
                S.flush()

        if stop == "D":
            return nc
        phase_e(0, "S", "S", KT_s, V_s, TS)
        if stop == "ES":
            return nc
        phase_e(1, "O", "C", KT_c, V_c, TC)

        if stop == "E":
            return nc
        with contextlib.ExitStack() as st:
            aT = sb(st, "aT", [128, 16, 512], BF16)
            lT = sb(st, "lT", [128, 16, 512], BF16)
            mg = sb(st, "mg", [128, 32, 512], BF16)
            wao_t = [sb(st, f"wao{i}", [128, 16, 128], BF16) for i in range(2)]
            wlo_t = [sb(st, f"wlo{i}", [128, 16, 128], BF16) for i in range(2)]
            ga_t = [sb(st, f"ga_t{i}", [128, 512]) for i in range(2)]
            gl_t = [sb(st, f"gl_t{i}", [128, 512]) for i in range(2)]
            f1 = sb(st, "f1", [128, 512])
            f2 = sb(st, "f2", [128, 512])
            wo_t2 = [sb(st, f"wo_t{i}", [128, 32, 512], BF16) for i in range(2)]
            g1bc = sb(st, "g1bc", [128, D])
            xx = [sb(st, f"xx{i}", [128, 512]) for i in range(2)]
            xo_ = [sb(st, f"xo_{i}", [128, 512]) for i in range(2)]
            cnt = 0
            for sidx in range(2):
                x_ap = xs if sidx == 0 else xo
                S.dma(g1bc[:], modd[sidx, 0].partition_broadcast(128), [("modd", sidx, 0)], ["g1bc"])
                for b in range(4):
                    tsl = slice(b * 512, (b + 1) * 512)
                    S.dma(aT[:], attnT[sidx][:, :, tsl].rearrange("h p t -> p h t"),
                          [("attnT", sidx, h, b) for h in range(16)], ["aT"])
                    S.dma(lT[:], lruT[sidx][:, :, tsl].rearrange("h p t -> p h t"),
                          [("lruT", sidx, n) for n in range(16)], ["lT"])
                    for f in range(32):
                        i2 = f % 2
                        S.dma(wao_t[i2][:], waob[f], [("waob", f)], [("wao", i2)])
                        S.dma(wlo_t[i2][:], wlob[f], [("wlob", f)], [("wlo", i2)])
                        S.dma(ga_t[i2][:], ga[sidx, f][:, tsl], [("S" if sidx == 0 else "O", "ga", f, b)], [("ga_t", i2)])
                        S.dma(gl_t[i2][:], gl[sidx, f][:, tsl], [("S" if sidx == 0 else "O", "gl", f, b)], [("gl_t", i2)])
                        pB = bank(i2 * 2)
                        pA = bank(i2 * 2 + 1)
                        for kc in range(16):
                            S.mm(pB, wao_t[i2][:, kc, :], aT[:, kc, :], kc == 0, kc == 15, [("wao", i2), "aT"], [("ps", i2 * 2)])
                        for kc in range(16):
                            S.mm(pA, wlo_t[i2][:, kc, :], lT[:, kc, :], kc == 0, kc == 15, [("wlo", i2), "lT"], [("ps", i2 * 2 + 1)])
                        S.tt(f1[:], pB, ga_t[i2][:], ALU.mult, [("ps", i2 * 2), ("ga_t", i2)], ["f1"])
                        S.tt(f2[:], pA, gl_t[i2][:], ALU.mult, [("ps", i2 * 2 + 1), ("gl_t", i2)], ["f2"])
                        S.tt(mg[:, f, :], f1[:], f2[:], ALU.add, ["f1", "f2"], [("mg", f)], eng="pool")
                    mgk = [("mg", f) for f in range(32)]
                    S.dma(wo_t2[0][:], woutb[0], [("woutb", 0)], [("wo_t", 0)])
                    for nch in range(8):
                        if nch + 1 < 8:
                            S.dma(wo_t2[(nch + 1) % 2][:], woutb[nch + 1], [("woutb", nch + 1)], [("wo_t", (nch + 1) % 2)])
                        wo_t = wo_t2[nch % 2]
                        wok = ("wo_t", nch % 2)
                        csl = slice(nch * 512, (nch + 1) * 512)
                        for t4 in range(4):
                            i2 = cnt % 2
                            cnt += 1
                            r0 = b * 512 + t4 * 128
                            S.dma(xx[i2][:], x_ap[r0:r0 + 128, csl], [], [("xx", i2)])
                            po = bank(4 + i2)
                            for kc in range(32):
                                S.mm(po, mg[:, kc, t4 * 128:(t4 + 1) * 128], wo_t[:, kc, :], kc == 0, kc == 31,
                                     mgk + [wok], [("ps", 4 + i2)])
                            S.tt(xo_[i2][:], po, g1bc[:, csl], ALU.mult, [("ps", 4 + i2), "g1bc"], [("xo_", i2)])
                            S.tt(xo_[i2][:], xo_[i2][:], xx[i2][:], ALU.add, [("xo_", i2), ("xx", i2)], [("xo_", i2)], eng="pool")
                            S.dma(x1[sidx * TS + r0: sidx * TS + r0 + 128, csl], xo_[i2][:], [("xo_", i2)],
                                  [("x1", sidx * 16 + b * 4 + t4, nch)])
                    S.flush(barrier=False)
            S.flush()

        if stop == "F":
            return nc
        with contextlib.ExitStack() as st:
            x1t = sb(st, "x1t", [128, D])
            h2 = sb(st, "h2", [128, D])
            accp = sb(st, "accp", [128, D])
            gb = [sb(st, f"gb{i}", [128, D]) for i in range(3)]
            h2b = sb(st, "h2b", [128, D], BF16)
            h2T = sb(st, "h2T", [128, 32, 128], BF16)
            wq_t = [sb(st, f"wq_t{i}", [128, 32, 128], BF16) for i in range(2)]
            qTp = sb(st, "qTp", [128, 16, 128])
            keys_t = sb(st, "keys_t", [128, 16, 128])
            scs = sb(st, "scs", [128, 16, 128])
            wk = sb(st, "wk", [128, 256])
            stop_ = sb(st, "stop_", [128, 16, 16])
            itop = sb(st, "itop", [128, 16, 16], U32)
            itf = sb(st, "itf", [128, 16, 16])
            i0s = sb(st, "i0s", [128, 8, 16])
            cand = sb(st, "cand", [128, 8, 256])
            cidx = sb(st, "cidx", [128, 8, 256])
            bs = sb(st, "bs", [128, 8, 16])
            posu = sb(st, "posu", [128, 8, 16], U32)
            posf = sb(st, "posf", [128, 8, 16])
            eidx = sb(st, "eidx", [128, 128])
            eidi = sb(st, "eidi", [128, 128], I32)
            gsum = sb(st, "gsum", [128, 8])
            gates = sb(st, "gates", [128, 128])
            dots = sb(st, "dots", [128, 128])
            coef = sb(st, "coef", [128, 128])
            iot = sb(st, "iot", [128, 256])
            ssq2 = sb(st, "ssq2", [128, 1])
            rstd2 = sb(st, "rstd2", [128, 1])
            ival = sb(st, "ival", [128, 8, 16])
            jval = sb(st, "jval", [128, 8, 16])
            e0 = sb(st, "e0", [128, 8, 16])
            e1 = sb(st, "e1", [128, 8, 16])
            thr16 = sb(st, "thr16", [128, 16])
            S.dma(keys_t[:], keysT[:, :, :], [], ["keys_t"])
            S.dma(iot[:], iota_in[:, :], [], ["iot"])
            S.ts(thr16[:], iot[:, 0:16], 16.0, None, ALU.mult, None, ["iot"], ["thr16"])
            V = nc.vector
            gcount = 0
            gbv = [gb[i // 2][:].bitcast(BF16)[:, (i % 2) * D:(i % 2 + 1) * D] for i in range(6)]
            gbk = [("gb", i // 2, i % 2) for i in range(6)]
            ubk = [("ub16", i) for i in range(64)]
            vbk = [("vb16", i) for i in range(64)]

            def gfull(i):
                return [("gb", i, 0), ("gb", i, 1)]

            def top16(src_ap, n, vals_ap, idx_ap, rk):
                S.op("dve", lambda: V.max(out=vals_ap[:, 0:8], in_=src_ap), rk, ["tv"])
                S.op("dve", lambda: V.max_index(out=idx_ap[:, 0:8], in_max=vals_ap[:, 0:8], in_values=src_ap),
                     rk + ["tv"], ["ti"])
                S.op("dve", lambda: V.match_replace(out=wk[:, 0:n], in_to_replace=vals_ap[:, 0:8], in_values=src_ap,
                                                    imm_value=-1e30), rk + ["tv"], ["wk"])
                S.op("dve", lambda: V.max(out=vals_ap[:, 8:16], in_=wk[:, 0:n]), ["wk"], ["tv"])
                S.op("dve", lambda: V.max_index(out=idx_ap[:, 8:16], in_max=vals_ap[:, 8:16], in_values=wk[:, 0:n]),
                     ["wk", "tv"], ["ti"])

            for ti in range(32):
                sidx = ti // 16
                y_ap = ys if sidx == 0 else yo
                rloc = (ti % 16) * 128
                S.dma(x1t[:], x1[ti * 128:(ti + 1) * 128, :], [("x1", ti, nch) for nch in range(8)], ["x1t"])
                S.dma(gb[0][:], modd[sidx, 1].partition_broadcast(128), [("modd", sidx, 1)], gfull(0))
                S.dma(gb[1][:], modd[sidx, 2].partition_broadcast(128), [("modd", sidx, 2)], gfull(1))
                S.act(gb[2][:].bitcast(BF16)[:, 0:D], x1t[:], AF.Square, ["x1t"], gfull(2) + ["ssq2"], accum_out=ssq2[:])
                S.ts(rstd2[:], ssq2[:], 1.0 / D, EPS, ALU.mult, ALU.add, ["ssq2"], ["rstd2"])
                S.act(rstd2[:], rstd2[:], AF.Sqrt, ["rstd2"], ["rstd2"])
                S.op("dve", lambda: nc.vector.reciprocal(out=rstd2[:], in_=rstd2[:]), ["rstd2"], ["rstd2"])
                S.stt(h2[:], x1t[:], rstd2[:, 0:1], gb[0][:], ALU.mult, ALU.mult, ["x1t", "rstd2"] + gfull(0), ["h2"])
                S.tt(h2[:], h2[:], gb[1][:], ALU.add, ["h2"] + gfull(1), ["h2"])
                S.cp(h2b[:], h2[:], ["h2"], ["h2b"], eng="act")
                for g8 in range(4):
                    pb = g8 % 2
                    for k8 in range(8):
                        kc = g8 * 8 + k8
                        S.tr(psb[:, pb * 1024 + k8 * 128: pb * 1024 + (k8 + 1) * 128],
                             h2b[:, kc * 128:(kc + 1) * 128], identb[:], ["h2b", "identb"], [("psb", pb)])
                    S.cp(h2T[:, g8 * 8:(g8 + 1) * 8, :], psb[:, pb * 1024:(pb + 1) * 1024].rearrange("p (a t) -> p a t", a=8),
                         [("psb", pb)], [("h2T", g8)], eng=("dve" if g8 % 2 == 0 else "act"))
                h2Tk = [("h2T", g8) for g8 in range(4)]
                for f in range(16):
                    i2 = f % 2
                    S.dma(wq_t[i2][:], wqb[f], [("wqb", f)], [("wq_t", i2)])
                    pq = ps[:, i2 * 512:i2 * 512 + 128]
                    for kc in range(32):
                        S.mm(pq, wq_t[i2][:, kc, :], h2T[:, kc, :], kc == 0, kc == 31, [("wq_t", i2)] + h2Tk, [("ps", i2)])
                    S.cp(qTp[:, f, :], pq, [("ps", i2)], [("qTp", f)], eng=("dve" if f % 2 == 0 else "act"))
                for hp in range(16):
                    bk = 2 + hp // 4
                    S.mm(ps[:, bk * 512 + (hp % 4) * 128: bk * 512 + (hp % 4 + 1) * 128], qTp[:, hp, :], keys_t[:, hp, :],
                         True, True, [("qTp", hp), "keys_t"], [("ps", bk)])
                S.cp(scs[:].rearrange("p a n -> p (a n)"), ps[:, 1024:3072], [("ps", 2), ("ps", 3), ("ps", 4), ("ps", 5)], ["scs"])
                for hp in range(16):
                    top16(scs[:, hp, :], 128, stop_[:, hp, :], itop[:, hp, :], ["scs"])
                S.cp(itf[:], itop[:], ["ti"], ["itf"])
                st4 = stop_[:].rearrange("p (h two) k -> p h two k", two=2)
                it4 = itf[:].rearrange("p (h two) k -> p h two k", two=2)
                c4 = cand[:].rearrange("p h (i j) -> p h i j", i=16)
                x4 = cidx[:].rearrange("p h (i j) -> p h i j", i=16)
                S.tt(c4, st4[:, :, 0, :].unsqueeze(3).to_broadcast([128, 8, 16, 16]),
                     st4[:, :, 1, :].unsqueeze(2).to_broadcast([128, 8, 16, 16]), ALU.add, ["tv"], ["cand"])
                S.ts(i0s[:], it4[:, :, 0, :], 128.0, None, ALU.mult, None, ["itf"], ["i0s"])
                S.tt(x4, i0s[:].unsqueeze(3).to_broadcast([128, 8, 16, 16]),
                     it4[:, :, 1, :].unsqueeze(2).to_broadcast([128, 8, 16, 16]), ALU.add, ["i0s", "itf"], ["cidx"])
                for h in range(8):
                    top16(cand[:, h, :], 256, bs[:, h, :], posu[:, h, :], ["cand"])
                    S.cp(posf[:, h, :], posu[:, h, :], ["ti"], [("posf", h)])
                posk = [("posf", h) for h in range(8)]
                A4 = gb[2][:, 0:2048].rearrange("p (h k i) -> p h k i", h=8, k=16)
                B4 = gb[2][:, 2048:4096].rearrange("p (h k i) -> p h k i", h=8, k=16)
                B16 = [128, 8, 16, 16]
                pos4 = posf[:].unsqueeze(3).to_broadcast(B16)
                thr4 = thr16[:].unsqueeze(1).unsqueeze(1).to_broadcast(B16)
                iot4 = iot[:, 0:16].unsqueeze(1).unsqueeze(1).to_broadcast(B16)
                S.tt(A4, pos4, thr4, ALU.is_ge, posk + ["thr16"], gfull(2))
                S.op("dve", lambda: V.tensor_reduce(out=ival[:], in_=A4, axis=AX.X, op=ALU.add), gfull(2), ["ival"])
                S.ts(ival[:], ival[:], -1.0, None, ALU.add, None, ["ival"], ["ival"])
                S.stt(jval[:].rearrange("p h k -> p (h k)"), ival[:].rearrange("p h k -> p (h k)"), -16.0,
                      posf[:].rearrange("p h k -> p (h k)"), ALU.mult, ALU.add, ["ival"] + posk, ["jval"])
                S.tt(A4, ival[:].unsqueeze(3).to_broadcast(B16), iot4, ALU.is_equal, ["ival", "iot"], gfull(2))
                S.tt(A4, A4, it4[:, :, 0, :].unsqueeze(2).to_broadcast(B16), ALU.mult, gfull(2) + ["itf"], gfull(2))
                S.op("dve", lambda: V.tensor_reduce(out=e0[:], in_=A4, axis=AX.X, op=ALU.add), gfull(2), ["e0"])
                S.tt(B4, jval[:].unsqueeze(3).to_broadcast(B16), iot4, ALU.is_equal, ["jval", "iot"], gfull(2))
                S.tt(B4, B4, it4[:, :, 1, :].unsqueeze(2).to_broadcast(B16), ALU.mult, gfull(2) + ["itf"], gfull(2))
                S.op("dve", lambda: V.tensor_reduce(out=e1[:], in_=B4, axis=AX.X, op=ALU.add), gfull(2), ["e1"])
                eik = [("eidx", h) for h in range(8)]
                S.stt(eidx[:], e0[:].rearrange("p h k -> p (h k)"), 128.0, e1[:].rearrange("p h k -> p (h k)"),
                      ALU.mult, ALU.add, ["e0", "e1"], eik)
                bsk = ["tv"]
                g3 = gates[:].rearrange("p (h k) -> p h k", h=8)
                S.tt(g3, bs[:], bs[:, :, 0:1].to_broadcast([128, 8, 16]), ALU.subtract, bsk, ["gates"])
                S.act(gates[:], gates[:], AF.Exp, ["gates"], ["gates"])
                S.op("dve", lambda: V.tensor_reduce(out=gsum[:], in_=g3, axis=AX.X, op=ALU.add), ["gates"], ["gsum"])
                S.op("dve", lambda: V.reciprocal(out=gsum[:], in_=gsum[:]), ["gsum"], ["gsum"])
                S.tt(g3, g3, gsum[:].unsqueeze(2).to_broadcast([128, 8, 16]), ALU.mult, ["gates", "gsum"], ["gates"])
                S.ts(eidx[:], eidx[:], 0.0, 16383.0, ALU.max, ALU.min, eik, eik)
                S.cp(eidi[:], eidx[:], eik, ["eidi"])
                G = nc.gpsimd
                for e in range(128):
                    bi_ = gcount % 6
                    gcount += 1
                    S.op("pool", lambda bi_=bi_, e=e: G.indirect_dma_start(
                        out=gbv[bi_], out_offset=None, in_=ub16[:, :],
                        in_offset=bass.IndirectOffsetOnAxis(ap=eidi[:, e:e + 1], axis=0)),
                        ["eidi"] + ubk, [gbk[bi_]], dma=True)
                    S.op("dve", lambda bi_=bi_, e=e: V.scalar_tensor_tensor(
                        out=h2b[:], in0=gbv[bi_], scalar=1.0, in1=h2[:], op0=ALU.mult, op1=ALU.mult,
                        accum_out=dots[:, e:e + 1]), [gbk[bi_], "h2"], ["h2b", ("dots", e)])
                dk = [("dots", e) for e in range(128)]
                S.act(coef[:], dots[:], AF.Gelu, dk, ["coef"])
                S.tt(coef[:], coef[:], gates[:], ALU.mult, ["coef", "gates"], ["coef"])
                for e in range(128):
                    bi_ = gcount % 6
                    gcount += 1
                    S.op("pool", lambda bi_=bi_, e=e: G.indirect_dma_start(
                        out=gbv[bi_], out_offset=None, in_=vb16[:, :],
                        in_offset=bass.IndirectOffsetOnAxis(ap=eidi[:, e:e + 1], axis=0)),
                        ["eidi"] + vbk, [gbk[bi_]], dma=True)
                    if e == 0:
                        S.ts(accp[:], gbv[bi_], coef[:, 0:1], None, ALU.mult, None, [gbk[bi_], "coef"], ["accp"])
                    else:
                        S.stt(accp[:], gbv[bi_], coef[:, e:e + 1], accp[:], ALU.mult, ALU.add,
                              [gbk[bi_], "coef", "accp"], ["accp"])
                S.dma(gb[2][:], modd[sidx, 3].partition_broadcast(128), [("modd", sidx, 3)], gfull(2))
                S.tt(accp[:], accp[:], gb[2][:], ALU.mult, ["accp"] + gfull(2), ["accp"])
                S.tt(accp[:], accp[:], x1t[:], ALU.add, ["accp", "x1t"], ["accp"], eng="pool")
                S.dma(y_ap[rloc:rloc + 128, :], accp[:], ["accp"], [("y", ti)])
                S.flush(barrier=False)
            S.flush()
    return nc


def _rope_tables(T):
    pos = np.arange(T)
    rows = (pos // 64).astype(np.float32)
    cols = (pos % 64).astype(np.float32)
    inv = (10000.0 ** (-np.arange(0, 64, 2, dtype=np.float32) / 64.0)).astype(np.float32)
    ar = rows[None, :] * inv[:, None]
    ac = cols[None, :] * inv[:, None]
    C = np.concatenate([np.cos(ar), np.cos(ar), np.cos(ac), np.cos(ac)], axis=0).astype(np.float32)
    Sn = np.concatenate([np.sin(ar), np.sin(ar), np.sin(ac), np.sin(ac)], axis=0).astype(np.float32)
    return np.ascontiguousarray(C), np.ascontiguousarray(Sn)


def _consts():
    ident = np.eye(128, dtype=np.float32)
    R = np.zeros((128, 128), np.float32)
    for base in (0, 64):
        for i in range(32):
            R[base + i, base + 32 + i] = -1.0
            R[base + 32 + i, base + i] = 1.0
    rotT = np.ascontiguousarray(R.T)
    ones = np.ones((128, 128), np.float32)
    return np.ascontiguousarray(np.stack([ident, rotT, ones], axis=1))


def make_in_maps(inp):
    f = lambda a: np.ascontiguousarray(np.asarray(a, dtype=np.float32))
    C, Sn = _rope_tables(TC)
    cst = _consts()
    iota = np.ascontiguousarray(np.tile(np.arange(256, dtype=np.float32)[None, :], (128, 1)))
    fm = lambda v, nch: np.ascontiguousarray(f(v).reshape(nch, 128).T)
    shared = {
        "w_ada": f(inp["w_ada"][0]), "badaT": fm(inp["b_ada"][0], 192),
        "g1T": fm(inp["g_norm1"][0], 32), "g2T": fm(inp["g_norm2"][0], 32),
        "w_in": f(inp["w_in"][0]),
        "qkg": np.ascontiguousarray(np.stack([f(inp["q_gain"][0]), f(inp["k_gain"][0])], axis=1)),
        "convwT": np.ascontiguousarray(f(inp["conv_w"][0]).reshape(4, 16, 128).transpose(2, 1, 0)),
        "convbT": fm(inp["conv_b"][0], 16),
        "lamT": np.ascontiguousarray(f(inp["lru_lam"][0]).reshape(32, 128).T),
        "baT": np.ascontiguousarray(f(inp["lru_ba"][0]).reshape(32, 128).T),
        "biT": np.ascontiguousarray(f(inp["lru_bi"][0]).reshape(32, 128).T),
        "lru_wa": f(inp["lru_wa"][0]).reshape(32, 128, 128),
        "lru_wi": f(inp["lru_wi"][0]).reshape(32, 128, 128),
        "w_attn_o": f(inp["w_attn_o"][0]), "w_lru_o": f(inp["w_lru_o"][0]), "w_out": f(inp["w_out"][0]),
        "peer_wq": f(inp["peer_wq"][0]),
        "keysT": np.ascontiguousarray(f(inp["peer_keys"][0]).reshape(16, 128, 128).transpose(2, 0, 1)),
        "peer_u": f(inp["peer_u"][0]), "peer_v": f(inp["peer_v"][0]),
        "ropeC": C, "ropeS": Sn, "consts": cst, "iota256": iota,
    }
    xp = f(inp["x_prompt"])
    xsm = f(inp["x_sample"])
    cp = f(inp["c_prompt"])
    csm = f(inp["c_sample"])
    maps = []
    for c in range(8):
        b, j = c // 4, c % 4
        cc = np.stack([csm[c], cp[b]], axis=0)
        cTl = np.ascontiguousarray(cc.reshape(2, 32, 128).transpose(2, 1, 0))
        ms = np.zeros((128, 4), np.float32)
        ms[:, j] = 1.0
        m = dict(shared)
        m.update({
            "xs": xsm[c], "xo": np.ascontiguousarray(xp[b, j * TS:(j + 1) * TS]), "xc": xp[b],
            "cT": cTl, "msel": ms,
            "ropeCo": np.ascontiguousarray(C[:, j * TS:(j + 1) * TS]),
            "ropeSo": np.ascontiguousarray(Sn[:, j * TS:(j + 1) * TS]),
        })
        maps.append(m)
    return maps


def kernel(**inputs):
    nc = build()
    maps = make_in_maps(inputs)
    res = run_bass_kernel_spmd(nc, maps, core_ids=list(range(8)))
    y_prompt = np.zeros((2, 8192, D), np.float32)
    y_sample = np.zeros((8, TS, D), np.float32)
    for c in range(8):
        b, j = c // 4, c % 4
        y_sample[c] = res.results[c]["ys"]
        y_prompt[b, j * TS:(j + 1) * TS] = res.results[c]["yo"]
    return (y_prompt, y_sample)
```
